# Optimizing a Trainium2 kernel written in Bass

```python
import math
import jax
import jax.numpy as jnp
from jax import lax
import numpy as np

D_MODEL = 1024
BATCH = 16
SEQ = 2048
DEPTH = 2

N_MIXERS = 2
ATTN_PATTERNS = ((128, 1), (512, 4), (2048, 16))
N_ATTN_GROUPS = len(ATTN_PATTERNS)
HEADS_PER_GROUP = 8
HEAD_DIM = 64
ATTN_WIDTH = HEADS_PER_GROUP * HEAD_DIM
ROT_DIM = HEAD_DIM // 4
ROPE_THETA = 500000.0
ATTN_BLOCK = 128
POOL_WINDOWS = (2, 4, 8, 16)
N_POOL_GROUPS = len(POOL_WINDOWS)
POOL_GROUP_DIM = D_MODEL // N_POOL_GROUPS
N_EXPERT_GROUPS = 4
EXPERTS_PER_GROUP = 4
N_EXPERTS = N_EXPERT_GROUPS * EXPERTS_PER_GROUP
EXPERT_TOP_K = 2
D_EXPERT = D_MODEL // 2
MOE_BLOCK = 256
RMS_EPS = 1e-6

kernel_name = "hybrid_dilated_attn_pool_hmoe"


def rmsnorm(x, g):
    xf = x.astype(jnp.float32)
    y = xf * lax.rsqrt(jnp.mean(xf * xf, axis=-1, keepdims=True) + RMS_EPS)
    return (y * g.astype(jnp.float32)).astype(x.dtype)


def rotary_tables(positions):
    inv_freq = ROPE_THETA ** (-(jnp.arange(ROT_DIM // 2, dtype=jnp.float32) * 2.0 / ROT_DIM))
    ang = positions.astype(jnp.float32)[..., None] * inv_freq
    return jnp.cos(ang)[:, :, None, :], jnp.sin(ang)[:, :, None, :]


def partial_rotary(t, cos, sin):
    half = ROT_DIM // 2
    cos = cos.astype(t.dtype)
    sin = sin.astype(t.dtype)
    t1, t2, rest = t[..., :half], t[..., half:ROT_DIM], t[..., ROT_DIM:]
    return jnp.concatenate([t1 * cos - t2 * sin, t2 * cos + t1 * sin, rest], axis=-1)


def dilated_window_attention(q, k, v, dil, steps):
    B, S, H, Dh = q.shape
    L = S // dil
    nb = -(-L // ATTN_BLOCK)
    Lp = nb * ATTN_BLOCK

    def to_phase(t):
        t = t.reshape(B, L, dil, H, Dh).transpose(0, 2, 3, 1, 4).reshape(B * dil, H, L, Dh)
        t = jnp.pad(t, ((0, 0), (0, 0), (0, Lp - L), (0, 0)))
        return t.reshape(B * dil, H, nb, ATTN_BLOCK, Dh)

    def with_prev(t):
        prev = jnp.pad(t[:, :, :-1], ((0, 0), (0, 0), (1, 0), (0, 0), (0, 0)))
        return jnp.concatenate([prev, t], axis=3)

    qb = to_phase(q)
    kw = with_prev(to_phase(k))
    vw = with_prev(to_phase(v))
    s = jnp.einsum('nhbqd,nhbkd->nhbqk', qb, kw).astype(jnp.float32) * (1.0 / math.sqrt(Dh))
    blk = jnp.arange(nb)[:, None, None] * ATTN_BLOCK
    qpos = blk + jnp.arange(ATTN_BLOCK)[None, :, None]
    kpos = blk - ATTN_BLOCK + jnp.arange(2 * ATTN_BLOCK)[None, None, :]
    dist = qpos - kpos
    valid = (dist >= 0) & (dist <= steps) & (kpos >= 0)
    s = jnp.where(valid, s, -jnp.inf)
    lse = jax.nn.logsumexp(s, axis=-1)
    p = jnp.exp(s - lse[..., None]).astype(v.dtype)
    o = jnp.einsum('nhbqk,nhbkd->nhbqd', p, vw)
    o = o.reshape(B, dil, H, Lp, Dh)[:, :, :, :L].transpose(0, 3, 1, 2, 4).reshape(B, S, H, Dh)
    lse = lse.reshape(B, dil, H, Lp)[:, :, :, :L].transpose(0, 3, 1, 2).reshape(B, S, H)
    return o, lse


def dilated_attention_mixer(y, w_in, w_out, cos, sin):
    B, S, _ = y.shape
    qkv = (y @ w_in).reshape(B, S, N_ATTN_GROUPS, 3, HEADS_PER_GROUP, HEAD_DIM)
    outs, lses = [], []
    for g, (window, dil) in enumerate(ATTN_PATTERNS):
        q = partial_rotary(qkv[:, :, g, 0], cos, sin)
        k = partial_rotary(qkv[:, :, g, 1], cos, sin)
        o, lse = dilated_window_attention(q, k, qkv[:, :, g, 2], dil, window // dil)
        outs.append(o)
        lses.append(lse)
    wts = jax.nn.softmax(jnp.stack(lses, axis=0), axis=0)
    o = jnp.sum(wts[..., None].astype(y.dtype) * jnp.stack(outs, axis=0), axis=0)
    return o.reshape(B, S, ATTN_WIDTH) @ w_out


def multiscale_pool_mixer(y, w_in, w_group, scale, w_out):
    B, S, D = y.shape
    u = (y @ w_in).reshape(B, S, N_POOL_GROUPS, POOL_GROUP_DIM)
    c = jnp.cumsum(u.astype(jnp.float32), axis=1)
    t1 = jnp.arange(S, dtype=jnp.int32) + 1
    outs = []
    for g, w in enumerate(POOL_WINDOWS):
        cg = c[:, :, g]
        shifted = jnp.pad(cg[:, :S - w], ((0, 0), (w, 0), (0, 0)))
        count = jnp.minimum(t1, w).astype(jnp.float32)[None, :, None]
        outs.append((cg - shifted) / count - u[:, :, g].astype(jnp.float32))
    pooled = jnp.stack(outs, axis=2).astype(y.dtype)
    z = jnp.einsum('bsgc,gce->bsge', pooled, w_group).reshape(B, S, D) * scale
    return z @ w_out


def routed_experts(xf, expert_idx, gates, w1, w3, w2):
    T, Dm = xf.shape
    K = expert_idx.shape[1]
    E = w1.shape[0]
    N = T * K
    flat_e = expert_idx.reshape(N)
    flat_tok = jnp.repeat(jnp.arange(T, dtype=jnp.int32), K)
    flat_g = gates.reshape(N).astype(xf.dtype)
    order = jnp.argsort(flat_e)
    se = flat_e[order]
    counts = jnp.bincount(flat_e, length=E)
    starts = jnp.cumsum(counts) - counts
    padded = ((counts + MOE_BLOCK - 1) // MOE_BLOCK) * MOE_BLOCK
    pends = jnp.cumsum(padded)
    pstarts = pends - padded
    dest = pstarts[se] + (jnp.arange(N, dtype=jnp.int32) - starts[se])
    n_blocks = -(-(N + E * (MOE_BLOCK - 1)) // MOE_BLOCK)
    P = n_blocks * MOE_BLOCK
    row_tok = jnp.full((P,), T, dtype=jnp.int32).at[dest].set(flat_tok[order])
    row_gate = jnp.zeros((P,), xf.dtype).at[dest].set(flat_g[order])
    block_e = jnp.minimum(
        jnp.searchsorted(pends, jnp.arange(n_blocks, dtype=jnp.int32) * MOE_BLOCK, side='right'), E - 1)
    xs = jnp.concatenate([xf, jnp.zeros((1, Dm), xf.dtype)], axis=0)[row_tok]
    xs = xs.reshape(n_blocks, MOE_BLOCK, Dm)

    def expert_block(args):
        xb, e = args
        hmid = jax.nn.silu(xb @ w1[e]) * (xb @ w3[e])
        return hmid @ w2[e]

    ys = lax.map(expert_block, (xs, block_e)).reshape(P, Dm) * row_gate[:, None]
    return jnp.zeros((T + 1, Dm), ys.dtype).at[row_tok].add(ys)[:T]


def hierarchical_moe(y, wg, bg, we, be, w1, w3, w2):
    B, S, D = y.shape
    xf = y.reshape(B * S, D)
    lg = (xf @ wg).astype(jnp.float32) + bg.astype(jnp.float32)
    pg = jax.nn.softmax(lg, axis=-1)
    g_star = jnp.argmax(pg, axis=-1).astype(jnp.int32)
    gate1 = jnp.take_along_axis(pg, g_star[:, None], axis=1)[:, 0]
    le = jnp.einsum('td,gde->tge', xf, we).astype(jnp.float32) + be.astype(jnp.float32)
    le = jnp.take_along_axis(le, g_star[:, None, None], axis=1)[:, 0]
    top_vals, top_idx = lax.top_k(le, EXPERT_TOP_K)
    gates = gate1[:, None] * jax.nn.softmax(top_vals, axis=-1)
    expert_idx = g_star[:, None] * EXPERTS_PER_GROUP + top_idx.astype(jnp.int32)
    out = routed_experts(xf, expert_idx, gates, w1, w3, w2)
    return out.reshape(B, S, D)


def setup_inputs(seed: int = 0) -> dict:
    key = jax.random.key(seed)
    ks = jax.random.split(key, 20)
    n_a = (DEPTH + 1) // 2
    n_b = DEPTH // 2
    D = D_MODEL
    nrm = jax.random.normal
    x = nrm(ks[0], (BATCH, SEQ, D), jnp.float32)
    offs = jax.random.randint(ks[1], (BATCH, 1), 0, 4096, dtype=jnp.int32)
    positions = (offs + jnp.arange(SEQ, dtype=jnp.int32)[None, :]).astype(jnp.int32)
    return {
        "x": x,
        "positions": positions,
        "norm_mix": 1.0 + 0.05 * nrm(ks[2], (DEPTH, D), jnp.float32),
        "norm_ffn": 1.0 + 0.05 * nrm(ks[3], (DEPTH, D), jnp.float32),
        "norm_final": 1.0 + 0.05 * nrm(ks[4], (D,), jnp.float32),
        "attn_w_in": nrm(ks[5], (n_a, D, N_ATTN_GROUPS * 3 * ATTN_WIDTH), jnp.float32) * D ** -0.5,
        "attn_w_out": nrm(ks[6], (n_a, ATTN_WIDTH, D), jnp.float32) * ATTN_WIDTH ** -0.5,
        "pool_w_in": nrm(ks[7], (n_b, D, D), jnp.float32) * D ** -0.5,
        "pool_w_group": nrm(ks[8], (n_b, N_POOL_GROUPS, POOL_GROUP_DIM, POOL_GROUP_DIM), jnp.float32) * POOL_GROUP_DIM ** -0.5,
        "pool_scale": 1.0 + 0.1 * nrm(ks[9], (n_b, D), jnp.float32),
        "pool_w_out": nrm(ks[10], (n_b, D, D), jnp.float32) * D ** -0.5,
        "router_group_w": nrm(ks[11], (DEPTH, D, N_EXPERT_GROUPS), jnp.float32) * D ** -0.5,
        "router_group_b": 0.01 * nrm(ks[12], (DEPTH, N_EXPERT_GROUPS), jnp.float32),
        "router_expert_w": nrm(ks[13], (DEPTH, N_EXPERT_GROUPS, D, EXPERTS_PER_GROUP), jnp.float32) * D ** -0.5,
        "router_expert_b": 0.01 * nrm(ks[14], (DEPTH, N_EXPERT_GROUPS, EXPERTS_PER_GROUP), jnp.float32),
        "expert_w1": nrm(ks[15], (DEPTH, N_EXPERTS, D, D_EXPERT), jnp.float32) * D ** -0.5,
        "expert_w3": nrm(ks[16], (DEPTH, N_EXPERTS, D, D_EXPERT), jnp.float32) * D ** -0.5,
        "expert_w2": nrm(ks[17], (DEPTH, N_EXPERTS, D_EXPERT, D), jnp.float32) * D_EXPERT ** -0.5,
    }


def reference(x, positions, norm_mix, norm_ffn, norm_final, attn_w_in, attn_w_out,
              pool_w_in, pool_w_group, pool_scale, pool_w_out,
              router_group_w, router_group_b, router_expert_w, router_expert_b,
              expert_w1, expert_w3, expert_w2):
    cos, sin = rotary_tables(positions)
    h = x
    for i in range(DEPTH):
        y = rmsnorm(h, norm_mix[i])
        j = i // N_MIXERS
        if i % N_MIXERS == 0:
            h = h + dilated_attention_mixer(y, attn_w_in[j], attn_w_out[j], cos, sin)
        else:
            h = h + multiscale_pool_mixer(y, pool_w_in[j], pool_w_group[j], pool_scale[j], pool_w_out[j])
        y = rmsnorm(h, norm_ffn[i])
        h = h + hierarchical_moe(y, router_group_w[i], router_group_b[i],
                                 router_expert_w[i], router_expert_b[i],
                                 expert_w1[i], expert_w3[i], expert_w2[i])
    return rmsnorm(h, norm_final)
```

```python
from contextlib import ExitStack
import math
import numpy as np
import ml_dtypes
import concourse.bass as bass
import concourse.mybir as mybir
from concourse.bass_utils import run_bass_kernel_spmd

F32 = mybir.dt.float32
BF16 = mybir.dt.bfloat16
I32 = mybir.dt.int32
ALU = mybir.AluOpType
AF = mybir.ActivationFunctionType
AX = mybir.AxisListType

NCORES = 8
SEQ = 2048
D = 1024
TPC = 4096
NT = 32
CAP_T = 5
CAP = CAP_T * 128
NEXP = 16
EPS = 1e-6
DILS = (1, 4, 16)
CUT = [0]
ZERO_FROM_TILE = 3
ATT_CFG = [3, 3]
M2_LAYOUT = [0]
M2_BANKS = [dict(h=[0], y=2, xt=[4, 5], ht=[6, 7]), dict(h=[0, 2], y=4, xt=[6], ht=[7])]

ENGS = ("pe", "act", "dve", "pool", "sp")
EPOCH = 12000
RING = {"sp": 40, "pool": 24, "act": 8}
DEF_COST = {"pe": 0.23, "act": 0.6, "dve": 0.6, "pool": 1.0, "sp": 0.1}
DEF_COST_DMA = 4.0
DMA_ISSUE = 0.3


class Buf:
    __slots__ = ("name", "w", "rs", "rd", "excl")

    def __init__(self, name):
        self.name = name
        self.excl = False
        self.w = []
        self.rs = []
        self.rd = []


class Op:
    __slots__ = ("eng", "fn", "deps", "needs_inc", "is_dma", "sem", "semval", "pre", "lidx", "seg", "cost", "fin")

    def __init__(self, eng, fn, is_dma):
        self.eng = eng
        self.fn = fn
        self.is_dma = is_dma
        self.lidx = 0
        self.seg = 0
        self.cost = 0.3
        self.fin = 0.0
        self.deps = []
        self.needs_inc = is_dma
        self.sem = None
        self.semval = None
        self.pre = None


class Prog:
    def __init__(self, nc):
        self.nc = nc
        self.es = ExitStack()
        self.streams = {e: [] for e in ENGS}
        self.ring = {}
        self.ring_n = {e: 0 for e in RING}
        for e, k in RING.items():
            self.ring[e] = [self._sem(f"dq_{e}_{i}") for i in range(k)]
        self.eng_sems = {e: [] for e in ENGS}
        self.nbuf = 0
        self.live_dma = []
        self.nops = 0
        self.seg = 0

    def _sem(self, name):
        return self.es.enter_context(self.nc.semaphore(name))

    def buf(self, name=None):
        self.nbuf += 1
        return Buf(name or f"b{self.nbuf}")

    def bufs(self, n):
        return [self.buf() for _ in range(n)]

    def _record(self, eng, fn, reads, writes, is_dma, c=None):
        o = Op(eng, fn, is_dma)
        self.nops += 1
        o.lidx = self.nops
        o.seg = self.seg
        o.cost = c if c is not None else (DEF_COST_DMA if is_dma else DEF_COST[eng])
        deps = {}
        ex = [b for b in reads if b.excl]
        if ex:
            reads = [b for b in reads if not b.excl]
            writes = list(writes) + [b for b in ex if b not in writes]
        for b in reads:
            for w_ in b.w:
                deps[id(w_)] = w_
        acc = []
        for b in writes:
            if is_dma and b.w and all(w_.is_dma for w_ in b.w) and not b.rs and not b.rd:
                acc.append(b)
                continue
            for w_ in b.w:
                deps[id(w_)] = w_
            for r in b.rs:
                deps[id(r)] = r
            for r in b.rd:
                deps[id(r)] = r
        o.deps = list(deps.values())
        for d in o.deps:
            d.needs_inc = True
        for b in reads:
            if is_dma:
                b.rd.append(o)
            else:
                b.rs.append(o)
        for b in writes:
            if b in acc:
                b.w.append(o)
            else:
                b.w = [o]
                b.rs = []
                b.rd = []
        if is_dma:
            self.live_dma.append(o)
        self.streams[eng].append(o)
        return o

    def op(self, eng, fn, reads=(), writes=(), c=None):
        return self._record(eng, fn, reads, writes, False, c)

    def dma(self, eng, fn, reads=(), writes=(), c=None):
        return self._record(eng, fn, reads, writes, True, c)

    def barrier(self):
        deps = list(self.live_dma)
        self.live_dma = []
        for d in deps:
            d.needs_inc = True
        self.seg += 1
        for e in ENGS:
            o = Op(e, lambda eng: eng.nop(), False)
            o.deps = list(deps)
            self.nops += 1
            o.lidx = self.nops
            o.seg = self.seg
            o.cost = 0.05
            self.streams[e].append(o)
        self.seg += 1

    def schedule(self):
        WINDOW = 48
        segs = {}
        for e in ENGS:
            for o in self.streams[e]:
                segs.setdefault(o.seg, {}).setdefault(e, []).append(o)
        final = {e: [] for e in ENGS}
        tnow = 0.0
        done = set()
        for sg in sorted(segs):
            per = segs[sg]
            if sg % 2 == 1:
                tails = [final[e2][-1 - i] for e2 in ENGS for i in range(min(len(final[e2]), 1))]
                tails = []
                for e2 in ENGS:
                    for o2 in reversed(final[e2]):
                        if not o2.is_dma:
                            tails.append(o2)
                            break
                for e in ENGS:
                    for o in per.get(e, []):
                        o.deps = list(o.deps) + tails
                        for d in tails:
                            d.needs_inc = True
                        o.fin = tnow
                        done.add(id(o))
                        final[e].append(o)
                continue
            et = {e: tnow for e in ENGS}
            pend = {e: list(per.get(e, [])) for e in ENGS}
            nleft = sum(len(v) for v in pend.values())
            while nleft:
                best = None
                for e in ENGS:
                    lst = pend[e]
                    for i in range(min(len(lst), WINDOW)):
                        o = lst[i]
                        st = et[e]
                        ok = True
                        for d in o.deps:
                            if id(d) not in done:
                                ok = False
                                break
                            if d.fin > st:
                                st = d.fin
                        if not ok:
                            continue
                        key = (st, o.lidx)
                        if best is None or key < best[0]:
                            best = (key, e, i, o, st)
                        if st <= et[e]:
                            break
                assert best is not None, "scheduler deadlock"
                _, e, i, o, st = best
                pend[e].pop(i)
                nleft -= 1
                if o.is_dma:
                    o.fin = st + o.cost
                    et[e] = st + DMA_ISSUE
                else:
                    o.fin = st + o.cost
                    et[e] = o.fin
                done.add(id(o))
                final[e].append(o)
            tnow = max([tnow] + [o.fin for e in ENGS for o in per.get(e, [])])
        self.streams = final
        self.est_us = tnow

    def finalize(self):
        nc = self.nc
        self.schedule()
        for e in RING:
            k = len(self.ring[e])
            i = 0
            for o in self.streams[e]:
                if not o.is_dma:
                    continue
                o.sem = self.ring[e][i % k]
                o.semval = 16 * (i // k + 1)
                if i >= k:
                    o.pre = (o.sem, 16 * (i // k))
                i += 1
        for e in ENGS:
            cnt = 0
            for o in self.streams[e]:
                if o.is_dma or not o.needs_inc:
                    continue
                ep = cnt // EPOCH
                while len(self.eng_sems[e]) <= ep:
                    self.eng_sems[e].append(self._sem(f"cs_{e}_{len(self.eng_sems[e])}"))
                o.sem = self.eng_sems[e][ep]
                o.semval = cnt % EPOCH + 1
                cnt += 1
        handles = {"pe": "tensor", "act": "scalar", "dve": "vector", "pool": "gpsimd", "sp": "sync"}
        stats = {}
        with nc.Block() as block:
            for e in ENGS:
                ops = self.streams[e]
                if not ops:
                    continue

                def body(eng, ops=ops, e=e):
                    waited = {}
                    nw = 0
                    for o in ops:
                        ws = []
                        if o.pre is not None:
                            ws.append(o.pre)
                        for d in o.deps:
                            if d.eng == e and e == "pe" and not d.is_dma:
                                continue
                            ws.append((d.sem, d.semval))
                        for (s, v) in ws:
                            key = id(s)
                            if waited.get(key, 0) >= v:
                                continue
                            waited[key] = v
                            eng.wait_ge(s, v)
                            nw += 1
                        ins = o.fn(eng)
                        if o.needs_inc:
                            ins.then_inc(o.sem, 16 if o.is_dma else 1)
                    for o in ops:
                        if o.is_dma:
                            key = id(o.sem)
                            if waited.get(key, 0) < o.semval:
                                waited[key] = o.semval
                                eng.wait_ge(o.sem, o.semval)
                    stats[e] = (len(ops), nw)

                getattr(block, handles[e])(body)
        self.stats = stats
        return stats


class Arena:
    def __init__(self, ap, n):
        self.ap = ap
        self.n = n
        self.off = 0
        self.top = n

    def top_reset(self):
        self.top = self.n

    def top_bf16(self, n):
        w = (n + 3) // 4 * 2
        self.top -= w
        assert self.off <= self.top, ("arena overflow (top)", self.off, self.top)
        return self.ap[:, self.top:self.top + w].bitcast(BF16)[:, 0:n]

    def mark(self):
        return self.off

    def release(self, m):
        self.off = m

    def f32(self, n):
        n2 = (n + 1) // 2 * 2
        assert self.off + n2 <= self.top, ("arena overflow", self.off, n2, self.top)
        a = self.ap[:, self.off:self.off + n]
        self.off += n2
        return a

    def bf16(self, n):
        w = (n + 3) // 4 * 2
        assert self.off + w <= self.top, ("arena overflow", self.off, w, self.top)
        a = self.ap[:, self.off:self.off + w].bitcast(BF16)[:, 0:n]
        self.off += w
        return a

    def i32(self, n):
        return self.f32(n).bitcast(I32)


def v3(ap, a, b):
    return ap.rearrange("p (a b) -> p a b", a=a, b=b)


def v4(ap, a, b, c):
    return ap.rearrange("p (a b c) -> p a b c", a=a, b=b, c=c)


def build_program(dbg=False, stop_after=None):
    nc = bass.Bass("TRN2", target_bir_lowering=False)
    P = Prog(nc)

    def din(name, shape, dt=F32):
        return nc.dram_tensor(name, list(shape), dt, kind="ExternalInput").ap()

    def dscr(name, shape, dt, out=False):
        if out:
            return nc.dram_tensor(name, list(shape), dt, kind="ExternalOutput").ap()
        return nc.dram_tensor(name, list(shape), dt).ap()

    x_d = din("x", [TPC, D])
    posT_d = din("posT", [128, NT], I32)
    nmix_d = din("norm_mix", [2, D])
    nffn_d = din("norm_ffn", [2, D])
    nfin_d = din("norm_final", [1, D])
    awin_d = din("attn_w_in", [D, 4608])
    awout_d = din("attn_w_out", [512, D])
    pwin_d = din("pool_w_in", [D, D])
    pwg_d = din("pool_w_group", [4, 256, 256])
    pscT_d = din("pool_scaleT", [128, 8])
    pwout_d = din("pool_w_out", [D, D])
    rgw_d = din("router_group_w", [2, D, 4])
    rgb_d = din("router_group_b", [2, 4])
    rew_d = din("router_expert_w", [2, 4, D, 4])
    reb_d = din("router_expert_b", [2, 16])
    w1_d = din("expert_w1", [2, NEXP, D, 512])
    w3_d = din("expert_w3", [2, NEXP, D, 512])
    w2_d = din("expert_w2", [2, NEXP, 512, D])
    c_identf_d = din("c_identf", [128, 128])
    c_mask_d = din("c_mask", [128, 512])
    c_triu_d = din("c_triu", [128, 128])
    c_ones_d = din("c_ones", [128, 128])
    c_invf_d = din("c_invf", [128, 8])
    c_eoff_d = din("c_eoff", [128, 16])
    c_rc_d = din("c_rc", [128, 16])
    c_tokp1_d = din("c_tokp1", [128, NT])
    c_slot_d = din("c_slot", [128, NEXP * CAP_T])
    out_d = nc.dram_tensor("out", [TPC, D], F32, kind="ExternalOutput").ap()

    H1 = dscr("H1", [TPC, D], F32, out=dbg)
    H2 = dscr("H2", [TPC, D], F32, out=dbg)
    H3 = dscr("H3", [TPC, D], F32, out=dbg)
    OGZ = [dscr(f"OGZ{g}", [TPC, 264], F32) for g in range(3)]
    XS = dscr("XS", [NEXP * CAP, 516], F32)
    YK = dscr("YK", [2 * TPC, D], BF16)
    bH = {id(h): P.bufs(NT) for h in (H1, H2, H3)}
    bOGZ = [P.bufs(NT) for _ in range(3)]
    bXS = P.buf()
    bYK = P.buf()
    bYKz = P.buf()
    bOUT = P.buf()

    ARENA_N = 46000
    arena_t = P.es.enter_context(nc.sbuf_tensor("arena", [128, ARENA_N], F32))
    A = Arena(arena_t, ARENA_N)
    ps_t = P.es.enter_context(nc.psum_tensor("ps", [128, 4096], F32))

    def bank(i, n=1):
        return ps_t[:, i * 512:(i + n) * 512]

    def bank_bf(i):
        return ps_t[:, i * 512:(i + 1) * 512].bitcast(BF16)

    pb = P.bufs(8)
    PF = {}
    for b_ in pb:
        b_.excl = True
    _bc = {}

    def bc_reg(e, val=None):
        val = NEXP * CAP - 1 if val is None else val
        if val not in _bc:
            _bc[val] = e.to_reg(val)
        return _bc[val]

    identf = A.f32(128)
    identb = A.bf16(128)
    maskb = A.bf16(512)
    bconst = P.buf()
    P.dma("sp", lambda e: e.dma_start(out=identf, in_=c_identf_d), writes=[bconst])
    zt = A.f32(516)
    bzt = P.buf()
    bXSz = P.buf()
    tmpm = zt[:, 0:512]
    P.dma("sp", lambda e: e.dma_start(out=tmpm, in_=c_mask_d), writes=[bzt])
    P.op("dve", lambda e: e.tensor_copy(out=identb, in_=identf), reads=[bconst], writes=[bconst])
    P.op("dve", lambda e: e.tensor_copy(out=maskb, in_=tmpm), reads=[bconst, bzt], writes=[bconst])
    P.op("pool", lambda e: e.memset(zt, 0.0), writes=[bzt])
    XSv = XS.rearrange("(n p) d -> n p d", p=128)
    zero_state = {"jobs": []}

    def xs_zero_begin():
        zero_state["jobs"] = [ex_ * CAP_T + j_ for ex_ in range(NEXP) for j_ in range(CAP_T) if j_ >= ZERO_FROM_TILE]

    def xs_zero_some(n, after=()):
        for _ in range(n):
            if zero_state["jobs"]:
                n_ = zero_state["jobs"].pop(0)
                P.dma("sp", lambda e, n_=n_: e.dma_start(out=XSv[n_], in_=zt), reads=[bzt, bXS] + list(after), writes=[bXSz], c=4.0)
    persist_mark = A.mark()

    def rmsnorm_tile(xt, bx, gbt, bg, outs, junk, bjunk, ss, rs, bss):
        ss = ss[:, 0:1]
        rs = rs[:, 0:1]
        P.op("act", lambda e: e.activation(out=junk, in_=xt, func=AF.Square, accum_out=ss),
             reads=[bx], writes=[bjunk, bss])
        P.op("act", lambda e: e.activation(out=rs, in_=ss, func=AF.Sqrt, scale=1.0 / D, bias=EPS),
             reads=[bss], writes=[bss])
        P.op("dve", lambda e: e.reciprocal(out=rs, in_=rs), reads=[bss], writes=[bss])
        for (o_ap, o_b) in outs:
            P.op("dve", lambda e, o_ap=o_ap: e.scalar_tensor_tensor(
                out=o_ap, in0=xt, scalar=rs[:, 0:1], in1=gbt, op0=ALU.mult, op1=ALU.mult),
                reads=[bx, bss, bg], writes=[o_b])

    def seq_norm_transpose(src, bsrc_tiles, s, gbt, bg, yT, byT, tl):
        for t in range(16):
            gt = s * 16 + t
            xt, bx = tl["xt"][t % 2], tl["bxt"][t % 2]
            yb, byb = tl["yb"][t % 2], tl["byb"][t % 2]
            P.dma("sp", lambda e, xt=xt, gt=gt: e.dma_start(out=xt, in_=src[gt * 128:(gt + 1) * 128, :]),
                  reads=[bsrc_tiles[gt]], writes=[bx])
            rmsnorm_tile(xt, bx, gbt, bg, [(yb, byb)], tl["junk"], tl["bjunk"], tl["ss"], tl["rs"], tl["bss"])
            bk = 4 + (t % 2)
            pbf = bank_bf(bk)
            for k in range(8):
                P.op("pe", lambda e, k=k, pbf=pbf, yb=yb: e.transpose(
                    out=pbf[:, k * 128:(k + 1) * 128], in_=yb[:, k * 128:(k + 1) * 128], identity=identb),
                    reads=[byb, bconst], writes=[pb[bk]])
            eng = "act" if t % 2 == 0 else "dve"
            if eng == "act":
                P.op("act", lambda e, pbf=pbf, t=t: e.copy(out=yT[:, :, t * 128:(t + 1) * 128], in_=v3(pbf, 8, 128)),
                     reads=[pb[bk]], writes=[byT[t]])
            else:
                P.op("dve", lambda e, pbf=pbf, t=t: e.tensor_copy(out=yT[:, :, t * 128:(t + 1) * 128], in_=v3(pbf, 8, 128)),
                     reads=[pb[bk]], writes=[byT[t]])

    def attn_phase():
        A.release(persist_mark)
        gbt = A.f32(D)
        bg = P.buf()
        P.dma("sp", lambda e: e.dma_start(out=gbt, in_=nmix_d[0:1, :].partition_broadcast(128)), writes=[bg])
        yT = v3(A.bf16(8 * SEQ), 8, SEQ)
        byT = P.bufs(16)
        wg = v3(A.bf16(8 * 1536), 8, 1536)
        bwg = P.buf()
        qT = v3(A.bf16(4 * SEQ), 4, SEQ)
        kT = v3(A.bf16(4 * SEQ), 4, SEQ)
        bqT, bkT = P.buf(), P.buf()
        Va = v4(A.bf16(16 * 520), 16, 8, 65)
        bVa = P.bufs(16)
        bVa1 = P.buf()
        wout = v3(A.bf16(4 * D), 4, D)
        bwout = P.buf()
        tl = dict(xt=[A.f32(D), A.f32(D)], bxt=P.bufs(2), yb=[A.bf16(D), A.bf16(D)], byb=P.bufs(2),
                  junk=A.f32(D), bjunk=P.buf(), ss=A.f32(2), rs=A.f32(2), bss=P.buf())
        qk = [A.bf16(D), A.bf16(D)]
        bqk = P.bufs(2)
        NE = ATT_CFG[0]
        Eb = [v4(A.bf16(4 * 512), 4, 2, 256) for _ in range(NE)]
        bE = [P.bufs(4) for _ in range(NE)]
        OZ = [A.f32(264) for _ in range(3)]
        bOZ = P.bufs(3)
        posi = A.i32(NT)
        posf = A.f32(NT)
        invf = A.f32(8)
        ang = A.f32(NT * 8)
        a2 = A.f32(NT * 8)
        nf = A.f32(NT * 8)
        ni = A.i32(NT * 8)
        mk = A.f32(NT * 8)
        cosT = v3(A.f32(NT * 8), NT, 8)
        sinT = v3(A.f32(NT * 8), NT, 8)
        brot = P.buf()
        rt = [A.f32(128) for _ in range(4)]
        brt = P.bufs(4)
        ozl = [[A.f32(264) for _ in range(3)] for _ in range(2)]
        bozl = [P.bufs(3) for _ in range(2)]
        zs = A.f32(8)
        us = A.f32(512)
        bmg = P.buf()
        ob = A.bf16(512)
        bob = P.buf()
        oT = v3(A.bf16(4 * 128), 4, 128)
        boT = P.buf()
        res = [tl["junk"], A.f32(D)]
        bres = [tl["bjunk"], P.buf()]

        P.dma("sp", lambda e: e.dma_start(out=posi, in_=posT_d), writes=[brot])
        P.dma("sp", lambda e: e.dma_start(out=invf, in_=c_invf_d), writes=[brot])
        P.op("dve", lambda e: e.tensor_copy(out=posf, in_=posi), reads=[brot], writes=[brot])
        P.op("dve", lambda e: e.tensor_tensor(
            out=v3(ang, NT, 8), in0=posf.unsqueeze(2).to_broadcast([128, NT, 8]),
            in1=invf.unsqueeze(1).to_broadcast([128, NT, 8]), op=ALU.mult), reads=[brot], writes=[brot])
        TWO_PI = 2.0 * math.pi
        C1 = 6.28125
        C2 = TWO_PI - C1
        for (tab, shift) in ((sinT, 0.0), (cosT, 0.5 * math.pi)):
            tabf = tab.rearrange("p a b -> p (a b)")
            P.op("dve", lambda e, shift=shift: e.tensor_scalar(out=a2, in0=ang, scalar1=shift, scalar2=None, op0=ALU.add),
                 reads=[brot], writes=[brot])
            P.op("dve", lambda e: e.tensor_scalar(out=ni, in0=a2, scalar1=1.0 / TWO_PI, scalar2=None, op0=ALU.mult),
                 reads=[brot], writes=[brot])
            P.op("dve", lambda e: e.tensor_copy(out=nf, in_=ni), reads=[brot], writes=[brot])
            P.op("dve", lambda e: e.scalar_tensor_tensor(out=a2, in0=nf, scalar=-C1, in1=a2, op0=ALU.mult, op1=ALU.add),
                 reads=[brot], writes=[brot])
            P.op("dve", lambda e: e.scalar_tensor_tensor(out=a2, in0=nf, scalar=-C2, in1=a2, op0=ALU.mult, op1=ALU.add),
                 reads=[brot], writes=[brot])
            P.op("dve", lambda e: e.tensor_scalar(out=mk, in0=a2, scalar1=math.pi, scalar2=None, op0=ALU.is_gt),
                 reads=[brot], writes=[brot])
            P.op("dve", lambda e: e.scalar_tensor_tensor(out=a2, in0=mk, scalar=-TWO_PI, in1=a2, op0=ALU.mult, op1=ALU.add),
                 reads=[brot], writes=[brot])
            P.op("dve", lambda e: e.tensor_scalar(out=mk, in0=a2, scalar1=-math.pi, scalar2=None, op0=ALU.is_lt),
                 reads=[brot], writes=[brot])
            P.op("dve", lambda e: e.scalar_tensor_tensor(out=a2, in0=mk, scalar=TWO_PI, in1=a2, op0=ALU.mult, op1=ALU.add),
                 reads=[brot], writes=[brot])
            P.op("dve", lambda e: e.tensor_scalar(out=a2, in0=a2, scalar1=math.pi, scalar2=-math.pi, op0=ALU.min, op1=ALU.max),
                 reads=[brot], writes=[brot])
            P.op("act", lambda e, tabf=tabf: e.activation(out=tabf, in_=a2, func=AF.Sin), reads=[brot], writes=[brot])

        if CUT[0] == 1:
            P.barrier()
            return
        P.op("pool", lambda e: e.memset(Va[:, :, :, 64:65], 1.0), writes=[bVa1])
        P.dma("pool", lambda e: e.dma_start(out=wout, in_=awout_d.rearrange("(k p) n -> p k n", p=128)), writes=[bwout])
        for i in range(3):
            P.op("pool", lambda e, i=i: e.memset(OZ[i], 0.0), writes=[bOZ[i]])

        x_tiles = [P.buf() for _ in range(NT)]
        xs_zero_begin()
        ei = 0
        ozi = 0
        for s in range(2):
            seq_norm_transpose(x_d, x_tiles, s, gbt, bg, yT, byT, tl)
            if CUT[0] == 2:
                P.barrier()
                return
            for g in range(3):
                d = DILS[g]
                nb = 16 // d
                P.dma("pool", lambda e, g=g: e.dma_start(
                    out=wg, in_=awin_d[:, g * 1536:(g + 1) * 1536].rearrange("(k p) n -> p k n", p=128)),
                    writes=[bwg])
                if CUT[0] == 31:
                    P.barrier()
                    return
                for t in range(16):
                    gt = s * 16 + t
                    if CUT[0] in (32, 33, 34) and t == 1:
                        P.barrier()
                        return
                    b0 = 0 if t % 2 == 0 else 2
                    for j in range(2):
                        for k in range(8):
                            P.op("pe", lambda e, j=j, k=k, b0=b0, t=t: e.matmul(
                                bank(b0 + j), lhsT=yT[:, k, t * 128:(t + 1) * 128], rhs=wg[:, k, j * 512:(j + 1) * 512],
                                start=(k == 0), stop=(k == 7)), reads=[byT[t], bwg], writes=[pb[b0 + j]])
                    qkt, bq = qk[t % 2], bqk[t % 2]
                    xs_zero_some(1, after=[bq])
                    psq = bank(b0, 2)
                    P.op("act", lambda e, qkt=qkt, psq=psq: e.copy(out=qkt, in_=psq),
                         reads=[pb[b0], pb[b0 + 1]], writes=[bq])
                    if CUT[0] == 32:
                        continue
                    psv = v3(psq, 16, 64)
                    qkv = v3(qkt, 16, 64)
                    cb = cosT[:, gt:gt + 1, :].to_broadcast([128, 16, 8])
                    sb = sinT[:, gt:gt + 1, :].to_broadcast([128, 16, 8])
                    t1 = psv[:, :, 0:8]
                    t2 = psv[:, :, 8:16]
                    r = [v3(x_, 16, 8) for x_ in rt]
                    rd = [pb[b0], pb[b0 + 1], brot]
                    P.op("dve", lambda e, t1=t1, cb=cb, r=r: e.tensor_tensor(out=r[0], in0=t1, in1=cb, op=ALU.mult), reads=rd, writes=[brt[0]])
                    P.op("dve", lambda e, t2=t2, sb=sb, r=r: e.tensor_tensor(out=r[1], in0=t2, in1=sb, op=ALU.mult), reads=rd, writes=[brt[1]])
                    P.op("dve", lambda e, t2=t2, cb=cb, r=r: e.tensor_tensor(out=r[2], in0=t2, in1=cb, op=ALU.mult), reads=rd, writes=[brt[2]])
                    P.op("dve", lambda e, t1=t1, sb=sb, r=r: e.tensor_tensor(out=r[3], in0=t1, in1=sb, op=ALU.mult), reads=rd, writes=[brt[3]])
                    P.op("dve", lambda e, qkv=qkv, r=r: e.tensor_tensor(out=qkv[:, :, 0:8], in0=r[0], in1=r[1], op=ALU.subtract),
                         reads=[brt[0], brt[1]], writes=[bq])
                    P.op("dve", lambda e, qkv=qkv, r=r: e.tensor_tensor(out=qkv[:, :, 8:16], in0=r[2], in1=r[3], op=ALU.add),
                         reads=[brt[2], brt[3]], writes=[bq])
                    if CUT[0] == 33:
                        continue
                    bk = 4 + (t % 2)
                    pbf = bank_bf(bk)
                    for c in range(8):
                        P.op("pe", lambda e, c=c, pbf=pbf, qkt=qkt: e.transpose(
                            out=pbf[:, c * 128:(c + 1) * 128], in_=qkt[:, c * 128:(c + 1) * 128], identity=identb),
                            reads=[bq, bconst], writes=[pb[bk]])
                    P.op("act", lambda e, pbf=pbf, t=t: e.copy(out=qT[:, :, t * 128:(t + 1) * 128], in_=v3(pbf[:, 0:512], 4, 128)),
                         reads=[pb[bk]], writes=[bqT])
                    P.op("dve", lambda e, pbf=pbf, t=t: e.tensor_copy(out=kT[:, :, t * 128:(t + 1) * 128], in_=v3(pbf[:, 512:1024], 4, 128)),
                         reads=[pb[bk]], writes=[bkT])
                if CUT[0] == 3:
                    P.barrier()
                    return
                for blk in range(16):
                    ph, b = blk // nb, blk % nb
                    st = b * 128 * d + ph
                    bk = 6 + (blk % 2)
                    for k in range(8):
                        P.op("pe", lambda e, k=k, st=st, d=d, bk=bk: e.matmul(
                            bank(bk), lhsT=yT[:, k, st:st + 127 * d + 1:d], rhs=wg[:, k, 1024:1536],
                            start=(k == 0), stop=(k == 7)), reads=byT[st // 128:(st + 127 * d) // 128 + 1] + [bwg], writes=[pb[bk]])
                    eng = "act" if blk % 2 == 0 else "dve"
                    if eng == "act":
                        P.op("act", lambda e, blk=blk, bk=bk: e.copy(out=Va[:, blk, :, 0:64], in_=v3(bank(bk), 8, 64)),
                             reads=[pb[bk]], writes=[bVa[blk]])
                    else:
                        P.op("dve", lambda e, blk=blk, bk=bk: e.tensor_copy(out=Va[:, blk, :, 0:64], in_=v3(bank(bk), 8, 64)),
                             reads=[pb[bk]], writes=[bVa[blk]])
                if CUT[0] == 4:
                    P.barrier()
                    return
                sbi = 0
                pvi = 0
                for ph in range(d):
                    prevE = None
                    for b in range(nb):
                        blk = ph * nb + b
                        nq = 256 if b + 1 < nb else 128
                        st = b * 128 * d + ph
                        Ec, bEc = Eb[ei % NE], bE[ei % NE]
                        ei += 1
                        for c in range(4):
                            sbk = 2 * (sbi % ATT_CFG[1])
                            sbi += 1
                            for hh in range(2):
                                P.op("pe", lambda e, c=c, hh=hh, st=st, d=d, nq=nq, sbk=sbk: e.matmul(
                                    bank(sbk + hh)[:, 0:nq],
                                    lhsT=kT[hh * 64:(hh + 1) * 64, c, st:st + 127 * d + 1:d],
                                    rhs=qT[hh * 64:(hh + 1) * 64, c, st:st + (nq - 1) * d + 1:d],
                                    start=True, stop=True), reads=[bqT, bkT], writes=[pb[sbk + hh]])
                            P.op("act", lambda e, Ec=Ec, c=c, nq=nq, sbk=sbk: e.activation(
                                out=Ec[:, c, :, 0:nq], in_=v3(bank(sbk, 2), 2, 512)[:, :, 0:nq], func=AF.Exp, scale=0.125),
                                reads=[pb[sbk], pb[sbk + 1]], writes=[bEc[c]])
                            P.op("dve", lambda e, Ec=Ec, c=c, nq=nq: e.tensor_tensor(
                                out=Ec[:, c, :, 0:nq], in0=Ec[:, c, :, 0:nq], in1=v3(maskb, 2, 256)[:, :, 0:nq], op=ALU.mult),
                                reads=[bEc[c], bconst], writes=[bEc[c]])
                        pvb = (4 + 2 * (pvi % 2)) if ATT_CFG[1] == 2 else 6
                        pvi += 1
                        for h in range(8):
                            c, hh = h // 2, h % 2
                            o_ap = bank(pvb + h // 4)[:, (h % 4) * 65:(h % 4) * 65 + 65]
                            if b > 0:
                                Ep, bEp = prevE
                                P.op("pe", lambda e, o_ap=o_ap, Ep=Ep, c=c, hh=hh, blk=blk, h=h: e.matmul(
                                    o_ap, lhsT=Ep[:, c, hh, 128:256], rhs=Va[:, blk - 1, h, :], start=True, stop=False),
                                    reads=[bEp[c], bVa[blk - 1], bVa1], writes=[pb[pvb + h // 4]])
                            P.op("pe", lambda e, o_ap=o_ap, Ec=Ec, c=c, hh=hh, blk=blk, h=h, b=b: e.matmul(
                                o_ap, lhsT=Ec[:, c, hh, 0:128], rhs=Va[:, blk, h, :], start=(b == 0), stop=True),
                                reads=[bEc[c], bVa[blk], bVa1], writes=[pb[pvb + h // 4]])
                        prevE = (Ec, bEc)
                        oz, boz = OZ[ozi % 3], bOZ[ozi % 3]
                        ozi += 1
                        pv = v3(bank(pvb, 2), 2, 512)[:, :, 0:260].rearrange("p a (h e) -> p a h e", h=4, e=65)
                        P.op("act", lambda e, oz=oz, pv=pv: e.copy(
                            out=v4(oz[:, 0:256].bitcast(BF16), 2, 4, 64), in_=pv[:, :, :, 0:64]),
                            reads=[pb[pvb], pb[pvb + 1]], writes=[boz])
                        P.op("dve", lambda e, oz=oz, pv=pv: e.tensor_copy(
                            out=v4(oz[:, 256:264], 2, 4, 1), in_=pv[:, :, :, 64:65]),
                            reads=[pb[pvb], pb[pvb + 1]], writes=[boz])
                        r0 = s * SEQ + st
                        touched = sorted({(r0 + d * i) // 128 for i in (0, 127)})
                        tb = [bOGZ[g][ti] for ti in range(touched[0], touched[-1] + 1)]
                        P.dma("sp", lambda e, oz=oz, g=g, r0=r0, d=d: e.dma_start(
                            out=OGZ[g][r0:r0 + 127 * d + 1:d, :], in_=oz), reads=[boz], writes=tb)
                if CUT[0] == 5:
                    P.barrier()
                    return
            if CUT[0] == 6:
                P.barrier()
                return
            for t in range(16):
                gt = s * 16 + t
                ol, bol = ozl[t % 2], bozl[t % 2]
                for g in range(3):
                    P.dma("sp", lambda e, g=g, ol=ol, gt=gt: e.dma_start(out=ol[g], in_=OGZ[g][gt * 128:(gt + 1) * 128, :]),
                          reads=[bOGZ[g][gt]], writes=[bol[g]])
                zv = [ol[g][:, 256:264] for g in range(3)]
                uv = [ol[g][:, 0:256].bitcast(BF16) for g in range(3)]
                P.op("dve", lambda e, zv=zv: e.tensor_tensor(out=zs, in0=zv[0], in1=zv[1], op=ALU.add), reads=[bol[0], bol[1]], writes=[bmg])
                P.op("dve", lambda e, zv=zv: e.tensor_tensor(out=zs, in0=zs, in1=zv[2], op=ALU.add), reads=[bol[2], bmg], writes=[bmg])
                P.op("dve", lambda e: e.reciprocal(out=zs, in_=zs), reads=[bmg], writes=[bmg])
                P.op("dve", lambda e, uv=uv: e.tensor_tensor(out=us, in0=uv[0], in1=uv[1], op=ALU.add), reads=[bol[0], bol[1], bmg], writes=[bmg])
                P.op("dve", lambda e, uv=uv: e.tensor_tensor(out=us, in0=us, in1=uv[2], op=ALU.add), reads=[bol[2], bmg], writes=[bmg])
                P.op("dve", lambda e: e.tensor_tensor(out=v3(ob, 8, 64), in0=v3(us, 8, 64),
                                                      in1=zs.unsqueeze(2).to_broadcast([128, 8, 64]), op=ALU.mult),
                     reads=[bmg], writes=[bob])
                bk = 4 + (t % 2)
                pbf = bank_bf(bk)
                for k in range(4):
                    P.op("pe", lambda e, k=k, pbf=pbf: e.transpose(out=pbf[:, k * 128:(k + 1) * 128], in_=ob[:, k * 128:(k + 1) * 128], identity=identb),
                         reads=[bob, bconst], writes=[pb[bk]])
                P.op("act", lambda e, pbf=pbf: e.copy(out=oT, in_=v3(pbf[:, 0:512], 4, 128)), reads=[pb[bk]], writes=[boT])
                b0 = 0 if t % 2 == 0 else 2
                for hf in range(2):
                    for k in range(4):
                        P.op("pe", lambda e, hf=hf, k=k, b0=b0: e.matmul(
                            bank(b0 + hf), lhsT=oT[:, k, :], rhs=wout[:, k, hf * 512:(hf + 1) * 512],
                            start=(k == 0), stop=(k == 3)), reads=[boT, bwout], writes=[pb[b0 + hf]])
                xt, bx = tl["xt"][t % 2], tl["bxt"][t % 2]
                P.dma("sp", lambda e, xt=xt, gt=gt: e.dma_start(out=xt, in_=x_d[gt * 128:(gt + 1) * 128, :]), writes=[bx])
                rs_, brs_ = res[t % 2], bres[t % 2]
                P.op("dve", lambda e, rs_=rs_, xt=xt, b0=b0: e.tensor_tensor(out=rs_, in0=xt, in1=bank(b0, 2), op=ALU.add),
                     reads=[bx, pb[b0], pb[b0 + 1]], writes=[brs_])
                P.dma("sp", lambda e, rs_=rs_, gt=gt: e.dma_start(out=H1[gt * 128:(gt + 1) * 128, :], in_=rs_),
                      reads=[brs_], writes=[bH[id(H1)][gt]])
        P.barrier()

    def moe_phase(li, Hin, Hout, final):
        A.release(persist_mark)
        bHin = bH[id(Hin)]
        gbt = A.f32(D)
        bg = P.buf()
        P.dma("sp", lambda e: e.dma_start(out=gbt, in_=nffn_d[li:li + 1, :].partition_broadcast(128)), writes=[bg])
        idx = A.i32(2 * NT)
        gat = A.f32(2 * NT)
        broute = P.buf()
        m_phase = A.mark()
        A.top_reset()
        w1b = [v3(A.top_bf16(8 * 512), 8, 512), None]
        w3b = [v3(A.top_bf16(8 * 512), 8, 512), None]
        w2b = [v3(A.top_bf16(4 * D), 4, D), None]
        bw = [P.bufs(3) for _ in range(2)]
        pre_w = {}

        def prefetch_expert0(after):
            if pre_w:
                return
            pre_w["done"] = True
            for (wt_, src_, i_) in ((w1b[0], w1_d, 0), (w3b[0], w3_d, 1), (w2b[0], w2_d, 2)):
                P.dma("pool", lambda e, wt_=wt_, src_=src_: e.dma_start(out=wt_, in_=src_[li, 0].rearrange("(k p) n -> p k n", p=128)),
                      reads=list(after), writes=[bw[0][i_]], c=22.0)

        xs_zero_some(1000)
        ybA = v3(A.f32(NT * 516), NT, 516)

        def yb_t(t):
            return ybA[:, t, 0:512].bitcast(BF16)
        bybA = P.bufs(NT)
        wr = v3(A.f32(8 * 20), 8, 20)
        bwr = P.buf()
        rb = A.f32(20)
        triu = A.f32(128)
        onesm = A.f32(128)
        eoff = A.f32(16)
        P.dma("sp", lambda e: e.dma_start(out=wr[:, :, 0:4], in_=rgw_d[li].rearrange("(k p) n -> p k n", p=128)), writes=[bwr])
        for g in range(4):
            P.dma("sp", lambda e, g=g: e.dma_start(out=wr[:, :, 4 + 4 * g:8 + 4 * g],
                                                    in_=rew_d[li, g].rearrange("(k p) n -> p k n", p=128)), writes=[bwr])
        P.dma("sp", lambda e: e.dma_start(out=rb[:, 0:4], in_=rgb_d[li:li + 1, :].partition_broadcast(128)), writes=[bwr])
        P.dma("sp", lambda e: e.dma_start(out=rb[:, 4:20], in_=reb_d[li:li + 1, :].partition_broadcast(128)), writes=[bwr])
        P.dma("sp", lambda e: e.dma_start(out=triu, in_=c_triu_d), writes=[bwr])
        P.dma("sp", lambda e: e.dma_start(out=onesm, in_=c_ones_d), writes=[bwr])
        P.dma("sp", lambda e: e.dma_start(out=eoff, in_=c_eoff_d), writes=[bwr])
        NB3 = 3
        xt2 = [A.f32(D) for _ in range(NB3)]
        bxt2 = P.bufs(NB3)
        yf = [A.f32(D) for _ in range(NB3)]
        byf = P.bufs(NB3)
        junk = A.f32(D)
        bjunk = P.buf()
        ss, rs = A.f32(2), A.f32(2)
        bss = P.buf()
        whl = v3(A.bf16(8 * 40), 8, 40)
        wtmp = v3(A.f32(8 * 20), 8, 20)
        bwhl = P.buf()
        P.op("dve", lambda e: e.tensor_copy(out=whl[:, :, 0:20], in_=wr), reads=[bwr], writes=[bwhl])
        P.op("dve", lambda e: e.tensor_tensor(out=wtmp, in0=wr, in1=whl[:, :, 0:20], op=ALU.subtract), reads=[bwr, bwhl], writes=[bwhl])
        P.op("dve", lambda e: e.tensor_copy(out=whl[:, :, 20:40], in_=wtmp), reads=[bwhl], writes=[bwhl])
        yl = [A.bf16(D) for _ in range(NB3)]
        byl = P.bufs(NB3)
        yhT = [v3(A.bf16(8 * 128), 8, 128) for _ in range(NB3)]
        ylT = [v3(A.bf16(8 * 128), 8, 128) for _ in range(NB3)]
        byhT, bylT = P.bufs(NB3), P.bufs(NB3)
        L = v3(A.f32(NT * 20), NT, 20)
        NG = 4
        GN = NT // NG
        bLg = P.bufs(NG)

        def tile_step(t):
            xt, bx = xt2[t % NB3], bxt2[t % NB3]
            P.dma("sp", lambda e, xt=xt, t=t: e.dma_start(out=xt, in_=Hin[t * 128:(t + 1) * 128, :]),
                  reads=[bHin[t]], writes=[bx])
            yft, byft = yf[t % NB3], byf[t % NB3]
            rmsnorm_tile(xt, bx, gbt, bg, [(yft, byft)], junk, bjunk, ss, rs, bss)
            P.op("act", lambda e, yft=yft, t=t: e.copy(out=yb_t(t), in_=yft), reads=[byft], writes=[bybA[t]], c=1.1)
            if t == 6:
                prefetch_expert0([bybA[t]])
            ylt, bylt = yl[t % NB3], byl[t % NB3]
            P.op("dve", lambda e, yft=yft, ylt=ylt, t=t: e.tensor_tensor(out=ylt, in0=yft, in1=yb_t(t), op=ALU.subtract),
                 reads=[byft, bybA[t]], writes=[bylt], c=1.1)
            b0 = 0 if t % 2 == 0 else 2
            ph_, pl_ = bank_bf(b0), bank_bf(b0 + 1)
            for k in range(8):
                P.op("pe", lambda e, k=k, t=t, ph_=ph_: e.transpose(
                    out=ph_[:, k * 128:(k + 1) * 128], in_=yb_t(t)[:, k * 128:(k + 1) * 128], identity=identb),
                    reads=[bybA[t], bconst], writes=[pb[b0]], c=0.08)
            for k in range(8):
                P.op("pe", lambda e, k=k, ylt=ylt, pl_=pl_: e.transpose(
                    out=pl_[:, k * 128:(k + 1) * 128], in_=ylt[:, k * 128:(k + 1) * 128], identity=identb),
                    reads=[bylt, bconst], writes=[pb[b0 + 1]], c=0.08)
            yh_, byh_ = yhT[t % NB3], byhT[t % NB3]
            yl_, byl_ = ylT[t % NB3], bylT[t % NB3]
            P.op("act", lambda e, yh_=yh_, ph_=ph_: e.copy(out=yh_, in_=v3(ph_, 8, 128)), reads=[pb[b0]], writes=[byh_], c=1.0)
            P.op("dve", lambda e, yl_=yl_, pl_=pl_: e.tensor_copy(out=yl_, in_=v3(pl_, 8, 128)), reads=[pb[b0 + 1]], writes=[byl_], c=1.0)
            lb = 4 + (t // 8) % 2
            c0 = (t % 8) * 60
            for k in range(8):
                P.op("pe", lambda e, k=k, yh_=yh_, lb=lb, c0=c0: e.matmul(
                    bank(lb)[:, c0:c0 + 40], lhsT=yh_[:, k, :], rhs=whl[:, k, :],
                    start=(k == 0), stop=(k == 7)), reads=[byh_, bwhl], writes=[pb[lb]], c=0.08)
            for k in range(8):
                P.op("pe", lambda e, k=k, yl_=yl_, lb=lb, c0=c0: e.matmul(
                    bank(lb)[:, c0 + 40:c0 + 60], lhsT=yl_[:, k, :], rhs=whl[:, k, 0:20],
                    start=(k == 0), stop=(k == 7)), reads=[byl_, bwhl], writes=[pb[lb]], c=0.08)
            if t % 8 == 7:
                hb = t // 8
                pv_ = v3(bank(lb)[:, 0:480], 8, 60)
                Lh = L[:, hb * 8:(hb + 1) * 8, :]
                bL_ = bLg[t // GN]
                P.op("dve", lambda e, pv_=pv_, Lh=Lh: e.tensor_tensor(
                    out=Lh, in0=pv_[:, :, 0:20], in1=rb.unsqueeze(1).to_broadcast([128, 8, 20]), op=ALU.add),
                    reads=[pb[lb], bwr], writes=[bL_])
                P.op("dve", lambda e, pv_=pv_, Lh=Lh: e.tensor_tensor(out=Lh, in0=Lh, in1=pv_[:, :, 20:40], op=ALU.add),
                     reads=[pb[lb], bL_], writes=[bL_])
                P.op("dve", lambda e, pv_=pv_, Lh=Lh: e.tensor_tensor(out=Lh, in0=Lh, in1=pv_[:, :, 40:60], op=ALU.add),
                     reads=[pb[lb], bL_], writes=[bL_])

        def T(n):
            return A.f32(NT * n)
        mg = T(1)
        G = v3(T(4), NT, 4)
        ex4 = v3(T(4), NT, 4)
        se = T(1)
        g1 = T(1)
        tmp16 = v3(T(16), NT, 16)
        sel = v3(T(4), NT, 4)
        m1, m2 = T(1), T(1)
        o1 = v3(T(4), NT, 4)
        o2 = v3(T(4), NT, 4)
        sel2 = v3(T(4), NT, 4)
        dl, exd, w1g, w2g = T(1), T(1), T(1), T(1)
        E1 = v3(T(16), NT, 16)
        E2 = v3(T(16), NT, 16)
        S16 = v3(T(16), NT, 16)
        Bc = [v3(T(16), NT, 16), v3(T(16), NT, 16)]
        rank = v3(T(16), NT, 16)
        valid = v3(T(16), NT, 16)
        pos16 = v3(T(16), NT, 16)
        idf = v3(T(2), 2, NT)
        vld = v3(T(2), 2, NT)
        tokp1 = A.f32(NT)
        btk = P.buf()
        P.dma("sp", lambda e: e.dma_start(out=tokp1, in_=c_tokp1_d), writes=[btk])
        BIG = float(NEXP * CAP + 64)
        idx3 = v3(idx, 2, NT)
        gat3 = v3(gat, 2, NT)
        brg = P.bufs(NG)
        broute_g = P.bufs(NG)
        bexg = P.bufs(NG)
        incs = []

        def route(gi):
            t0, t1 = gi * GN, (gi + 1) * GN
            sl = slice(t0, t1)
            br = brg[gi]
            bL_ = bLg[gi]
            deps_prev = [brg[gi - 1]] if gi > 0 else []

            def dv(fn, reads=(), writes=()):
                P.op("dve", fn, reads=[br, bL_, bwr] + list(reads), writes=[br] + list(writes), c=0.3)

            def bc1(ap1):
                return ap1.unsqueeze(2).to_broadcast([128, GN, 4])

            def g4(ap3):
                return ap3.rearrange("p a (g e) -> p a g e", g=4, e=4)
            lgv = L[:, sl, 0:4]
            lev = L[:, sl, 4:20]
            mg_, se_, g1_, m1_, m2_, dl_, exd_, w1_, w2_ = [x_[:, sl] for x_ in (mg, se, g1, m1, m2, dl, exd, w1g, w2g)]
            G_, ex4_, sel_, o1_, o2_, sel2_ = [x_[:, sl, :] for x_ in (G, ex4, sel, o1, o2, sel2)]
            tmp_, E1_, E2_, S_, rank_, valid_, pos_ = [x_[:, sl, :] for x_ in (tmp16, E1, E2, S16, rank, valid, pos16)]
            B0, B1 = Bc[0][:, sl, :], Bc[1][:, sl, :]
            dv(lambda e: e.tensor_reduce(out=mg_, in_=lgv, axis=AX.X, op=ALU.max))
            dv(lambda e: e.tensor_tensor(out=G_, in0=lgv, in1=bc1(mg_), op=ALU.is_equal))
            dv(lambda e: e.tensor_tensor(out=ex4_, in0=lgv, in1=bc1(mg_), op=ALU.subtract))
            P.op("act", lambda e: e.activation(out=ex4_, in_=ex4_, func=AF.Exp), reads=[br], writes=[br], c=0.3)
            dv(lambda e: e.tensor_reduce(out=se_, in_=ex4_, axis=AX.X, op=ALU.add))
            dv(lambda e: e.reciprocal(out=g1_, in_=se_))
            dv(lambda e: e.tensor_tensor(out=g4(tmp_), in0=g4(lev), in1=G_.unsqueeze(3).to_broadcast([128, GN, 4, 4]), op=ALU.mult))
            dv(lambda e: e.tensor_reduce(out=sel_, in_=tmp_.rearrange("p a (g e) -> p a e g", g=4, e=4), axis=AX.X, op=ALU.add))
            dv(lambda e: e.tensor_reduce(out=m1_, in_=sel_, axis=AX.X, op=ALU.max))
            dv(lambda e: e.tensor_tensor(out=o1_, in0=sel_, in1=bc1(m1_), op=ALU.is_equal))
            dv(lambda e: e.tensor_scalar(out=sel2_, in0=o1_, scalar1=-1e30, scalar2=None, op0=ALU.mult))
            dv(lambda e: e.tensor_tensor(out=sel2_, in0=sel2_, in1=sel_, op=ALU.add))
            dv(lambda e: e.tensor_reduce(out=m2_, in_=sel2_, axis=AX.X, op=ALU.max))
            dv(lambda e: e.tensor_tensor(out=o2_, in0=sel2_, in1=bc1(m2_), op=ALU.is_equal))
            dv(lambda e: e.tensor_tensor(out=dl_, in0=m2_, in1=m1_, op=ALU.subtract))
            P.op("act", lambda e: e.activation(out=exd_, in_=dl_, func=AF.Exp), reads=[br], writes=[br], c=0.3)
            dv(lambda e: e.tensor_scalar(out=w1_, in0=exd_, scalar1=1.0, scalar2=None, op0=ALU.add))
            dv(lambda e: e.reciprocal(out=w1_, in_=w1_))
            dv(lambda e: e.tensor_tensor(out=w2_, in0=exd_, in1=w1_, op=ALU.mult))
            dv(lambda e: e.tensor_tensor(out=w1_, in0=w1_, in1=g1_, op=ALU.mult))
            dv(lambda e: e.tensor_tensor(out=w2_, in0=w2_, in1=g1_, op=ALU.mult))
            for (Ek, ok) in ((E1_, o1_), (E2_, o2_)):
                dv(lambda e, Ek=Ek, ok=ok: e.tensor_tensor(
                    out=g4(Ek), in0=G_.unsqueeze(3).to_broadcast([128, GN, 4, 4]),
                    in1=ok.unsqueeze(2).to_broadcast([128, GN, 4, 4]), op=ALU.mult))
            dv(lambda e: e.tensor_tensor(out=S_, in0=E1_, in1=E2_, op=ALU.add))
            Sf = S16.rearrange("p a b -> p (a b)")[:, t0 * 16:t1 * 16]
            cs = slice(gi * GN * 16, (gi + 1) * GN * 16)
            P.op("pe", lambda e: e.matmul(bank(6)[:, cs], lhsT=triu, rhs=Sf, start=True, stop=True), reads=[br, bwr], writes=[pb[6]])
            P.op("pe", lambda e: e.matmul(bank(7)[:, cs], lhsT=onesm, rhs=Sf, start=True, stop=True), reads=[br, bwr], writes=[pb[7]])
            psA = v3(bank(6)[:, cs], GN, 16)
            psB = v3(bank(7)[:, cs], GN, 16)
            P.op("dve", lambda e: e.tensor_copy(out=B0, in_=psB), reads=[pb[7], br], writes=[br])
            cur = [B0, B1]
            sft = 1
            while sft < GN:
                a_, b_ = cur
                dv(lambda e, a_=a_, b_=b_, sft=sft: e.tensor_copy(out=b_[:, 0:sft, :], in_=a_[:, 0:sft, :]))
                dv(lambda e, a_=a_, b_=b_, sft=sft: e.tensor_tensor(out=b_[:, sft:GN, :], in0=a_[:, sft:GN, :], in1=a_[:, 0:GN - sft, :], op=ALU.add))
                cur = [b_, a_]
                sft *= 2
            inc = cur[0]
            if gi > 0:
                pin = incs[gi - 1]
                dv(lambda e, inc=inc, pin=pin: e.tensor_tensor(out=inc, in0=inc, in1=pin[:, GN - 1:GN, :].to_broadcast([128, GN, 16]), op=ALU.add),
                   reads=deps_prev)
            incs.append(inc)
            P.op("dve", lambda e, inc=inc: e.tensor_tensor(out=rank_, in0=inc, in1=psB, op=ALU.subtract), reads=[pb[7], br], writes=[br])
            P.op("dve", lambda e: e.tensor_tensor(out=rank_, in0=rank_, in1=psA, op=ALU.add), reads=[pb[6], br], writes=[br])
            dv(lambda e: e.tensor_scalar(out=valid_, in0=rank_, scalar1=float(CAP), scalar2=None, op0=ALU.is_lt))
            dv(lambda e: e.tensor_tensor(out=pos_, in0=rank_, in1=eoff.unsqueeze(1).to_broadcast([128, GN, 16]), op=ALU.add))
            dv(lambda e: e.tensor_scalar(out=pos_, in0=pos_, scalar1=-BIG, scalar2=None, op0=ALU.add))
            dv(lambda e: e.tensor_tensor(out=pos_, in0=pos_, in1=valid_, op=ALU.mult))
            dv(lambda e: e.tensor_scalar(out=pos_, in0=pos_, scalar1=BIG, scalar2=None, op0=ALU.add))
            brt = broute_g[gi]
            for k, (Ek, wk) in enumerate(((E1_, w1_), (E2_, w2_))):
                dv(lambda e, Ek=Ek: e.tensor_tensor(out=tmp_, in0=Ek, in1=pos_, op=ALU.mult))
                dv(lambda e, k=k: e.tensor_reduce(out=idf[:, k, sl], in_=tmp_, axis=AX.X, op=ALU.add))
                dv(lambda e, Ek=Ek: e.tensor_tensor(out=tmp_, in0=Ek, in1=valid_, op=ALU.mult))
                dv(lambda e, k=k: e.tensor_reduce(out=vld[:, k, sl], in_=tmp_, axis=AX.X, op=ALU.add))
                P.op("dve", lambda e, k=k, wk=wk: e.tensor_tensor(out=gat3[:, k, sl], in0=wk, in1=vld[:, k, sl], op=ALU.mult),
                     reads=[br], writes=[brt], c=0.3)
            P.op("dve", lambda e: e.tensor_copy(out=idx3[:, :, sl], in_=idf[:, :, sl]), reads=[br], writes=[brt], c=0.3)
            bex = bexg[gi]
            P.op("dve", lambda e: e.tensor_copy(out=ybA[:, sl, 512:513], in_=tokp1[:, sl].unsqueeze(2)), reads=[btk], writes=[bex], c=0.3)
            P.op("dve", lambda e: e.tensor_copy(out=ybA[:, sl, 513:514], in_=gat3[:, 0, sl].unsqueeze(2)), reads=[bex, brt], writes=[bex], c=0.3)
            P.op("dve", lambda e: e.tensor_copy(out=ybA[:, sl, 514:515], in_=gat3[:, 1, sl].unsqueeze(2)), reads=[bex, brt], writes=[bex], c=0.3)
            P.op("dve", lambda e: e.tensor_copy(out=ybA[:, sl, 515:516], in_=idf[:, 1, sl].unsqueeze(2)), reads=[bex, br], writes=[bex], c=0.3)
            for t in range(t0, t1):
                for k in range(2):
                    P.dma("pool", lambda e, t=t, k=k: e.indirect_dma_start(
                        out=XS, out_offset=bass.IndirectOffsetOnAxis(ap=idx[:, k * NT + t:k * NT + t + 1], axis=0),
                        in_=ybA[:, t, :], in_offset=None, bounds_check=bc_reg(e), oob_is_err=False),
                        reads=[bybA[t], brt, bXSz, bex], writes=[bXS], c=5.0)

        for gi in range(NG):
            for t in range(gi * GN, (gi + 1) * GN):
                tile_step(t)
            route(gi)
        P.barrier()
        if CUT[0] == 101:
            return

        A.release(m_phase)
        w1b[1] = v3(A.bf16(8 * 512), 8, 512)
        w3b[1] = v3(A.bf16(8 * 512), 8, 512)
        w2b[1] = v3(A.bf16(4 * D), 4, D)
        NXS = 8
        xs = [A.f32(516) for _ in range(NXS)]
        bxs = P.bufs(NXS)
        xsT = [v3(A.bf16(8 * 128), 8, 128) for _ in range(2)]
        bxsT = P.bufs(2)
        s1 = [A.f32(512) for _ in range(2)]
        bs1 = P.bufs(2)
        hm = [A.bf16(512) for _ in range(2)]
        bhm = P.bufs(2)
        hmT = [v3(A.bf16(4 * 128), 4, 128) for _ in range(2)]
        bhmT = P.bufs(2)
        ys = [A.bf16(D) for _ in range(6)]
        bys = P.bufs(6)
        cslot = A.f32(NEXP * CAP_T)
        bcs = P.buf()
        P.dma("sp", lambda e: e.dma_start(out=cslot, in_=c_slot_d), writes=[bcs])
        BIG2 = 20000.0
        NST = NEXP * CAP_T
        ev = v3(A.f32(NST * 4), NST, 4)
        bev = P.buf()
        for q4 in range(4):
            n0, n1 = q4 * NST // 4, (q4 + 1) * NST // 4
            P.dma("sp", lambda e, n0=n0, n1=n1: e.dma_start(
                out=ev[:, n0:n1, :], in_=XS[n0 * 128:n1 * 128, 512:516].rearrange("(j p) c -> p j c", p=128)),
                reads=[bXS], writes=[bev], c=20.0)
        kf, dg, gate, dest, inv = [A.f32(NST) for _ in range(5)]
        di = A.i32(NST)
        brt_ = P.buf()

        def dv2(fn):
            P.op("dve", fn, reads=[bev, brt_, bcs], writes=[brt_], c=0.3)
        dv2(lambda e: e.tensor_tensor(out=kf, in0=ev[:, :, 3], in1=cslot, op=ALU.is_equal))
        dv2(lambda e: e.tensor_tensor(out=dg, in0=ev[:, :, 2], in1=ev[:, :, 1], op=ALU.subtract))
        dv2(lambda e: e.tensor_tensor(out=dg, in0=dg, in1=kf, op=ALU.mult))
        dv2(lambda e: e.tensor_tensor(out=gate, in0=dg, in1=ev[:, :, 1], op=ALU.add))
        dv2(lambda e: e.scalar_tensor_tensor(out=dest, in0=kf, scalar=float(TPC), in1=ev[:, :, 0], op0=ALU.mult, op1=ALU.add))
        dv2(lambda e: e.tensor_scalar(out=inv, in0=ev[:, :, 0], scalar1=0.0, scalar2=None, op0=ALU.is_equal))
        dv2(lambda e: e.scalar_tensor_tensor(out=dest, in0=inv, scalar=BIG2, in1=dest, op0=ALU.mult, op1=ALU.add))
        dv2(lambda e: e.tensor_scalar(out=dest, in0=dest, scalar1=-1.0, scalar2=None, op0=ALU.add))
        dv2(lambda e: e.tensor_copy(out=di, in_=dest))
        BK = M2_BANKS[M2_LAYOUT[0]]
        yb0 = BK['y']
        it = 0
        for ex in range(NEXP):
            wb_ = ex % 2
            if ex > 0:
                P.dma("pool", lambda e, ex=ex, w_=w1b[wb_]: e.dma_start(out=w_, in_=w1_d[li, ex].rearrange("(k p) n -> p k n", p=128)), writes=[bw[wb_][0]], c=22.0)
                P.dma("pool", lambda e, ex=ex, w_=w3b[wb_]: e.dma_start(out=w_, in_=w3_d[li, ex].rearrange("(k p) n -> p k n", p=128)), writes=[bw[wb_][1]], c=22.0)
                P.dma("pool", lambda e, ex=ex, w_=w2b[wb_]: e.dma_start(out=w_, in_=w2_d[li, ex].rearrange("(k p) n -> p k n", p=128)), writes=[bw[wb_][2]], c=22.0)
            for j in range(CAP_T):
                r0 = ex * CAP + j * 128
                x_, bx_ = xs[it % NXS], bxs[it % NXS]
                P.dma("sp", lambda e, x_=x_, r0=r0: e.dma_start(out=x_, in_=XS[r0:r0 + 128, :]), reads=[bXS], writes=[bx_])
                xb_ = x_[:, 0:512].bitcast(BF16)
                tbx = BK['xt'][it % len(BK['xt'])]
                pbf = bank_bf(tbx)
                for k in range(8):
                    P.op("pe", lambda e, k=k, pbf=pbf, xb_=xb_: e.transpose(out=pbf[:, k * 128:(k + 1) * 128], in_=xb_[:, k * 128:(k + 1) * 128], identity=identb),
                         reads=[bx_, bconst], writes=[pb[tbx]], c=0.08)
                xT_, bxT_ = xsT[it % 2], bxsT[it % 2]
                P.op("act", lambda e, xT_=xT_, pbf=pbf: e.copy(out=xT_, in_=v3(pbf, 8, 128)), reads=[pb[tbx]], writes=[bxT_], c=1.0)
                hb = BK['h'][it % len(BK['h'])]
                for (wi, wt, bk) in ((0, w1b[wb_], hb), (1, w3b[wb_], hb + 1)):
                    for k in range(8):
                        P.op("pe", lambda e, k=k, wt=wt, bk=bk, xT_=xT_: e.matmul(bank(bk), lhsT=xT_[:, k, :], rhs=wt[:, k, :], start=(k == 0), stop=(k == 7)),
                             reads=[bxT_, bw[wb_][wi]], writes=[pb[bk]])
                s1_, bs1_ = s1[it % 2], bs1[it % 2]
                P.op("act", lambda e, s1_=s1_, hb=hb: e.activation(out=s1_, in_=bank(hb), func=AF.Silu), reads=[pb[hb]], writes=[bs1_])
                hm_, bhm_ = hm[it % 2], bhm[it % 2]
                P.op("dve", lambda e, hm_=hm_, s1_=s1_, hb=hb: e.tensor_tensor(out=hm_, in0=s1_, in1=bank(hb + 1), op=ALU.mult), reads=[bs1_, pb[hb + 1]], writes=[bhm_])
                tbh = BK['ht'][it % len(BK['ht'])]
                pbf2 = bank_bf(tbh)
                for k in range(4):
                    P.op("pe", lambda e, k=k, pbf2=pbf2, hm_=hm_: e.transpose(out=pbf2[:, k * 128:(k + 1) * 128], in_=hm_[:, k * 128:(k + 1) * 128], identity=identb),
                         reads=[bhm_, bconst], writes=[pb[tbh]], c=0.08)
                hT_, bhT_ = hmT[it % 2], bhmT[it % 2]
                P.op("dve", lambda e, hT_=hT_, pbf2=pbf2: e.tensor_copy(out=hT_, in_=v3(pbf2[:, 0:512], 4, 128)), reads=[pb[tbh]], writes=[bhT_])
                for hf in range(2):
                    for k in range(4):
                        P.op("pe", lambda e, k=k, hf=hf, hT_=hT_, w2_=w2b[wb_]: e.matmul(bank(yb0 + hf), lhsT=hT_[:, k, :], rhs=w2_[:, k, hf * 512:(hf + 1) * 512],
                                                                      start=(k == 0), stop=(k == 3)),
                             reads=[bhT_, bw[wb_][2]], writes=[pb[yb0 + hf]])
                ys_, bys_ = ys[it % 6], bys[it % 6]
                P.op("act", lambda e, ys_=ys_, it=it: e.activation(out=ys_, in_=bank(yb0, 2), func=AF.Copy, scale=gate[:, it:it + 1]),
                     reads=[pb[yb0], pb[yb0 + 1], brt_], writes=[bys_], c=1.1)
                P.dma("pool", lambda e, ys_=ys_, it=it: e.indirect_dma_start(
                    out=YK, out_offset=bass.IndirectOffsetOnAxis(ap=di[:, it:it + 1], axis=0),
                    in_=ys_, in_offset=None, bounds_check=bc_reg(e, 2 * TPC - 1), oob_is_err=False),
                    reads=[bys_, brt_], writes=[bYK], c=5.0)
                it += 1
        P.barrier()
        if CUT[0] == 102:
            return

        A.release(m_phase)
        A.top_reset()
        if not final:
            pw = dict(win=v3(A.top_bf16(8 * D), 8, D), wou=v3(A.top_bf16(8 * D), 8, D), wgp=v4(A.top_bf16(4 * 2 * 256), 4, 2, 256),
                      bwin=P.buf(), bwou=P.buf(), bwgp=P.buf())
            PF["pool_w"] = pw
            P.dma("pool", lambda e: e.dma_start(out=pw["win"], in_=pwin_d.rearrange("(k p) n -> p k n", p=128)), writes=[pw["bwin"]], c=15.0)
            P.dma("pool", lambda e: e.dma_start(out=pw["wou"], in_=pwout_d.rearrange("(k p) n -> p k n", p=128)), writes=[pw["bwou"]], c=15.0)
            for g in range(4):
                P.dma("pool", lambda e, g=g: e.dma_start(out=pw["wgp"][:, g, :, :], in_=pwg_d[g].rearrange("(k p) n -> p k n", p=128)), writes=[pw["bwgp"]])
        y1 = [A.bf16(D) for _ in range(2)]
        y2 = [A.bf16(D) for _ in range(2)]
        by1, by2 = P.bufs(2), P.bufs(2)
        ht = [A.f32(D) for _ in range(2)]
        bht = P.bufs(2)
        acc = [A.f32(D) for _ in range(2)]
        bacc = P.bufs(2)
        hn = [A.f32(D) for _ in range(2)]
        bhn = P.bufs(2)
        gft = None
        if final:
            gft = A.f32(D)
            bgf = P.buf()
            P.dma("sp", lambda e: e.dma_start(out=gft, in_=nfin_d[0:1, :].partition_broadcast(128)), writes=[bgf])
            junk = A.f32(D)
            bjunk = P.buf()
            ss, rs = A.f32(2), A.f32(2)
            bss = P.buf()
            fo = [A.f32(D) for _ in range(2)]
            bfo = P.bufs(2)
        for t in range(NT):
            i = t % 2
            P.dma("sp", lambda e, t=t, i=i: e.dma_start(out=y1[i], in_=YK[t * 128:(t + 1) * 128, :]), reads=[bYK], writes=[by1[i]])
            P.dma("sp", lambda e, t=t, i=i: e.dma_start(out=y2[i], in_=YK[TPC + t * 128:TPC + (t + 1) * 128, :]), reads=[bYK], writes=[by2[i]])
            P.dma("sp", lambda e, t=t, i=i: e.dma_start(out=ht[i], in_=Hin[t * 128:(t + 1) * 128, :]), reads=[bHin[t]], writes=[bht[i]])
            P.op("dve", lambda e, i=i: e.tensor_tensor(out=acc[i], in0=ht[i], in1=y1[i], op=ALU.add),
                 reads=[by1[i], bht[i]], writes=[bacc[i]], c=1.1)
            P.op("dve", lambda e, i=i: e.tensor_tensor(out=hn[i], in0=acc[i], in1=y2[i], op=ALU.add),
                 reads=[by2[i], bacc[i]], writes=[bhn[i]], c=1.1)
            if final:
                rmsnorm_tile(hn[i], bhn[i], gft, bgf, [(fo[i], bfo[i])], junk, bjunk, ss, rs, bss)
                P.dma("sp", lambda e, t=t, i=i: e.dma_start(out=out_d[t * 128:(t + 1) * 128, :], in_=fo[i]), reads=[bfo[i]], writes=[bOUT])
            else:
                P.dma("sp", lambda e, t=t, i=i: e.dma_start(out=Hout[t * 128:(t + 1) * 128, :], in_=hn[i]), reads=[bhn[i]], writes=[bH[id(Hout)][t]])
        P.barrier()

    def pool_phase():
        A.release(persist_mark)
        gbt = A.f32(D)
        bg = P.buf()
        P.dma("sp", lambda e: e.dma_start(out=gbt, in_=nmix_d[1:2, :].partition_broadcast(128)), writes=[bg])
        yT = v3(A.bf16(8 * SEQ), 8, SEQ)
        byT = P.bufs(16)
        zT = v3(A.bf16(8 * SEQ), 8, SEQ)
        bzT = P.buf()
        plT = v3(A.bf16(4 * SEQ), 4, SEQ)
        bplT = P.bufs(4)
        pw = PF["pool_w"]
        win, wou, wgp = pw["win"], pw["wou"], pw["wgp"]
        bwin, bwou, bwgp = pw["bwin"], pw["bwou"], pw["bwgp"]
        psc = A.f32(8)
        rc = A.f32(16)
        bpc = P.buf()
        P.dma("sp", lambda e: e.dma_start(out=psc, in_=pscT_d), writes=[bpc])
        P.dma("sp", lambda e: e.dma_start(out=rc, in_=c_rc_d), writes=[bpc])
        tl = dict(xt=[A.f32(D), A.f32(D)], bxt=P.bufs(2), yb=[A.bf16(D), A.bf16(D)], byb=P.bufs(2),
                  junk=A.f32(D), bjunk=P.buf(), ss=A.f32(2), rs=A.f32(2), bss=P.buf())
        uT = [A.f32(SEQ) for _ in range(2)]
        buT = P.bufs(2)
        sA = [A.f32(SEQ) for _ in range(2)]
        bsA = P.bufs(2)
        res = [tl["junk"], A.f32(D)]
        bres = [tl["bjunk"], P.buf()]
        h2_tiles = bH[id(H2)]
        xs_zero_begin()
        for s in range(2):
            seq_norm_transpose(H2, h2_tiles, s, gbt, bg, yT, byT, tl)
            for c in range(8):
                g = c // 2
                w = 2 << g
                u_, bu_ = uT[c % 2], buT[c % 2]
                xs_zero_some(5, after=[bu_])
                for q in range(4):
                    bk = (c * 4 + q) % 4
                    for k in range(8):
                        P.op("pe", lambda e, k=k, c=c, q=q, bk=bk: e.matmul(bank(bk), lhsT=win[:, k, c * 128:(c + 1) * 128], rhs=yT[:, k, q * 512:(q + 1) * 512],
                                                                         start=(k == 0), stop=(k == 7)), reads=[bwin] + byT[4 * q:4 * q + 4], writes=[pb[bk]])
                    P.op("act", lambda e, u_=u_, q=q, bk=bk: e.copy(out=u_[:, q * 512:(q + 1) * 512], in_=bank(bk)), reads=[pb[bk]], writes=[bu_])
                cur, bcur = u_, bu_
                sft = 1
                pp = 0
                while sft < w:
                    nx, bnx = sA[pp], bsA[pp]
                    P.op("dve", lambda e, nx=nx, cur=cur, sft=sft: e.tensor_copy(out=nx[:, 0:sft], in_=cur[:, 0:sft]), reads=[bcur], writes=[bnx])
                    P.op("dve", lambda e, nx=nx, cur=cur, sft=sft: e.tensor_tensor(out=nx[:, sft:SEQ], in0=cur[:, sft:SEQ], in1=cur[:, 0:SEQ - sft], op=ALU.add),
                         reads=[bcur], writes=[bnx])
                    cur, bcur = nx, bnx
                    pp = 1 - pp
                    sft *= 2
                P.op("dve", lambda e, cur=cur, u_=u_, c=c, w=w: e.scalar_tensor_tensor(out=plT[:, c % 4, :], in0=cur, scalar=1.0 / w, in1=u_, op0=ALU.mult, op1=ALU.subtract),
                     reads=[bcur, bu_], writes=[bplT[c % 4]])
                tmpc = sA[pp]
                btmpc = bsA[pp]
                P.op("dve", lambda e, cur=cur, tmpc=tmpc, w=w: e.tensor_tensor(out=tmpc[:, 0:w - 1], in0=cur[:, 0:w - 1], in1=rc[:, 0:w - 1], op=ALU.mult),
                     reads=[bcur, bpc], writes=[btmpc])
                P.op("dve", lambda e, tmpc=tmpc, u_=u_, c=c, w=w: e.tensor_tensor(out=plT[:, c % 4, 0:w - 1], in0=tmpc[:, 0:w - 1], in1=u_[:, 0:w - 1], op=ALU.subtract),
                     reads=[btmpc, bu_], writes=[bplT[c % 4]])
                if c % 2 == 1:
                    for eo in range(2):
                        co = 2 * g + eo
                        for q in range(4):
                            bk = 4 + (co * 4 + q) % 4
                            for ci in range(2):
                                P.op("pe", lambda e, g=g, eo=eo, ci=ci, q=q, bk=bk: e.matmul(
                                    bank(bk), lhsT=wgp[:, g, ci, eo * 128:(eo + 1) * 128], rhs=plT[:, (2 * g + ci) % 4, q * 512:(q + 1) * 512],
                                    start=(ci == 0), stop=(ci == 1)), reads=[bwgp, bplT[(2 * g + ci) % 4]], writes=[pb[bk]])
                            P.op("act", lambda e, co=co, q=q, bk=bk: e.activation(out=zT[:, co, q * 512:(q + 1) * 512], in_=bank(bk), func=AF.Copy, scale=psc[:, co:co + 1]),
                                 reads=[pb[bk], bpc], writes=[bzT])
            for t in range(16):
                gt = s * 16 + t
                b0 = 0 if t % 2 == 0 else 2
                for hf in range(2):
                    for k in range(8):
                        P.op("pe", lambda e, hf=hf, k=k, b0=b0, t=t: e.matmul(bank(b0 + hf), lhsT=zT[:, k, t * 128:(t + 1) * 128], rhs=wou[:, k, hf * 512:(hf + 1) * 512],
                                                                          start=(k == 0), stop=(k == 7)), reads=[bzT, bwou], writes=[pb[b0 + hf]])
                xt, bx = tl["xt"][t % 2], tl["bxt"][t % 2]
                P.dma("sp", lambda e, xt=xt, gt=gt: e.dma_start(out=xt, in_=H2[gt * 128:(gt + 1) * 128, :]), reads=[h2_tiles[gt]], writes=[bx])
                rs_, brs_ = res[t % 2], bres[t % 2]
                P.op("dve", lambda e, rs_=rs_, xt=xt, b0=b0: e.tensor_tensor(out=rs_, in0=xt, in1=bank(b0, 2), op=ALU.add),
                     reads=[bx, pb[b0], pb[b0 + 1]], writes=[brs_])
                P.dma("sp", lambda e, rs_=rs_, gt=gt: e.dma_start(out=H3[gt * 128:(gt + 1) * 128, :], in_=rs_), reads=[brs_], writes=[bH[id(H3)][gt]])
        P.barrier()

    phases = [("attn", attn_phase), ("moe0", lambda: moe_phase(0, H1, H2, False)),
              ("pool", pool_phase), ("moe1", lambda: moe_phase(1, H3, None, True))]
    for name, fn in phases:
        fn()
        if stop_after == name:
            break
    stats = P.finalize()
    P.es.close()
    return nc, stats


def make_constants():
    k = np.arange(128)[:, None]
    q = np.arange(128)[None, :]
    m = np.concatenate([(k <= q), (k >= q)], axis=1).astype(np.float32)
    mask = np.concatenate([m, m], axis=1)
    inv_freq = (500000.0 ** (-(np.arange(8, dtype=np.float32) * 2.0 / 16.0))).astype(np.float32)
    return dict(
        c_identf=np.eye(128, dtype=np.float32),
        c_mask=mask,
        c_triu=(k < q).astype(np.float32),
        c_ones=np.ones((128, 128), np.float32),
        c_invf=np.broadcast_to(inv_freq[None, :], (128, 8)).copy(),
        c_eoff=np.broadcast_to((np.arange(16, dtype=np.float32) * CAP)[None, :], (128, 16)).copy(),
        c_rc=np.broadcast_to((1.0 / np.arange(1, 17, dtype=np.float32))[None, :], (128, 16)).copy(),
        c_tokp1=(np.arange(NT, dtype=np.float32)[None, :] * 128 + np.arange(128, dtype=np.float32)[:, None] + 1.0).astype(np.float32),
        c_slot=(np.arange(NEXP * CAP_T, dtype=np.float32)[None, :] * 128 + np.arange(128, dtype=np.float32)[:, None]).astype(np.float32),
    )


def make_in_maps(inputs, ncores=NCORES):
    f = lambda a: np.ascontiguousarray(np.asarray(a, dtype=np.float32))
    x = f(inputs["x"])
    pos = np.asarray(inputs["positions"]).astype(np.int32)
    shared = dict(
        norm_mix=f(inputs["norm_mix"]), norm_ffn=f(inputs["norm_ffn"]),
        norm_final=f(inputs["norm_final"]).reshape(1, D),
        attn_w_in=f(inputs["attn_w_in"])[0], attn_w_out=f(inputs["attn_w_out"])[0],
        pool_w_in=f(inputs["pool_w_in"])[0], pool_w_group=f(inputs["pool_w_group"])[0],
        pool_scaleT=np.ascontiguousarray(f(inputs["pool_scale"])[0].reshape(8, 128).T),
        pool_w_out=f(inputs["pool_w_out"])[0],
        router_group_w=f(inputs["router_group_w"]), router_group_b=f(inputs["router_group_b"]),
        router_expert_w=f(inputs["router_expert_w"]),
        router_expert_b=f(inputs["router_expert_b"]).reshape(2, 16),
        expert_w1=f(inputs["expert_w1"]), expert_w3=f(inputs["expert_w3"]), expert_w2=f(inputs["expert_w2"]),
    )
    shared.update(make_constants())
    maps = []
    for c in range(ncores):
        xs = x[2 * c:2 * c + 2].reshape(TPC, D)
        p = pos[2 * c:2 * c + 2].reshape(NT, 128).T
        m = dict(shared)
        m["x"] = np.ascontiguousarray(xs)
        m["posT"] = np.ascontiguousarray(p)
        maps.append(m)
    return maps


_CACHE = {}


def kernel(**inputs):
    if "nc" not in _CACHE:
        _CACHE["nc"] = build_program()[0]
    nc = _CACHE["nc"]
    maps = make_in_maps(inputs)
    res = run_bass_kernel_spmd(nc, maps, core_ids=list(range(NCORES)))
    outs = [np.asarray(r["out"]).reshape(2, SEQ, D) for r in res.results]
    return np.concatenate(outs, axis=0).astype(np.float32)
```

```python
from contextlib import ExitStack
import math
import numpy as np
import ml_dtypes
import concourse.bass as bass
import concourse.mybir as mybir
from concourse.bass_utils import run_bass_kernel_spmd

F32 = mybir.dt.float32
BF16 = mybir.dt.bfloat16
I32 = mybir.dt.int32
ALU = mybir.AluOpType
AF = mybir.ActivationFunctionType
AX = mybir.AxisListType

NCORES = 8
SEQ = 2048
D = 1024
TPC = 4096
NT = 32
CAP_T = 5
CAP = CAP_T * 128
NEXP = 16
EPS = 1e-6
DILS = (1, 4, 16)
CUT = [0]
ZERO_FROM_TILE = 3
ATT_CFG = [3, 3]
M2_LAYOUT = [0]
M2_BANKS = [dict(h=[0], y=2, xt=[4, 5], ht=[6, 7]), dict(h=[0, 2], y=4, xt=[6], ht=[7])]

ENGS = ("pe", "act", "dve", "pool", "sp")
EPOCH = 12000
RING = {"sp": 40, "pool": 24, "act": 8}
DEF_COST = {"pe": 0.23, "act": 0.6, "dve": 0.8, "pool": 1.0, "sp": 0.1}
DEF_COST_DMA = 4.0
DMA_ISSUE = 0.3


class Buf:
    __slots__ = ("name", "w", "rs", "rd", "excl")

    def __init__(self, name):
        self.name = name
        self.excl = False
        self.w = []
        self.rs = []
        self.rd = []


class Op:
    __slots__ = ("eng", "fn", "deps", "needs_inc", "is_dma", "sem", "semval", "pre", "lidx", "seg", "cost", "fin")

    def __init__(self, eng, fn, is_dma):
        self.eng = eng
        self.fn = fn
        self.is_dma = is_dma
        self.lidx = 0
        self.seg = 0
        self.cost = 0.3
        self.fin = 0.0
        self.deps = []
        self.needs_inc = is_dma
        self.sem = None
        self.semval = None
        self.pre = None


class Prog:
    def __init__(self, nc):
        self.nc = nc
        self.es = ExitStack()
        self.streams = {e: [] for e in ENGS}
        self.ring = {}
        self.ring_n = {e: 0 for e in RING}
        for e, k in RING.items():
            self.ring[e] = [self._sem(f"dq_{e}_{i}") for i in range(k)]
        self.eng_sems = {e: [] for e in ENGS}
        self.nbuf = 0
        self.live_dma = []
        self.nops = 0
        self.seg = 0

    def _sem(self, name):
        return self.es.enter_context(self.nc.semaphore(name))

    def buf(self, name=None):
        self.nbuf += 1
        return Buf(name or f"b{self.nbuf}")

    def bufs(self, n):
        return [self.buf() for _ in range(n)]

    def _record(self, eng, fn, reads, writes, is_dma, c=None):
        o = Op(eng, fn, is_dma)
        self.nops += 1
        o.lidx = self.nops
        o.seg = self.seg
        o.cost = c if c is not None else (DEF_COST_DMA if is_dma else DEF_COST[eng])
        deps = {}
        ex = [b for b in reads if b.excl]
        if ex:
            reads = [b for b in reads if not b.excl]
            writes = list(writes) + [b for b in ex if b not in writes]
        for b in reads:
            for w_ in b.w:
                deps[id(w_)] = w_
        acc = []
        for b in writes:
            if is_dma and b.w and all(w_.is_dma for w_ in b.w) and not b.rs and not b.rd:
                acc.append(b)
                continue
            for w_ in b.w:
                deps[id(w_)] = w_
            for r in b.rs:
                deps[id(r)] = r
            for r in b.rd:
                deps[id(r)] = r
        o.deps = list(deps.values())
        for d in o.deps:
            d.needs_inc = True
        for b in reads:
            if is_dma:
                b.rd.append(o)
            else:
                b.rs.append(o)
        for b in writes:
            if b in acc:
                b.w.append(o)
            else:
                b.w = [o]
                b.rs = []
                b.rd = []
        if is_dma:
            self.live_dma.append(o)
        self.streams[eng].append(o)
        return o

    def op(self, eng, fn, reads=(), writes=(), c=None):
        return self._record(eng, fn, reads, writes, False, c)

    def dma(self, eng, fn, reads=(), writes=(), c=None):
        return self._record(eng, fn, reads, writes, True, c)

    def barrier(self):
        deps = list(self.live_dma)
        self.live_dma = []
        for d in deps:
            d.needs_inc = True
        self.seg += 1
        for e in ENGS:
            o = Op(e, lambda eng: eng.nop(), False)
            o.deps = list(deps)
            self.nops += 1
            o.lidx = self.nops
            o.seg = self.seg
            o.cost = 0.05
            self.streams[e].append(o)
        self.seg += 1

    def schedule(self):
        WINDOW = 48
        segs = {}
        for e in ENGS:
            for o in self.streams[e]:
                segs.setdefault(o.seg, {}).setdefault(e, []).append(o)
        final = {e: [] for e in ENGS}
        tnow = 0.0
        done = set()
        for sg in sorted(segs):
            per = segs[sg]
            if sg % 2 == 1:
                tails = [final[e2][-1 - i] for e2 in ENGS for i in range(min(len(final[e2]), 1))]
                tails = []
                for e2 in ENGS:
                    for o2 in reversed(final[e2]):
                        if not o2.is_dma:
                            tails.append(o2)
                            break
                for e in ENGS:
                    for o in per.get(e, []):
                        o.deps = list(o.deps) + tails
                        for d in tails:
                            d.needs_inc = True
                        o.fin = tnow
                        done.add(id(o))
                        final[e].append(o)
                continue
            et = {e: tnow for e in ENGS}
            pend = {e: list(per.get(e, [])) for e in ENGS}
            nleft = sum(len(v) for v in pend.values())
            while nleft:
                best = None
                for e in ENGS:
                    lst = pend[e]
                    for i in range(min(len(lst), WINDOW)):
                        o = lst[i]
                        st = et[e]
                        ok = True
                        for d in o.deps:
                            if id(d) not in done:
                                ok = False
                                break
                            if d.fin > st:
                                st = d.fin
                        if not ok:
                            continue
                        key = (st, o.lidx)
                        if best is None or key < best[0]:
                            best = (key, e, i, o, st)
                        if st <= et[e]:
                            break
                assert best is not None, "scheduler deadlock"
                _, e, i, o, st = best
                pend[e].pop(i)
                nleft -= 1
                if o.is_dma:
                    o.fin = st + o.cost
                    et[e] = st + DMA_ISSUE
                else:
                    o.fin = st + o.cost
                    et[e] = o.fin
                done.add(id(o))
                final[e].append(o)
            tnow = max([tnow] + [o.fin for e in ENGS for o in per.get(e, [])])
        self.streams = final
        self.est_us = tnow

    def finalize(self):
        nc = self.nc
        self.schedule()
        for e in RING:
            k = len(self.ring[e])
            i = 0
            for o in self.streams[e]:
                if not o.is_dma:
                    continue
                o.sem = self.ring[e][i % k]
                o.semval = 16 * (i // k + 1)
                if i >= k:
                    o.pre = (o.sem, 16 * (i // k))
                i += 1
        for e in ENGS:
            cnt = 0
            for o in self.streams[e]:
                if o.is_dma or not o.needs_inc:
                    continue
                ep = cnt // EPOCH
                while len(self.eng_sems[e]) <= ep:
                    self.eng_sems[e].append(self._sem(f"cs_{e}_{len(self.eng_sems[e])}"))
                o.sem = self.eng_sems[e][ep]
                o.semval = cnt % EPOCH + 1
                cnt += 1
        handles = {"pe": "tensor", "act": "scalar", "dve": "vector", "pool": "gpsimd", "sp": "sync"}
        stats = {}
        with nc.Block() as block:
            for e in ENGS:
                ops = self.streams[e]
                if not ops:
                    continue

                def body(eng, ops=ops, e=e):
                    waited = {}
                    nw = 0
                    for o in ops:
                        ws = []
                        if o.pre is not None:
                            ws.append(o.pre)
                        for d in o.deps:
                            if d.eng == e and e == "pe" and not d.is_dma:
                                continue
                            ws.append((d.sem, d.semval))
                        for (s, v) in ws:
                            key = id(s)
                            if waited.get(key, 0) >= v:
                                continue
                            waited[key] = v
                            eng.wait_ge(s, v)
                            nw += 1
                        ins = o.fn(eng)
                        if o.needs_inc:
                            ins.then_inc(o.sem, 16 if o.is_dma else 1)
                    for o in ops:
                        if o.is_dma:
                            key = id(o.sem)
                            if waited.get(key, 0) < o.semval:
                                waited[key] = o.semval
                                eng.wait_ge(o.sem, o.semval)
                    stats[e] = (len(ops), nw)

                getattr(block, handles[e])(body)
        self.stats = stats
        return stats


class Arena:
    def __init__(self, ap, n):
        self.ap = ap
        self.n = n
        self.off = 0
        self.top = n

    def top_reset(self):
        self.top = self.n

    def top_bf16(self, n):
        w = (n + 3) // 4 * 2
        self.top -= w
        assert self.off <= self.top, ("arena overflow (top)", self.off, self.top)
        return self.ap[:, self.top:self.top + w].bitcast(BF16)[:, 0:n]

    def mark(self):
        return self.off

    def release(self, m):
        self.off = m

    def f32(self, n):
        n2 = (n + 1) // 2 * 2
        assert self.off + n2 <= self.top, ("arena overflow", self.off, n2, self.top)
        a = self.ap[:, self.off:self.off + n]
        self.off += n2
        return a

    def bf16(self, n):
        w = (n + 3) // 4 * 2
        assert self.off + w <= self.top, ("arena overflow", self.off, w, self.top)
        a = self.ap[:, self.off:self.off + w].bitcast(BF16)[:, 0:n]
        self.off += w
        return a

    def i32(self, n):
        return self.f32(n).bitcast(I32)


def v3(ap, a, b):
    return ap.rearrange("p (a b) -> p a b", a=a, b=b)


def v4(ap, a, b, c):
    return ap.rearrange("p (a b c) -> p a b c", a=a, b=b, c=c)


def build_program(dbg=False, stop_after=None):
    nc = bass.Bass("TRN2", target_bir_lowering=False)
    P = Prog(nc)

    def din(name, shape, dt=F32):
        return nc.dram_tensor(name, list(shape), dt, kind="ExternalInput").ap()

    def dscr(name, shape, dt, out=False):
        if out:
            return nc.dram_tensor(name, list(shape), dt, kind="ExternalOutput").ap()
        return nc.dram_tensor(name, list(shape), dt).ap()

    x_d = din("x", [TPC, D])
    posT_d = din("posT", [128, NT], I32)
    nmix_d = din("norm_mix", [2, D])
    nffn_d = din("norm_ffn", [2, D])
    nfin_d = din("norm_final", [1, D])
    awin_d = din("attn_w_in", [D, 4608])
    awout_d = din("attn_w_out", [512, D])
    pwin_d = din("pool_w_in", [D, D])
    pwg_d = din("pool_w_group", [4, 256, 256])
    pscT_d = din("pool_scaleT", [128, 8])
    pwout_d = din("pool_w_out", [D, D])
    rgw_d = din("router_group_w", [2, D, 4])
    rgb_d = din("router_group_b", [2, 4])
    rew_d = din("router_expert_w", [2, 4, D, 4])
    reb_d = din("router_expert_b", [2, 16])
    w1_d = din("expert_w1", [2, NEXP, D, 512])
    w3_d = din("expert_w3", [2, NEXP, D, 512])
    w2_d = din("expert_w2", [2, NEXP, 512, D])
    c_identf_d = din("c_identf", [128, 128])
    c_mask_d = din("c_mask", [128, 512])
    c_triu_d = din("c_triu", [128, 128])
    c_ones_d = din("c_ones", [128, 128])
    c_invf_d = din("c_invf", [128, 8])
    c_eoff_d = din("c_eoff", [128, 16])
    c_rc_d = din("c_rc", [128, 16])
    c_tokp1_d = din("c_tokp1", [128, NT])
    c_slot_d = din("c_slot", [128, NEXP * CAP_T])
    out_d = nc.dram_tensor("out", [TPC, D], F32, kind="ExternalOutput").ap()

    H1 = dscr("H1", [TPC, D], F32, out=dbg)
    H2 = dscr("H2", [TPC, D], F32, out=dbg)
    H3 = dscr("H3", [TPC, D], F32, out=dbg)
    OGZ = [dscr(f"OGZ{g}", [TPC, 264], F32) for g in range(3)]
    XS = dscr("XS", [NEXP * CAP, 516], F32)
    YK = dscr("YK", [2 * TPC, D], BF16)
    bH = {id(h): P.bufs(NT) for h in (H1, H2, H3)}
    bOGZ = [P.bufs(NT) for _ in range(3)]
    bXS = P.buf()
    bYK = P.buf()
    bYKz = P.buf()
    bOUT = P.buf()

    ARENA_N = 46000
    arena_t = P.es.enter_context(nc.sbuf_tensor("arena", [128, ARENA_N], F32))
    A = Arena(arena_t, ARENA_N)
    ps_t = P.es.enter_context(nc.psum_tensor("ps", [128, 4096], F32))

    def bank(i, n=1):
        return ps_t[:, i * 512:(i + n) * 512]

    def bank_bf(i):
        return ps_t[:, i * 512:(i + 1) * 512].bitcast(BF16)

    pb = P.bufs(8)
    PF = {}
    for b_ in pb:
        b_.excl = True
    _bc = {}

    def bc_reg(e, val=None):
        val = NEXP * CAP - 1 if val is None else val
        if val not in _bc:
            _bc[val] = e.to_reg(val)
        return _bc[val]

    identf = A.f32(128)
    identb = A.bf16(128)
    maskb = A.bf16(512)
    bconst = P.buf()
    P.dma("sp", lambda e: e.dma_start(out=identf, in_=c_identf_d), writes=[bconst])
    zt = A.f32(516)
    bzt = P.buf()
    bXSz = P.buf()
    tmpm = zt[:, 0:512]
    P.dma("sp", lambda e: e.dma_start(out=tmpm, in_=c_mask_d), writes=[bzt])
    P.op("dve", lambda e: e.tensor_copy(out=identb, in_=identf), reads=[bconst], writes=[bconst])
    P.op("dve", lambda e: e.tensor_copy(out=maskb, in_=tmpm), reads=[bconst, bzt], writes=[bconst])
    P.op("pool", lambda e: e.memset(zt, 0.0), writes=[bzt])
    XSv = XS.rearrange("(n p) d -> n p d", p=128)
    zero_state = {"jobs": []}

    def xs_zero_begin():
        zero_state["jobs"] = [ex_ * CAP_T + j_ for ex_ in range(NEXP) for j_ in range(CAP_T) if j_ >= ZERO_FROM_TILE]

    def xs_zero_some(n, after=()):
        for _ in range(n):
            if zero_state["jobs"]:
                n_ = zero_state["jobs"].pop(0)
                P.dma("sp", lambda e, n_=n_: e.dma_start(out=XSv[n_], in_=zt), reads=[bzt, bXS] + list(after), writes=[bXSz], c=4.0)
    persist_mark = A.mark()

    def rmsnorm_tile(xt, bx, gbt, bg, outs, junk, bjunk, ss, rs, bss):
        ss = ss[:, 0:1]
        rs = rs[:, 0:1]
        P.op("act", lambda e: e.activation(out=junk, in_=xt, func=AF.Square, accum_out=ss),
             reads=[bx], writes=[bjunk, bss])
        P.op("act", lambda e: e.activation(out=rs, in_=ss, func=AF.Sqrt, scale=1.0 / D, bias=EPS),
             reads=[bss], writes=[bss])
        P.op("dve", lambda e: e.reciprocal(out=rs, in_=rs), reads=[bss], writes=[bss])
        for (o_ap, o_b) in outs:
            P.op("dve", lambda e, o_ap=o_ap: e.scalar_tensor_tensor(
                out=o_ap, in0=xt, scalar=rs[:, 0:1], in1=gbt, op0=ALU.mult, op1=ALU.mult),
                reads=[bx, bss, bg], writes=[o_b])

    def seq_norm_transpose(src, bsrc_tiles, s, gbt, bg, yT, byT, tl):
        for t in range(16):
            gt = s * 16 + t
            xt, bx = tl["xt"][t % 2], tl["bxt"][t % 2]
            yb, byb = tl["yb"][t % 2], tl["byb"][t % 2]
            P.dma("sp", lambda e, xt=xt, gt=gt: e.dma_start(out=xt, in_=src[gt * 128:(gt + 1) * 128, :]),
                  reads=[bsrc_tiles[gt]], writes=[bx])
            rmsnorm_tile(xt, bx, gbt, bg, [(yb, byb)], tl["junk"], tl["bjunk"], tl["ss"], tl["rs"], tl["bss"])
            bk = 4 + (t % 2)
            pbf = bank_bf(bk)
            for k in range(8):
                P.op("pe", lambda e, k=k, pbf=pbf, yb=yb: e.transpose(
                    out=pbf[:, k * 128:(k + 1) * 128], in_=yb[:, k * 128:(k + 1) * 128], identity=identb),
                    reads=[byb, bconst], writes=[pb[bk]])
            eng = "act" if t % 2 == 0 else "dve"
            if eng == "act":
                P.op("act", lambda e, pbf=pbf, t=t: e.copy(out=yT[:, :, t * 128:(t + 1) * 128], in_=v3(pbf, 8, 128)),
                     reads=[pb[bk]], writes=[byT[t]])
            else:
                P.op("dve", lambda e, pbf=pbf, t=t: e.tensor_copy(out=yT[:, :, t * 128:(t + 1) * 128], in_=v3(pbf, 8, 128)),
                     reads=[pb[bk]], writes=[byT[t]])

    def attn_phase():
        A.release(persist_mark)
        gbt = A.f32(D)
        bg = P.buf()
        P.dma("sp", lambda e: e.dma_start(out=gbt, in_=nmix_d[0:1, :].partition_broadcast(128)), writes=[bg])
        yT = v3(A.bf16(8 * SEQ), 8, SEQ)
        byT = P.bufs(16)
        wg = v3(A.bf16(8 * 1536), 8, 1536)
        bwg = P.buf()
        qT = v3(A.bf16(4 * SEQ), 4, SEQ)
        kT = v3(A.bf16(4 * SEQ), 4, SEQ)
        bqT, bkT = P.buf(), P.buf()
        Va = v4(A.bf16(16 * 520), 16, 8, 65)
        bVa = P.bufs(16)
        bVa1 = P.buf()
        wout = v3(A.bf16(4 * D), 4, D)
        bwout = P.buf()
        tl = dict(xt=[A.f32(D), A.f32(D)], bxt=P.bufs(2), yb=[A.bf16(D), A.bf16(D)], byb=P.bufs(2),
                  junk=A.f32(D), bjunk=P.buf(), ss=A.f32(2), rs=A.f32(2), bss=P.buf())
        qk = [A.bf16(D), A.bf16(D)]
        bqk = P.bufs(2)
        NE = ATT_CFG[0]
        Eb = [v4(A.bf16(4 * 512), 4, 2, 256) for _ in range(NE)]
        bE = [P.bufs(4) for _ in range(NE)]
        OZ = [A.f32(264) for _ in range(3)]
        bOZ = P.bufs(3)
        posi = A.i32(NT)
        posf = A.f32(NT)
        invf = A.f32(8)
        ang = A.f32(NT * 8)
        a2 = A.f32(NT * 8)
        nf = A.f32(NT * 8)
        ni = A.i32(NT * 8)
        mk = A.f32(NT * 8)
        cosT = v3(A.f32(NT * 8), NT, 8)
        sinT = v3(A.f32(NT * 8), NT, 8)
        brot = P.buf()
        rt = [A.f32(128) for _ in range(4)]
        brt = P.bufs(4)
        ozl = [[A.f32(264) for _ in range(3)] for _ in range(2)]
        bozl = [P.bufs(3) for _ in range(2)]
        zs = A.f32(8)
        us = A.f32(512)
        bmg = P.buf()
        ob = A.bf16(512)
        bob = P.buf()
        oT = v3(A.bf16(4 * 128), 4, 128)
        boT = P.buf()
        res = [tl["junk"], A.f32(D)]
        bres = [tl["bjunk"], P.buf()]

        P.dma("sp", lambda e: e.dma_start(out=posi, in_=posT_d), writes=[brot])
        P.dma("sp", lambda e: e.dma_start(out=invf, in_=c_invf_d), writes=[brot])
        P.op("dve", lambda e: e.tensor_copy(out=posf, in_=posi), reads=[brot], writes=[brot])
        P.op("dve", lambda e: e.tensor_tensor(
            out=v3(ang, NT, 8), in0=posf.unsqueeze(2).to_broadcast([128, NT, 8]),
            in1=invf.unsqueeze(1).to_broadcast([128, NT, 8]), op=ALU.mult), reads=[brot], writes=[brot])
        TWO_PI = 2.0 * math.pi
        C1 = 6.28125
        C2 = TWO_PI - C1
        for (tab, shift) in ((sinT, 0.0), (cosT, 0.5 * math.pi)):
            tabf = tab.rearrange("p a b -> p (a b)")
            P.op("dve", lambda e, shift=shift: e.tensor_scalar(out=a2, in0=ang, scalar1=shift, scalar2=None, op0=ALU.add),
                 reads=[brot], writes=[brot])
            P.op("dve", lambda e: e.tensor_scalar(out=ni, in0=a2, scalar1=1.0 / TWO_PI, scalar2=None, op0=ALU.mult),
                 reads=[brot], writes=[brot])
            P.op("dve", lambda e: e.tensor_copy(out=nf, in_=ni), reads=[brot], writes=[brot])
            P.op("dve", lambda e: e.scalar_tensor_tensor(out=a2, in0=nf, scalar=-C1, in1=a2, op0=ALU.mult, op1=ALU.add),
                 reads=[brot], writes=[brot])
            P.op("dve", lambda e: e.scalar_tensor_tensor(out=a2, in0=nf, scalar=-C2, in1=a2, op0=ALU.mult, op1=ALU.add),
                 reads=[brot], writes=[brot])
            P.op("dve", lambda e: e.tensor_scalar(out=mk, in0=a2, scalar1=math.pi, scalar2=None, op0=ALU.is_gt),
                 reads=[brot], writes=[brot])
            P.op("dve", lambda e: e.scalar_tensor_tensor(out=a2, in0=mk, scalar=-TWO_PI, in1=a2, op0=ALU.mult, op1=ALU.add),
                 reads=[brot], writes=[brot])
            P.op("dve", lambda e: e.tensor_scalar(out=mk, in0=a2, scalar1=-math.pi, scalar2=None, op0=ALU.is_lt),
                 reads=[brot], writes=[brot])
            P.op("dve", lambda e: e.scalar_tensor_tensor(out=a2, in0=mk, scalar=TWO_PI, in1=a2, op0=ALU.mult, op1=ALU.add),
                 reads=[brot], writes=[brot])
            P.op("dve", lambda e: e.tensor_scalar(out=a2, in0=a2, scalar1=math.pi, scalar2=-math.pi, op0=ALU.min, op1=ALU.max),
                 reads=[brot], writes=[brot])
            P.op("act", lambda e, tabf=tabf: e.activation(out=tabf, in_=a2, func=AF.Sin), reads=[brot], writes=[brot])

        if CUT[0] == 1:
            P.barrier()
            return
        P.op("pool", lambda e: e.memset(Va[:, :, :, 64:65], 1.0), writes=[bVa1])
        P.dma("pool", lambda e: e.dma_start(out=wout, in_=awout_d.rearrange("(k p) n -> p k n", p=128)), writes=[bwout])
        for i in range(3):
            P.op("pool", lambda e, i=i: e.memset(OZ[i], 0.0), writes=[bOZ[i]])

        x_tiles = [P.buf() for _ in range(NT)]
        xs_zero_begin()
        ei = 0
        ozi = 0
        for s in range(2):
            seq_norm_transpose(x_d, x_tiles, s, gbt, bg, yT, byT, tl)
            if CUT[0] == 2:
                P.barrier()
                return
            for g in range(3):
                d = DILS[g]
                nb = 16 // d
                P.dma("pool", lambda e, g=g: e.dma_start(
                    out=wg, in_=awin_d[:, g * 1536:(g + 1) * 1536].rearrange("(k p) n -> p k n", p=128)),
                    writes=[bwg])
                if CUT[0] == 31:
                    P.barrier()
                    return
                for t in range(16):
                    gt = s * 16 + t
                    if CUT[0] in (32, 33, 34) and t == 1:
                        P.barrier()
                        return
                    b0 = 0 if t % 2 == 0 else 2
                    for j in range(2):
                        for k in range(8):
                            P.op("pe", lambda e, j=j, k=k, b0=b0, t=t: e.matmul(
                                bank(b0 + j), lhsT=yT[:, k, t * 128:(t + 1) * 128], rhs=wg[:, k, j * 512:(j + 1) * 512],
                                start=(k == 0), stop=(k == 7)), reads=[byT[t], bwg], writes=[pb[b0 + j]])
                    qkt, bq = qk[t % 2], bqk[t % 2]
                    xs_zero_some(1, after=[bq])
                    psq = bank(b0, 2)
                    P.op("act", lambda e, qkt=qkt, psq=psq: e.copy(out=qkt, in_=psq),
                         reads=[pb[b0], pb[b0 + 1]], writes=[bq])
                    if CUT[0] == 32:
                        continue
                    psv = v3(psq, 16, 64)
                    qkv = v3(qkt, 16, 64)
                    cb = cosT[:, gt:gt + 1, :].to_broadcast([128, 16, 8])
                    sb = sinT[:, gt:gt + 1, :].to_broadcast([128, 16, 8])
                    t1 = psv[:, :, 0:8]
                    t2 = psv[:, :, 8:16]
                    r = [v3(x_, 16, 8) for x_ in rt]
                    rd = [pb[b0], pb[b0 + 1], brot]
                    P.op("dve", lambda e, t1=t1, cb=cb, r=r: e.tensor_tensor(out=r[0], in0=t1, in1=cb, op=ALU.mult), reads=rd, writes=[brt[0]])
                    P.op("dve", lambda e, t2=t2, sb=sb, r=r: e.tensor_tensor(out=r[1], in0=t2, in1=sb, op=ALU.mult), reads=rd, writes=[brt[1]])
                    P.op("dve", lambda e, t2=t2, cb=cb, r=r: e.tensor_tensor(out=r[2], in0=t2, in1=cb, op=ALU.mult), reads=rd, writes=[brt[2]])
                    P.op("dve", lambda e, t1=t1, sb=sb, r=r: e.tensor_tensor(out=r[3], in0=t1, in1=sb, op=ALU.mult), reads=rd, writes=[brt[3]])
                    P.op("dve", lambda e, qkv=qkv, r=r: e.tensor_tensor(out=qkv[:, :, 0:8], in0=r[0], in1=r[1], op=ALU.subtract),
                         reads=[brt[0], brt[1]], writes=[bq])
                    P.op("dve", lambda e, qkv=qkv, r=r: e.tensor_tensor(out=qkv[:, :, 8:16], in0=r[2], in1=r[3], op=ALU.add),
                         reads=[brt[2], brt[3]], writes=[bq])
                    if CUT[0] == 33:
                        continue
                    bk = 4 + (t % 2)
                    pbf = bank_bf(bk)
                    for c in range(8):
                        P.op("pe", lambda e, c=c, pbf=pbf, qkt=qkt: e.transpose(
                            out=pbf[:, c * 128:(c + 1) * 128], in_=qkt[:, c * 128:(c + 1) * 128], identity=identb),
                            reads=[bq, bconst], writes=[pb[bk]])
                    P.op("act", lambda e, pbf=pbf, t=t: e.copy(out=qT[:, :, t * 128:(t + 1) * 128], in_=v3(pbf[:, 0:512], 4, 128)),
                         reads=[pb[bk]], writes=[bqT])
                    P.op("dve", lambda e, pbf=pbf, t=t: e.tensor_copy(out=kT[:, :, t * 128:(t + 1) * 128], in_=v3(pbf[:, 512:1024], 4, 128)),
                         reads=[pb[bk]], writes=[bkT])
                if CUT[0] == 3:
                    P.barrier()
                    return
                for blk in range(16):
                    ph, b = blk // nb, blk % nb
                    st = b * 128 * d + ph
                    bk = 6 + (blk % 2)
                    for k in range(8):
                        P.op("pe", lambda e, k=k, st=st, d=d, bk=bk: e.matmul(
                            bank(bk), lhsT=yT[:, k, st:st + 127 * d + 1:d], rhs=wg[:, k, 1024:1536],
                            start=(k == 0), stop=(k == 7)), reads=byT[st // 128:(st + 127 * d) // 128 + 1] + [bwg], writes=[pb[bk]])
                    eng = "act" if blk % 2 == 0 else "dve"
                    if eng == "act":
                        P.op("act", lambda e, blk=blk, bk=bk: e.copy(out=Va[:, blk, :, 0:64], in_=v3(bank(bk), 8, 64)),
                             reads=[pb[bk]], writes=[bVa[blk]])
                    else:
                        P.op("dve", lambda e, blk=blk, bk=bk: e.tensor_copy(out=Va[:, blk, :, 0:64], in_=v3(bank(bk), 8, 64)),
                             reads=[pb[bk]], writes=[bVa[blk]])
                if CUT[0] == 4:
                    P.barrier()
                    return
                sbi = 0
                pvi = 0
                for ph in range(d):
                    prevE = None
                    for b in range(nb):
                        blk = ph * nb + b
                        nq = 256 if b + 1 < nb else 128
                        st = b * 128 * d + ph
                        Ec, bEc = Eb[ei % NE], bE[ei % NE]
                        ei += 1
                        for c in range(4):
                            sbk = 2 * (sbi % ATT_CFG[1])
                            sbi += 1
                            for hh in range(2):
                                P.op("pe", lambda e, c=c, hh=hh, st=st, d=d, nq=nq, sbk=sbk: e.matmul(
                                    bank(sbk + hh)[:, 0:nq],
                                    lhsT=kT[hh * 64:(hh + 1) * 64, c, st:st + 127 * d + 1:d],
                                    rhs=qT[hh * 64:(hh + 1) * 64, c, st:st + (nq - 1) * d + 1:d],
                                    start=True, stop=True), reads=[bqT, bkT], writes=[pb[sbk + hh]])
                            P.op("act", lambda e, Ec=Ec, c=c, nq=nq, sbk=sbk: e.activation(
                                out=Ec[:, c, :, 0:nq], in_=v3(bank(sbk, 2), 2, 512)[:, :, 0:nq], func=AF.Exp, scale=0.125),
                                reads=[pb[sbk], pb[sbk + 1]], writes=[bEc[c]])
                            P.op("dve", lambda e, Ec=Ec, c=c, nq=nq: e.tensor_tensor(
                                out=Ec[:, c, :, 0:nq], in0=Ec[:, c, :, 0:nq], in1=v3(maskb, 2, 256)[:, :, 0:nq], op=ALU.mult),
                                reads=[bEc[c], bconst], writes=[bEc[c]])
                        pvb = (4 + 2 * (pvi % 2)) if ATT_CFG[1] == 2 else 6
                        pvi += 1
                        for h in range(8):
                            c, hh = h // 2, h % 2
                            o_ap = bank(pvb + h // 4)[:, (h % 4) * 65:(h % 4) * 65 + 65]
                            if b > 0:
                                Ep, bEp = prevE
                                P.op("pe", lambda e, o_ap=o_ap, Ep=Ep, c=c, hh=hh, blk=blk, h=h: e.matmul(
                                    o_ap, lhsT=Ep[:, c, hh, 128:256], rhs=Va[:, blk - 1, h, :], start=True, stop=False),
                                    reads=[bEp[c], bVa[blk - 1], bVa1], writes=[pb[pvb + h // 4]])
                            P.op("pe", lambda e, o_ap=o_ap, Ec=Ec, c=c, hh=hh, blk=blk, h=h, b=b: e.matmul(
                                o_ap, lhsT=Ec[:, c, hh, 0:128], rhs=Va[:, blk, h, :], start=(b == 0), stop=True),
                                reads=[bEc[c], bVa[blk], bVa1], writes=[pb[pvb + h // 4]])
                        prevE = (Ec, bEc)
                        oz, boz = OZ[ozi % 3], bOZ[ozi % 3]
                        ozi += 1
                        pv = v3(bank(pvb, 2), 2, 512)[:, :, 0:260].rearrange("p a (h e) -> p a h e", h=4, e=65)
                        P.op("act", lambda e, oz=oz, pv=pv: e.copy(
                            out=v4(oz[:, 0:256].bitcast(BF16), 2, 4, 64), in_=pv[:, :, :, 0:64]),
                            reads=[pb[pvb], pb[pvb + 1]], writes=[boz])
                        P.op("dve", lambda e, oz=oz, pv=pv: e.tensor_copy(
                            out=v4(oz[:, 256:264], 2, 4, 1), in_=pv[:, :, :, 64:65]),
                            reads=[pb[pvb], pb[pvb + 1]], writes=[boz])
                        r0 = s * SEQ + st
                        touched = sorted({(r0 + d * i) // 128 for i in (0, 127)})
                        tb = [bOGZ[g][ti] for ti in range(touched[0], touched[-1] + 1)]
                        P.dma("sp", lambda e, oz=oz, g=g, r0=r0, d=d: e.dma_start(
                            out=OGZ[g][r0:r0 + 127 * d + 1:d, :], in_=oz), reads=[boz], writes=tb)
                if CUT[0] == 5:
                    P.barrier()
                    return
            if CUT[0] == 6:
                P.barrier()
                return
            for t in range(16):
                gt = s * 16 + t
                ol, bol = ozl[t % 2], bozl[t % 2]
                for g in range(3):
                    P.dma("sp", lambda e, g=g, ol=ol, gt=gt: e.dma_start(out=ol[g], in_=OGZ[g][gt * 128:(gt + 1) * 128, :]),
                          reads=[bOGZ[g][gt]], writes=[bol[g]])
                zv = [ol[g][:, 256:264] for g in range(3)]
                uv = [ol[g][:, 0:256].bitcast(BF16) for g in range(3)]
                P.op("dve", lambda e, zv=zv: e.tensor_tensor(out=zs, in0=zv[0], in1=zv[1], op=ALU.add), reads=[bol[0], bol[1]], writes=[bmg])
                P.op("dve", lambda e, zv=zv: e.tensor_tensor(out=zs, in0=zs, in1=zv[2], op=ALU.add), reads=[bol[2], bmg], writes=[bmg])
                P.op("dve", lambda e: e.reciprocal(out=zs, in_=zs), reads=[bmg], writes=[bmg])
                P.op("dve", lambda e, uv=uv: e.tensor_tensor(out=us, in0=uv[0], in1=uv[1], op=ALU.add), reads=[bol[0], bol[1], bmg], writes=[bmg])
                P.op("dve", lambda e, uv=uv: e.tensor_tensor(out=us, in0=us, in1=uv[2], op=ALU.add), reads=[bol[2], bmg], writes=[bmg])
                P.op("dve", lambda e: e.tensor_tensor(out=v3(ob, 8, 64), in0=v3(us, 8, 64),
                                                      in1=zs.unsqueeze(2).to_broadcast([128, 8, 64]), op=ALU.mult),
                     reads=[bmg], writes=[bob])
                bk = 4 + (t % 2)
                pbf = bank_bf(bk)
                for k in range(4):
                    P.op("pe", lambda e, k=k, pbf=pbf: e.transpose(out=pbf[:, k * 128:(k + 1) * 128], in_=ob[:, k * 128:(k + 1) * 128], identity=identb),
                         reads=[bob, bconst], writes=[pb[bk]])
                P.op("act", lambda e, pbf=pbf: e.copy(out=oT, in_=v3(pbf[:, 0:512], 4, 128)), reads=[pb[bk]], writes=[boT])
                b0 = 0 if t % 2 == 0 else 2
                for hf in range(2):
                    for k in range(4):
                        P.op("pe", lambda e, hf=hf, k=k, b0=b0: e.matmul(
                            bank(b0 + hf), lhsT=oT[:, k, :], rhs=wout[:, k, hf * 512:(hf + 1) * 512],
                            start=(k == 0), stop=(k == 3)), reads=[boT, bwout], writes=[pb[b0 + hf]])
                xt, bx = tl["xt"][t % 2], tl["bxt"][t % 2]
                P.dma("sp", lambda e, xt=xt, gt=gt: e.dma_start(out=xt, in_=x_d[gt * 128:(gt + 1) * 128, :]), writes=[bx])
                rs_, brs_ = res[t % 2], bres[t % 2]
                P.op("dve", lambda e, rs_=rs_, xt=xt, b0=b0: e.tensor_tensor(out=rs_, in0=xt, in1=bank(b0, 2), op=ALU.add),
                     reads=[bx, pb[b0], pb[b0 + 1]], writes=[brs_])
                P.dma("sp", lambda e, rs_=rs_, gt=gt: e.dma_start(out=H1[gt * 128:(gt + 1) * 128, :], in_=rs_),
                      reads=[brs_], writes=[bH[id(H1)][gt]])
        P.barrier()

    def moe_phase(li, Hin, Hout, final):
        A.release(persist_mark)
        bHin = bH[id(Hin)]
        gbt = A.f32(D)
        bg = P.buf()
        P.dma("sp", lambda e: e.dma_start(out=gbt, in_=nffn_d[li:li + 1, :].partition_broadcast(128)), writes=[bg])
        idx = A.i32(2 * NT)
        gat = A.f32(2 * NT)
        broute = P.buf()
        m_phase = A.mark()
        A.top_reset()
        w1b = [v3(A.top_bf16(8 * 512), 8, 512), None]
        w3b = [v3(A.top_bf16(8 * 512), 8, 512), None]
        w2b = [v3(A.top_bf16(4 * D), 4, D), None]
        bw = [P.bufs(3) for _ in range(2)]
        pre_w = {}

        def prefetch_expert0(after):
            if pre_w:
                return
            pre_w["done"] = True
            for (wt_, src_, i_) in ((w1b[0], w1_d, 0), (w3b[0], w3_d, 1), (w2b[0], w2_d, 2)):
                P.dma("pool", lambda e, wt_=wt_, src_=src_: e.dma_start(out=wt_, in_=src_[li, 0].rearrange("(k p) n -> p k n", p=128)),
                      reads=list(after), writes=[bw[0][i_]], c=22.0)

        xs_zero_some(1000)
        ybA = v3(A.f32(NT * 516), NT, 516)

        def yb_t(t):
            return ybA[:, t, 0:512].bitcast(BF16)
        bybA = P.bufs(NT)
        wr = v3(A.f32(8 * 20), 8, 20)
        bwr = P.buf()
        rb = A.f32(20)
        triu = A.f32(128)
        onesm = A.f32(128)
        eoff = A.f32(16)
        P.dma("sp", lambda e: e.dma_start(out=wr[:, :, 0:4], in_=rgw_d[li].rearrange("(k p) n -> p k n", p=128)), writes=[bwr])
        for g in range(4):
            P.dma("sp", lambda e, g=g: e.dma_start(out=wr[:, :, 4 + 4 * g:8 + 4 * g],
                                                    in_=rew_d[li, g].rearrange("(k p) n -> p k n", p=128)), writes=[bwr])
        P.dma("sp", lambda e: e.dma_start(out=rb[:, 0:4], in_=rgb_d[li:li + 1, :].partition_broadcast(128)), writes=[bwr])
        P.dma("sp", lambda e: e.dma_start(out=rb[:, 4:20], in_=reb_d[li:li + 1, :].partition_broadcast(128)), writes=[bwr])
        P.dma("sp", lambda e: e.dma_start(out=triu, in_=c_triu_d), writes=[bwr])
        P.dma("sp", lambda e: e.dma_start(out=onesm, in_=c_ones_d), writes=[bwr])
        P.dma("sp", lambda e: e.dma_start(out=eoff, in_=c_eoff_d), writes=[bwr])
        NB3 = 3
        xt2 = [A.f32(D) for _ in range(NB3)]
        bxt2 = P.bufs(NB3)
        yf = [A.f32(D) for _ in range(NB3)]
        byf = P.bufs(NB3)
        junk = A.f32(D)
        bjunk = P.buf()
        ss, rs = A.f32(2), A.f32(2)
        bss = P.buf()
        whl = v3(A.bf16(8 * 40), 8, 40)
        wtmp = v3(A.f32(8 * 20), 8, 20)
        bwhl = P.buf()
        P.op("dve", lambda e: e.tensor_copy(out=whl[:, :, 0:20], in_=wr), reads=[bwr], writes=[bwhl])
        P.op("dve", lambda e: e.tensor_tensor(out=wtmp, in0=wr, in1=whl[:, :, 0:20], op=ALU.subtract), reads=[bwr, bwhl], writes=[bwhl])
        P.op("dve", lambda e: e.tensor_copy(out=whl[:, :, 20:40], in_=wtmp), reads=[bwhl], writes=[bwhl])
        yl = [A.bf16(D) for _ in range(NB3)]
        byl = P.bufs(NB3)
        yhT = [v3(A.bf16(8 * 128), 8, 128) for _ in range(NB3)]
        ylT = [v3(A.bf16(8 * 128), 8, 128) for _ in range(NB3)]
        byhT, bylT = P.bufs(NB3), P.bufs(NB3)
        L = v3(A.f32(NT * 20), NT, 20)
        NG = 4
        GN = NT // NG
        bLg = P.bufs(NG)

        def tile_step(t):
            xt, bx = xt2[t % NB3], bxt2[t % NB3]
            P.dma("sp", lambda e, xt=xt, t=t: e.dma_start(out=xt, in_=Hin[t * 128:(t + 1) * 128, :]),
                  reads=[bHin[t]], writes=[bx])
            yft, byft = yf[t % NB3], byf[t % NB3]
            rmsnorm_tile(xt, bx, gbt, bg, [(yft, byft)], junk, bjunk, ss, rs, bss)
            P.op("act", lambda e, yft=yft, t=t: e.copy(out=yb_t(t), in_=yft), reads=[byft], writes=[bybA[t]], c=1.1)
            if t == 6:
                prefetch_expert0([bybA[t]])
            ylt, bylt = yl[t % NB3], byl[t % NB3]
            P.op("dve", lambda e, yft=yft, ylt=ylt, t=t: e.tensor_tensor(out=ylt, in0=yft, in1=yb_t(t), op=ALU.subtract),
                 reads=[byft, bybA[t]], writes=[bylt], c=1.1)
            b0 = 0 if t % 2 == 0 else 2
            ph_, pl_ = bank_bf(b0), bank_bf(b0 + 1)
            for k in range(8):
                P.op("pe", lambda e, k=k, t=t, ph_=ph_: e.transpose(
                    out=ph_[:, k * 128:(k + 1) * 128], in_=yb_t(t)[:, k * 128:(k + 1) * 128], identity=identb),
                    reads=[bybA[t], bconst], writes=[pb[b0]], c=0.08)
            for k in range(8):
                P.op("pe", lambda e, k=k, ylt=ylt, pl_=pl_: e.transpose(
                    out=pl_[:, k * 128:(k + 1) * 128], in_=ylt[:, k * 128:(k + 1) * 128], identity=identb),
                    reads=[bylt, bconst], writes=[pb[b0 + 1]], c=0.08)
            yh_, byh_ = yhT[t % NB3], byhT[t % NB3]
            yl_, byl_ = ylT[t % NB3], bylT[t % NB3]
            P.op("act", lambda e, yh_=yh_, ph_=ph_: e.copy(out=yh_, in_=v3(ph_, 8, 128)), reads=[pb[b0]], writes=[byh_], c=1.0)
            P.op("dve", lambda e, yl_=yl_, pl_=pl_: e.tensor_copy(out=yl_, in_=v3(pl_, 8, 128)), reads=[pb[b0 + 1]], writes=[byl_], c=1.0)
            lb = 4 + (t // 8) % 2
            c0 = (t % 8) * 60
            for k in range(8):
                P.op("pe", lambda e, k=k, yh_=yh_, lb=lb, c0=c0: e.matmul(
                    bank(lb)[:, c0:c0 + 40], lhsT=yh_[:, k, :], rhs=whl[:, k, :],
                    start=(k == 0), stop=(k == 7)), reads=[byh_, bwhl], writes=[pb[lb]], c=0.08)
            for k in range(8):
                P.op("pe", lambda e, k=k, yl_=yl_, lb=lb, c0=c0: e.matmul(
                    bank(lb)[:, c0 + 40:c0 + 60], lhsT=yl_[:, k, :], rhs=whl[:, k, 0:20],
                    start=(k == 0), stop=(k == 7)), reads=[byl_, bwhl], writes=[pb[lb]], c=0.08)
            if t % 8 == 7:
                hb = t // 8
                pv_ = v3(bank(lb)[:, 0:480], 8, 60)
                Lh = L[:, hb * 8:(hb + 1) * 8, :]
                bL_ = bLg[t // GN]
                P.op("dve", lambda e, pv_=pv_, Lh=Lh: e.tensor_tensor(
                    out=Lh, in0=pv_[:, :, 0:20], in1=rb.unsqueeze(1).to_broadcast([128, 8, 20]), op=ALU.add),
                    reads=[pb[lb], bwr], writes=[bL_])
                P.op("dve", lambda e, pv_=pv_, Lh=Lh: e.tensor_tensor(out=Lh, in0=Lh, in1=pv_[:, :, 20:40], op=ALU.add),
                     reads=[pb[lb], bL_], writes=[bL_])
                P.op("dve", lambda e, pv_=pv_, Lh=Lh: e.tensor_tensor(out=Lh, in0=Lh, in1=pv_[:, :, 40:60], op=ALU.add),
                     reads=[pb[lb], bL_], writes=[bL_])

        def T(n):
            return A.f32(NT * n)
        mg = T(1)
        G = v3(T(4), NT, 4)
        ex4 = v3(T(4), NT, 4)
        se = T(1)
        g1 = T(1)
        tmp16 = v3(T(16), NT, 16)
        sel = v3(T(4), NT, 4)
        m1, m2 = T(1), T(1)
        o1 = v3(T(4), NT, 4)
        o2 = v3(T(4), NT, 4)
        sel2 = v3(T(4), NT, 4)
        dl, exd, w1g, w2g = T(1), T(1), T(1), T(1)
        E1 = v3(T(16), NT, 16)
        E2 = v3(T(16), NT, 16)
        S16 = v3(T(16), NT, 16)
        Bc = [v3(T(16), NT, 16), v3(T(16), NT, 16)]
        rank = v3(T(16), NT, 16)
        valid = v3(T(16), NT, 16)
        pos16 = v3(T(16), NT, 16)
        idf = v3(T(2), 2, NT)
        vld = v3(T(2), 2, NT)
        tokp1 = A.f32(NT)
        btk = P.buf()
        P.dma("sp", lambda e: e.dma_start(out=tokp1, in_=c_tokp1_d), writes=[btk])
        BIG = float(NEXP * CAP + 64)
        idx3 = v3(idx, 2, NT)
        gat3 = v3(gat, 2, NT)
        brg = P.bufs(NG)
        broute_g = P.bufs(NG)
        bexg = P.bufs(NG)
        incs = []

        def route(gi):
            t0, t1 = gi * GN, (gi + 1) * GN
            sl = slice(t0, t1)
            br = brg[gi]
            bL_ = bLg[gi]
            deps_prev = [brg[gi - 1]] if gi > 0 else []

            def dv(fn, reads=(), writes=()):
                P.op("dve", fn, reads=[br, bL_, bwr] + list(reads), writes=[br] + list(writes), c=0.3)

            def bc1(ap1):
                return ap1.unsqueeze(2).to_broadcast([128, GN, 4])

            def g4(ap3):
                return ap3.rearrange("p a (g e) -> p a g e", g=4, e=4)
            lgv = L[:, sl, 0:4]
            lev = L[:, sl, 4:20]
            mg_, se_, g1_, m1_, m2_, dl_, exd_, w1_, w2_ = [x_[:, sl] for x_ in (mg, se, g1, m1, m2, dl, exd, w1g, w2g)]
            G_, ex4_, sel_, o1_, o2_, sel2_ = [x_[:, sl, :] for x_ in (G, ex4, sel, o1, o2, sel2)]
            tmp_, E1_, E2_, S_, rank_, valid_, pos_ = [x_[:, sl, :] for x_ in (tmp16, E1, E2, S16, rank, valid, pos16)]
            B0, B1 = Bc[0][:, sl, :], Bc[1][:, sl, :]
            dv(lambda e: e.tensor_reduce(out=mg_, in_=lgv, axis=AX.X, op=ALU.max))
            dv(lambda e: e.tensor_tensor(out=G_, in0=lgv, in1=bc1(mg_), op=ALU.is_equal))
            dv(lambda e: e.tensor_tensor(out=ex4_, in0=lgv, in1=bc1(mg_), op=ALU.subtract))
            P.op("act", lambda e: e.activation(out=ex4_, in_=ex4_, func=AF.Exp), reads=[br], writes=[br], c=0.3)
            dv(lambda e: e.tensor_reduce(out=se_, in_=ex4_, axis=AX.X, op=ALU.add))
            dv(lambda e: e.reciprocal(out=g1_, in_=se_))
            dv(lambda e: e.tensor_tensor(out=g4(tmp_), in0=g4(lev), in1=G_.unsqueeze(3).to_broadcast([128, GN, 4, 4]), op=ALU.mult))
            dv(lambda e: e.tensor_reduce(out=sel_, in_=tmp_.rearrange("p a (g e) -> p a e g", g=4, e=4), axis=AX.X, op=ALU.add))
            dv(lambda e: e.tensor_reduce(out=m1_, in_=sel_, axis=AX.X, op=ALU.max))
            dv(lambda e: e.tensor_tensor(out=o1_, in0=sel_, in1=bc1(m1_), op=ALU.is_equal))
            dv(lambda e: e.tensor_scalar(out=sel2_, in0=o1_, scalar1=-1e30, scalar2=None, op0=ALU.mult))
            dv(lambda e: e.tensor_tensor(out=sel2_, in0=sel2_, in1=sel_, op=ALU.add))
            dv(lambda e: e.tensor_reduce(out=m2_, in_=sel2_, axis=AX.X, op=ALU.max))
            dv(lambda e: e.tensor_tensor(out=o2_, in0=sel2_, in1=bc1(m2_), op=ALU.is_equal))
            dv(lambda e: e.tensor_tensor(out=dl_, in0=m2_, in1=m1_, op=ALU.subtract))
            P.op("act", lambda e: e.activation(out=exd_, in_=dl_, func=AF.Exp), reads=[br], writes=[br], c=0.3)
            dv(lambda e: e.tensor_scalar(out=w1_, in0=exd_, scalar1=1.0, scalar2=None, op0=ALU.add))
            dv(lambda e: e.reciprocal(out=w1_, in_=w1_))
            dv(lambda e: e.tensor_tensor(out=w2_, in0=exd_, in1=w1_, op=ALU.mult))
            dv(lambda e: e.tensor_tensor(out=w1_, in0=w1_, in1=g1_, op=ALU.mult))
            dv(lambda e: e.tensor_tensor(out=w2_, in0=w2_, in1=g1_, op=ALU.mult))
            for (Ek, ok) in ((E1_, o1_), (E2_, o2_)):
                dv(lambda e, Ek=Ek, ok=ok: e.tensor_tensor(
                    out=g4(Ek), in0=G_.unsqueeze(3).to_broadcast([128, GN, 4, 4]),
                    in1=ok.unsqueeze(2).to_broadcast([128, GN, 4, 4]), op=ALU.mult))
            dv(lambda e: e.tensor_tensor(out=S_, in0=E1_, in1=E2_, op=ALU.add))
            Sf = S16.rearrange("p a b -> p (a b)")[:, t0 * 16:t1 * 16]
            cs = slice(gi * GN * 16, (gi + 1) * GN * 16)
            P.op("pe", lambda e: e.matmul(bank(6)[:, cs], lhsT=triu, rhs=Sf, start=True, stop=True), reads=[br, bwr], writes=[pb[6]])
            P.op("pe", lambda e: e.matmul(bank(7)[:, cs], lhsT=onesm, rhs=Sf, start=True, stop=True), reads=[br, bwr], writes=[pb[7]])
            psA = v3(bank(6)[:, cs], GN, 16)
            psB = v3(bank(7)[:, cs], GN, 16)
            P.op("dve", lambda e: e.tensor_copy(out=B0, in_=psB), reads=[pb[7], br], writes=[br])
            cur = [B0, B1]
            sft = 1
            while sft < GN:
                a_, b_ = cur
                dv(lambda e, a_=a_, b_=b_, sft=sft: e.tensor_copy(out=b_[:, 0:sft, :], in_=a_[:, 0:sft, :]))
                dv(lambda e, a_=a_, b_=b_, sft=sft: e.tensor_tensor(out=b_[:, sft:GN, :], in0=a_[:, sft:GN, :], in1=a_[:, 0:GN - sft, :], op=ALU.add))
                cur = [b_, a_]
                sft *= 2
            inc = cur[0]
            if gi > 0:
                pin = incs[gi - 1]
                dv(lambda e, inc=inc, pin=pin: e.tensor_tensor(out=inc, in0=inc, in1=pin[:, GN - 1:GN, :].to_broadcast([128, GN, 16]), op=ALU.add),
                   reads=deps_prev)
            incs.append(inc)
            P.op("dve", lambda e, inc=inc: e.tensor_tensor(out=rank_, in0=inc, in1=psB, op=ALU.subtract), reads=[pb[7], br], writes=[br])
            P.op("dve", lambda e: e.tensor_tensor(out=rank_, in0=rank_, in1=psA, op=ALU.add), reads=[pb[6], br], writes=[br])
            dv(lambda e: e.tensor_scalar(out=valid_, in0=rank_, scalar1=float(CAP), scalar2=None, op0=ALU.is_lt))
            dv(lambda e: e.tensor_tensor(out=pos_, in0=rank_, in1=eoff.unsqueeze(1).to_broadcast([128, GN, 16]), op=ALU.add))
            dv(lambda e: e.tensor_scalar(out=pos_, in0=pos_, scalar1=-BIG, scalar2=None, op0=ALU.add))
            dv(lambda e: e.tensor_tensor(out=pos_, in0=pos_, in1=valid_, op=ALU.mult))
            dv(lambda e: e.tensor_scalar(out=pos_, in0=pos_, scalar1=BIG, scalar2=None, op0=ALU.add))
            brt = broute_g[gi]
            for k, (Ek, wk) in enumerate(((E1_, w1_), (E2_, w2_))):
                dv(lambda e, Ek=Ek: e.tensor_tensor(out=tmp_, in0=Ek, in1=pos_, op=ALU.mult))
                dv(lambda e, k=k: e.tensor_reduce(out=idf[:, k, sl], in_=tmp_, axis=AX.X, op=ALU.add))
                dv(lambda e, Ek=Ek: e.tensor_tensor(out=tmp_, in0=Ek, in1=valid_, op=ALU.mult))
                dv(lambda e, k=k: e.tensor_reduce(out=vld[:, k, sl], in_=tmp_, axis=AX.X, op=ALU.add))
                P.op("dve", lambda e, k=k, wk=wk: e.tensor_tensor(out=gat3[:, k, sl], in0=wk, in1=vld[:, k, sl], op=ALU.mult),
                     reads=[br], writes=[brt], c=0.3)
            P.op("dve", lambda e: e.tensor_copy(out=idx3[:, :, sl], in_=idf[:, :, sl]), reads=[br], writes=[brt], c=0.3)
            bex = bexg[gi]
            P.op("dve", lambda e: e.tensor_copy(out=ybA[:, sl, 512:513], in_=tokp1[:, sl].unsqueeze(2)), reads=[btk], writes=[bex], c=0.3)
            P.op("dve", lambda e: e.tensor_copy(out=ybA[:, sl, 513:514], in_=gat3[:, 0, sl].unsqueeze(2)), reads=[bex, brt], writes=[bex], c=0.3)
            P.op("dve", lambda e: e.tensor_copy(out=ybA[:, sl, 514:515], in_=gat3[:, 1, sl].unsqueeze(2)), reads=[bex, brt], writes=[bex], c=0.3)
            P.op("dve", lambda e: e.tensor_copy(out=ybA[:, sl, 515:516], in_=idf[:, 1, sl].unsqueeze(2)), reads=[bex, br], writes=[bex], c=0.3)
            for t in range(t0, t1):
                for k in range(2):
                    P.dma("pool", lambda e, t=t, k=k: e.indirect_dma_start(
                        out=XS, out_offset=bass.IndirectOffsetOnAxis(ap=idx[:, k * NT + t:k * NT + t + 1], axis=0),
                        in_=ybA[:, t, :], in_offset=None, bounds_check=bc_reg(e), oob_is_err=False),
                        reads=[bybA[t], brt, bXSz, bex], writes=[bXS], c=5.0)

        for gi in range(NG):
            for t in range(gi * GN, (gi + 1) * GN):
                tile_step(t)
            route(gi)
        P.barrier()
        if CUT[0] == 101:
            return

        A.release(m_phase)
        w1b[1] = v3(A.bf16(8 * 512), 8, 512)
        w3b[1] = v3(A.bf16(8 * 512), 8, 512)
        w2b[1] = v3(A.bf16(4 * D), 4, D)
        NXS = 8
        xs = [A.f32(516) for _ in range(NXS)]
        bxs = P.bufs(NXS)
        xsT = [v3(A.bf16(8 * 128), 8, 128) for _ in range(2)]
        bxsT = P.bufs(2)
        s1 = [A.f32(512) for _ in range(2)]
        bs1 = P.bufs(2)
        hm = [A.bf16(512) for _ in range(2)]
        bhm = P.bufs(2)
        hmT = [v3(A.bf16(4 * 128), 4, 128) for _ in range(2)]
        bhmT = P.bufs(2)
        ys = [A.bf16(D) for _ in range(6)]
        bys = P.bufs(6)
        cslot = A.f32(NEXP * CAP_T)
        bcs = P.buf()
        P.dma("sp", lambda e: e.dma_start(out=cslot, in_=c_slot_d), writes=[bcs])
        BIG2 = 20000.0
        NST = NEXP * CAP_T
        ev = v3(A.f32(NST * 4), NST, 4)
        bev = P.buf()
        for q4 in range(4):
            n0, n1 = q4 * NST // 4, (q4 + 1) * NST // 4
            P.dma("sp", lambda e, n0=n0, n1=n1: e.dma_start(
                out=ev[:, n0:n1, :], in_=XS[n0 * 128:n1 * 128, 512:516].rearrange("(j p) c -> p j c", p=128)),
                reads=[bXS], writes=[bev], c=20.0)
        kf, dg, gate, dest, inv = [A.f32(NST) for _ in range(5)]
        di = A.i32(NST)
        brt_ = P.buf()

        def dv2(fn):
            P.op("dve", fn, reads=[bev, brt_, bcs], writes=[brt_], c=0.3)
        dv2(lambda e: e.tensor_tensor(out=kf, in0=ev[:, :, 3], in1=cslot, op=ALU.is_equal))
        dv2(lambda e: e.tensor_tensor(out=dg, in0=ev[:, :, 2], in1=ev[:, :, 1], op=ALU.subtract))
        dv2(lambda e: e.tensor_tensor(out=dg, in0=dg, in1=kf, op=ALU.mult))
        dv2(lambda e: e.tensor_tensor(out=gate, in0=dg, in1=ev[:, :, 1], op=ALU.add))
        dv2(lambda e: e.scalar_tensor_tensor(out=dest, in0=kf, scalar=float(TPC), in1=ev[:, :, 0], op0=ALU.mult, op1=ALU.add))
        dv2(lambda e: e.tensor_scalar(out=inv, in0=ev[:, :, 0], scalar1=0.0, scalar2=None, op0=ALU.is_equal))
        dv2(lambda e: e.scalar_tensor_tensor(out=dest, in0=inv, scalar=BIG2, in1=dest, op0=ALU.mult, op1=ALU.add))
        dv2(lambda e: e.tensor_scalar(out=dest, in0=dest, scalar1=-1.0, scalar2=None, op0=ALU.add))
        dv2(lambda e: e.tensor_copy(out=di, in_=dest))
        BK = M2_BANKS[M2_LAYOUT[0]]
        yb0 = BK['y']
        it = 0
        for ex in range(NEXP):
            wb_ = ex % 2
            if ex > 0:
                P.dma("pool", lambda e, ex=ex, w_=w1b[wb_]: e.dma_start(out=w_, in_=w1_d[li, ex].rearrange("(k p) n -> p k n", p=128)), writes=[bw[wb_][0]], c=22.0)
                P.dma("pool", lambda e, ex=ex, w_=w3b[wb_]: e.dma_start(out=w_, in_=w3_d[li, ex].rearrange("(k p) n -> p k n", p=128)), writes=[bw[wb_][1]], c=22.0)
                P.dma("pool", lambda e, ex=ex, w_=w2b[wb_]: e.dma_start(out=w_, in_=w2_d[li, ex].rearrange("(k p) n -> p k n", p=128)), writes=[bw[wb_][2]], c=22.0)
            for j in range(CAP_T):
                r0 = ex * CAP + j * 128
                x_, bx_ = xs[it % NXS], bxs[it % NXS]
                P.dma("sp", lambda e, x_=x_, r0=r0: e.dma_start(out=x_, in_=XS[r0:r0 + 128, :]), reads=[bXS], writes=[bx_])
                xb_ = x_[:, 0:512].bitcast(BF16)
                tbx = BK['xt'][it % len(BK['xt'])]
                pbf = bank_bf(tbx)
                for k in range(8):
                    P.op("pe", lambda e, k=k, pbf=pbf, xb_=xb_: e.transpose(out=pbf[:, k * 128:(k + 1) * 128], in_=xb_[:, k * 128:(k + 1) * 128], identity=identb),
                         reads=[bx_, bconst], writes=[pb[tbx]], c=0.08)
                xT_, bxT_ = xsT[it % 2], bxsT[it % 2]
                P.op("act", lambda e, xT_=xT_, pbf=pbf: e.copy(out=xT_, in_=v3(pbf, 8, 128)), reads=[pb[tbx]], writes=[bxT_], c=1.0)
                hb = BK['h'][it % len(BK['h'])]
                for (wi, wt, bk) in ((0, w1b[wb_], hb), (1, w3b[wb_], hb + 1)):
                    for k in range(8):
                        P.op("pe", lambda e, k=k, wt=wt, bk=bk, xT_=xT_: e.matmul(bank(bk), lhsT=xT_[:, k, :], rhs=wt[:, k, :], start=(k == 0), stop=(k == 7)),
                             reads=[bxT_, bw[wb_][wi]], writes=[pb[bk]])
                s1_, bs1_ = s1[it % 2], bs1[it % 2]
                P.op("act", lambda e, s1_=s1_, hb=hb: e.activation(out=s1_, in_=bank(hb), func=AF.Silu), reads=[pb[hb]], writes=[bs1_])
                hm_, bhm_ = hm[it % 2], bhm[it % 2]
                P.op("dve", lambda e, hm_=hm_, s1_=s1_, hb=hb: e.tensor_tensor(out=hm_, in0=s1_, in1=bank(hb + 1), op=ALU.mult), reads=[bs1_, pb[hb + 1]], writes=[bhm_])
                tbh = BK['ht'][it % len(BK['ht'])]
                pbf2 = bank_bf(tbh)
                for k in range(4):
                    P.op("pe", lambda e, k=k, pbf2=pbf2, hm_=hm_: e.transpose(out=pbf2[:, k * 128:(k + 1) * 128], in_=hm_[:, k * 128:(k + 1) * 128], identity=identb),
                         reads=[bhm_, bconst], writes=[pb[tbh]], c=0.08)
                hT_, bhT_ = hmT[it % 2], bhmT[it % 2]
                P.op("dve", lambda e, hT_=hT_, pbf2=pbf2: e.tensor_copy(out=hT_, in_=v3(pbf2[:, 0:512], 4, 128)), reads=[pb[tbh]], writes=[bhT_])
                for hf in range(2):
                    for k in range(4):
                        P.op("pe", lambda e, k=k, hf=hf, hT_=hT_, w2_=w2b[wb_]: e.matmul(bank(yb0 + hf), lhsT=hT_[:, k, :], rhs=w2_[:, k, hf * 512:(hf + 1) * 512],
                                                                      start=(k == 0), stop=(k == 3)),
                             reads=[bhT_, bw[wb_][2]], writes=[pb[yb0 + hf]])
                ys_, bys_ = ys[it % 6], bys[it % 6]
                P.op("act", lambda e, ys_=ys_, it=it: e.activation(out=ys_, in_=bank(yb0, 2), func=AF.Copy, scale=gate[:, it:it + 1]),
                     reads=[pb[yb0], pb[yb0 + 1], brt_], writes=[bys_], c=1.1)
                P.dma("pool", lambda e, ys_=ys_, it=it: e.indirect_dma_start(
                    out=YK, out_offset=bass.IndirectOffsetOnAxis(ap=di[:, it:it + 1], axis=0),
                    in_=ys_, in_offset=None, bounds_check=bc_reg(e, 2 * TPC - 1), oob_is_err=False),
                    reads=[bys_, brt_], writes=[bYK], c=5.0)
                it += 1
        P.barrier()
        if CUT[0] == 102:
            return

        A.release(m_phase)
        A.top_reset()
        if not final:
            pw = dict(win=v3(A.top_bf16(8 * D), 8, D), wou=v3(A.top_bf16(8 * D), 8, D), wgp=v4(A.top_bf16(4 * 2 * 256), 4, 2, 256),
                      bwin=P.buf(), bwou=P.buf(), bwgp=P.buf())
            PF["pool_w"] = pw
            P.dma("pool", lambda e: e.dma_start(out=pw["win"], in_=pwin_d.rearrange("(k p) n -> p k n", p=128)), writes=[pw["bwin"]], c=15.0)
            P.dma("pool", lambda e: e.dma_start(out=pw["wou"], in_=pwout_d.rearrange("(k p) n -> p k n", p=128)), writes=[pw["bwou"]], c=15.0)
            for g in range(4):
                P.dma("pool", lambda e, g=g: e.dma_start(out=pw["wgp"][:, g, :, :], in_=pwg_d[g].rearrange("(k p) n -> p k n", p=128)), writes=[pw["bwgp"]])
        y1 = [A.bf16(D) for _ in range(2)]
        y2 = [A.bf16(D) for _ in range(2)]
        by1, by2 = P.bufs(2), P.bufs(2)
        ht = [A.f32(D) for _ in range(2)]
        bht = P.bufs(2)
        acc = [A.f32(D) for _ in range(2)]
        bacc = P.bufs(2)
        hn = [A.f32(D) for _ in range(2)]
        bhn = P.bufs(2)
        gft = None
        if final:
            gft = A.f32(D)
            bgf = P.buf()
            P.dma("sp", lambda e: e.dma_start(out=gft, in_=nfin_d[0:1, :].partition_broadcast(128)), writes=[bgf])
            junk = A.f32(D)
            bjunk = P.buf()
            ss, rs = A.f32(2), A.f32(2)
            bss = P.buf()
            fo = [A.f32(D) for _ in range(2)]
            bfo = P.bufs(2)
        for t in range(NT):
            i = t % 2
            P.dma("sp", lambda e, t=t, i=i: e.dma_start(out=y1[i], in_=YK[t * 128:(t + 1) * 128, :]), reads=[bYK], writes=[by1[i]])
            P.dma("sp", lambda e, t=t, i=i: e.dma_start(out=y2[i], in_=YK[TPC + t * 128:TPC + (t + 1) * 128, :]), reads=[bYK], writes=[by2[i]])
            P.dma("sp", lambda e, t=t, i=i: e.dma_start(out=ht[i], in_=Hin[t * 128:(t + 1) * 128, :]), reads=[bHin[t]], writes=[bht[i]])
            P.op("dve", lambda e, i=i: e.tensor_tensor(out=acc[i], in0=ht[i], in1=y1[i], op=ALU.add),
                 reads=[by1[i], bht[i]], writes=[bacc[i]], c=1.1)
            P.op("dve", lambda e, i=i: e.tensor_tensor(out=hn[i], in0=acc[i], in1=y2[i], op=ALU.add),
                 reads=[by2[i], bacc[i]], writes=[bhn[i]], c=1.1)
            if final:
                rmsnorm_tile(hn[i], bhn[i], gft, bgf, [(fo[i], bfo[i])], junk, bjunk, ss, rs, bss)
                P.dma("sp", lambda e, t=t, i=i: e.dma_start(out=out_d[t * 128:(t + 1) * 128, :], in_=fo[i]), reads=[bfo[i]], writes=[bOUT])
            else:
                P.dma("sp", lambda e, t=t, i=i: e.dma_start(out=Hout[t * 128:(t + 1) * 128, :], in_=hn[i]), reads=[bhn[i]], writes=[bH[id(Hout)][t]])
        P.barrier()

    def pool_phase():
        A.release(persist_mark)
        gbt = A.f32(D)
        bg = P.buf()
        P.dma("sp", lambda e: e.dma_start(out=gbt, in_=nmix_d[1:2, :].partition_broadcast(128)), writes=[bg])
        yT = v3(A.bf16(8 * SEQ), 8, SEQ)
        byT = P.bufs(16)
        zT = v3(A.bf16(8 * SEQ), 8, SEQ)
        bzT = P.buf()
        plT = v3(A.bf16(4 * SEQ), 4, SEQ)
        bplT = P.bufs(4)
        pw = PF["pool_w"]
        win, wou, wgp = pw["win"], pw["wou"], pw["wgp"]
        bwin, bwou, bwgp = pw["bwin"], pw["bwou"], pw["bwgp"]
        psc = A.f32(8)
        rc = A.f32(16)
        bpc = P.buf()
        P.dma("sp", lambda e: e.dma_start(out=psc, in_=pscT_d), writes=[bpc])
        P.dma("sp", lambda e: e.dma_start(out=rc, in_=c_rc_d), writes=[bpc])
        tl = dict(xt=[A.f32(D), A.f32(D)], bxt=P.bufs(2), yb=[A.bf16(D), A.bf16(D)], byb=P.bufs(2),
                  junk=A.f32(D), bjunk=P.buf(), ss=A.f32(2), rs=A.f32(2), bss=P.buf())
        uT = [A.f32(SEQ) for _ in range(2)]
        buT = P.bufs(2)
        sA = [A.f32(SEQ) for _ in range(2)]
        bsA = P.bufs(2)
        res = [tl["junk"], A.f32(D)]
        bres = [tl["bjunk"], P.buf()]
        h2_tiles = bH[id(H2)]
        xs_zero_begin()
        for s in range(2):
            seq_norm_transpose(H2, h2_tiles, s, gbt, bg, yT, byT, tl)
            for c in range(8):
                g = c // 2
                w = 2 << g
                u_, bu_ = uT[c % 2], buT[c % 2]
                xs_zero_some(5, after=[bu_])
                for q in range(4):
                    bk = (c * 4 + q) % 4
                    for k in range(8):
                        P.op("pe", lambda e, k=k, c=c, q=q, bk=bk: e.matmul(bank(bk), lhsT=win[:, k, c * 128:(c + 1) * 128], rhs=yT[:, k, q * 512:(q + 1) * 512],
                                                                         start=(k == 0), stop=(k == 7)), reads=[bwin] + byT[4 * q:4 * q + 4], writes=[pb[bk]])
                    P.op("act", lambda e, u_=u_, q=q, bk=bk: e.copy(out=u_[:, q * 512:(q + 1) * 512], in_=bank(bk)), reads=[pb[bk]], writes=[bu_])
                cur, bcur = u_, bu_
                sft = 1
                pp = 0
                while sft < w:
                    nx, bnx = sA[pp], bsA[pp]
                    P.op("dve", lambda e, nx=nx, cur=cur, sft=sft: e.tensor_copy(out=nx[:, 0:sft], in_=cur[:, 0:sft]), reads=[bcur], writes=[bnx])
                    P.op("dve", lambda e, nx=nx, cur=cur, sft=sft: e.tensor_tensor(out=nx[:, sft:SEQ], in0=cur[:, sft:SEQ], in1=cur[:, 0:SEQ - sft], op=ALU.add),
                         reads=[bcur], writes=[bnx])
                    cur, bcur = nx, bnx
                    pp = 1 - pp
                    sft *= 2
                P.op("dve", lambda e, cur=cur, u_=u_, c=c, w=w: e.scalar_tensor_tensor(out=plT[:, c % 4, :], in0=cur, scalar=1.0 / w, in1=u_, op0=ALU.mult, op1=ALU.subtract),
                     reads=[bcur, bu_], writes=[bplT[c % 4]])
                tmpc = sA[pp]
                btmpc = bsA[pp]
                P.op("dve", lambda e, cur=cur, tmpc=tmpc, w=w: e.tensor_tensor(out=tmpc[:, 0:w - 1], in0=cur[:, 0:w - 1], in1=rc[:, 0:w - 1], op=ALU.mult),
                     reads=[bcur, bpc], writes=[btmpc])
                P.op("dve", lambda e, tmpc=tmpc, u_=u_, c=c, w=w: e.tensor_tensor(out=plT[:, c % 4, 0:w - 1], in0=tmpc[:, 0:w - 1], in1=u_[:, 0:w - 1], op=ALU.subtract),
                     reads=[btmpc, bu_], writes=[bplT[c % 4]])
                if c % 2 == 1:
                    for eo in range(2):
                        co = 2 * g + eo
                        for q in range(4):
                            bk = 4 + (co * 4 + q) % 4
                            for ci in range(2):
                                P.op("pe", lambda e, g=g, eo=eo, ci=ci, q=q, bk=bk: e.matmul(
                                    bank(bk), lhsT=wgp[:, g, ci, eo * 128:(eo + 1) * 128], rhs=plT[:, (2 * g + ci) % 4, q * 512:(q + 1) * 512],
                                    start=(ci == 0), stop=(ci == 1)), reads=[bwgp, bplT[(2 * g + ci) % 4]], writes=[pb[bk]])
                            P.op("act", lambda e, co=co, q=q, bk=bk: e.activation(out=zT[:, co, q * 512:(q + 1) * 512], in_=bank(bk), func=AF.Copy, scale=psc[:, co:co + 1]),
                                 reads=[pb[bk], bpc], writes=[bzT])
            for t in range(16):
                gt = s * 16 + t
                b0 = 0 if t % 2 == 0 else 2
                for hf in range(2):
                    for k in range(8):
                        P.op("pe", lambda e, hf=hf, k=k, b0=b0, t=t: e.matmul(bank(b0 + hf), lhsT=zT[:, k, t * 128:(t + 1) * 128], rhs=wou[:, k, hf * 512:(hf + 1) * 512],
                                                                          start=(k == 0), stop=(k == 7)), reads=[bzT, bwou], writes=[pb[b0 + hf]])
                xt, bx = tl["xt"][t % 2], tl["bxt"][t % 2]
                P.dma("sp", lambda e, xt=xt, gt=gt: e.dma_start(out=xt, in_=H2[gt * 128:(gt + 1) * 128, :]), reads=[h2_tiles[gt]], writes=[bx])
                rs_, brs_ = res[t % 2], bres[t % 2]
                P.op("dve", lambda e, rs_=rs_, xt=xt, b0=b0: e.tensor_tensor(out=rs_, in0=xt, in1=bank(b0, 2), op=ALU.add),
                     reads=[bx, pb[b0], pb[b0 + 1]], writes=[brs_])
                P.dma("sp", lambda e, rs_=rs_, gt=gt: e.dma_start(out=H3[gt * 128:(gt + 1) * 128, :], in_=rs_), reads=[brs_], writes=[bH[id(H3)][gt]])
        P.barrier()

    phases = [("attn", attn_phase), ("moe0", lambda: moe_phase(0, H1, H2, False)),
              ("pool", pool_phase), ("moe1", lambda: moe_phase(1, H3, None, True))]
    for name, fn in phases:
        fn()
        if stop_after == name:
            break
    stats = P.finalize()
    P.es.close()
    return nc, stats


def make_constants():
    k = np.arange(128)[:, None]
    q = np.arange(128)[None, :]
    m = np.concatenate([(k <= q), (k >= q)], axis=1).astype(np.float32)
    mask = np.concatenate([m, m], axis=1)
    inv_freq = (500000.0 ** (-(np.arange(8, dtype=np.float32) * 2.0 / 16.0))).astype(np.float32)
    return dict(
        c_identf=np.eye(128, dtype=np.float32),
        c_mask=mask,
        c_triu=(k < q).astype(np.float32),
        c_ones=np.ones((128, 128), np.float32),
        c_invf=np.broadcast_to(inv_freq[None, :], (128, 8)).copy(),
        c_eoff=np.broadcast_to((np.arange(16, dtype=np.float32) * CAP)[None, :], (128, 16)).copy(),
        c_rc=np.broadcast_to((1.0 / np.arange(1, 17, dtype=np.float32))[None, :], (128, 16)).copy(),
        c_tokp1=(np.arange(NT, dtype=np.float32)[None, :] * 128 + np.arange(128, dtype=np.float32)[:, None] + 1.0).astype(np.float32),
        c_slot=(np.arange(NEXP * CAP_T, dtype=np.float32)[None, :] * 128 + np.arange(128, dtype=np.float32)[:, None]).astype(np.float32),
    )


def make_in_maps(inputs, ncores=NCORES):
    f = lambda a: np.ascontiguousarray(np.asarray(a, dtype=np.float32))
    x = f(inputs["x"])
    pos = np.asarray(inputs["positions"]).astype(np.int32)
    shared = dict(
        norm_mix=f(inputs["norm_mix"]), norm_ffn=f(inputs["norm_ffn"]),
        norm_final=f(inputs["norm_final"]).reshape(1, D),
        attn_w_in=f(inputs["attn_w_in"])[0], attn_w_out=f(inputs["attn_w_out"])[0],
        pool_w_in=f(inputs["pool_w_in"])[0], pool_w_group=f(inputs["pool_w_group"])[0],
        pool_scaleT=np.ascontiguousarray(f(inputs["pool_scale"])[0].reshape(8, 128).T),
        pool_w_out=f(inputs["pool_w_out"])[0],
        router_group_w=f(inputs["router_group_w"]), router_group_b=f(inputs["router_group_b"]),
        router_expert_w=f(inputs["router_expert_w"]),
        router_expert_b=f(inputs["router_expert_b"]).reshape(2, 16),
        expert_w1=f(inputs["expert_w1"]), expert_w3=f(inputs["expert_w3"]), expert_w2=f(inputs["expert_w2"]),
    )
    shared.update(make_constants())
    maps = []
    for c in range(ncores):
        xs = x[2 * c:2 * c + 2].reshape(TPC, D)
        p = pos[2 * c:2 * c + 2].reshape(NT, 128).T
        m = dict(shared)
        m["x"] = np.ascontiguousarray(xs)
        m["posT"] = np.ascontiguousarray(p)
        maps.append(m)
    return maps


_CACHE = {}


def kernel(**inputs):
    if "nc" not in _CACHE:
        _CACHE["nc"] = build_program()[0]
    nc = _CACHE["nc"]
    maps = make_in_maps(inputs)
    res = run_bass_kernel_spmd(nc, maps, core_ids=list(range(NCORES)))
    outs = [np.asarray(r["out"]).reshape(2, SEQ, D) for r in res.results]
    return np.concatenate(outs, axis=0).astype(np.float32)
```

```python
from contextlib import ExitStack
import math
import numpy as np
import ml_dtypes
import concourse.bass as bass
import concourse.mybir as mybir
from concourse.bass_utils import run_bass_kernel_spmd

F32 = mybir.dt.float32
BF16 = mybir.dt.bfloat16
I32 = mybir.dt.int32
ALU = mybir.AluOpType
AF = mybir.ActivationFunctionType
AX = mybir.AxisListType

NCORES = 8
SEQ = 2048
D = 1024
TPC = 4096
NT = 32
CAP_T = 5
CAP = CAP_T * 128
NEXP = 16
EPS = 1e-6
DILS = (1, 4, 16)
CUT = [0]
ZERO_FROM_TILE = 3
ATT_CFG = [3, 3]
M2_LAYOUT = [0]
M2_BANKS = [dict(h=[0], y=2, xt=[4, 5], ht=[6, 7]), dict(h=[0, 2], y=4, xt=[6], ht=[7])]

ENGS = ("pe", "act", "dve", "pool", "sp")
EPOCH = 12000
RING = {"sp": 40, "pool": 24, "act": 8}
DEF_COST = {"pe": 0.23, "act": 0.6, "dve": 0.8, "pool": 1.0, "sp": 0.1}
DEF_COST_DMA = 4.0
DMA_ISSUE = 0.3


class Buf:
    __slots__ = ("name", "w", "rs", "rd", "excl")

    def __init__(self, name):
        self.name = name
        self.excl = False
        self.w = []
        self.rs = []
        self.rd = []


class Op:
    __slots__ = ("eng", "fn", "deps", "needs_inc", "is_dma", "sem", "semval", "pre", "lidx", "seg", "cost", "fin")

    def __init__(self, eng, fn, is_dma):
        self.eng = eng
        self.fn = fn
        self.is_dma = is_dma
        self.lidx = 0
        self.seg = 0
        self.cost = 0.3
        self.fin = 0.0
        self.deps = []
        self.needs_inc = is_dma
        self.sem = None
        self.semval = None
        self.pre = None


class Prog:
    def __init__(self, nc):
        self.nc = nc
        self.es = ExitStack()
        self.streams = {e: [] for e in ENGS}
        self.ring = {}
        self.ring_n = {e: 0 for e in RING}
        for e, k in RING.items():
            self.ring[e] = [self._sem(f"dq_{e}_{i}") for i in range(k)]
        self.eng_sems = {e: [] for e in ENGS}
        self.nbuf = 0
        self.live_dma = []
        self.nops = 0
        self.seg = 0

    def _sem(self, name):
        return self.es.enter_context(self.nc.semaphore(name))

    def buf(self, name=None):
        self.nbuf += 1
        return Buf(name or f"b{self.nbuf}")

    def bufs(self, n):
        return [self.buf() for _ in range(n)]

    def _record(self, eng, fn, reads, writes, is_dma, c=None):
        o = Op(eng, fn, is_dma)
        self.nops += 1
        o.lidx = self.nops
        o.seg = self.seg
        o.cost = c if c is not None else (DEF_COST_DMA if is_dma else DEF_COST[eng])
        deps = {}
        ex = [b for b in reads if b.excl]
        if ex:
            reads = [b for b in reads if not b.excl]
            writes = list(writes) + [b for b in ex if b not in writes]
        for b in reads:
            for w_ in b.w:
                deps[id(w_)] = w_
        acc = []
        for b in writes:
            if is_dma and b.w and all(w_.is_dma for w_ in b.w) and not b.rs and not b.rd:
                acc.append(b)
                continue
            for w_ in b.w:
                deps[id(w_)] = w_
            for r in b.rs:
                deps[id(r)] = r
            for r in b.rd:
                deps[id(r)] = r
        o.deps = list(deps.values())
        for d in o.deps:
            d.needs_inc = True
        for b in reads:
            if is_dma:
                b.rd.append(o)
            else:
                b.rs.append(o)
        for b in writes:
            if b in acc:
                b.w.append(o)
            else:
                b.w = [o]
                b.rs = []
                b.rd = []
        if is_dma:
            self.live_dma.append(o)
        self.streams[eng].append(o)
        return o

    def op(self, eng, fn, reads=(), writes=(), c=None):
        return self._record(eng, fn, reads, writes, False, c)

    def dma(self, eng, fn, reads=(), writes=(), c=None):
        return self._record(eng, fn, reads, writes, True, c)

    def barrier(self):
        deps = list(self.live_dma)
        self.live_dma = []
        for d in deps:
            d.needs_inc = True
        self.seg += 1
        for e in ENGS:
            o = Op(e, lambda eng: eng.nop(), False)
            o.deps = list(deps)
            self.nops += 1
            o.lidx = self.nops
            o.seg = self.seg
            o.cost = 0.05
            self.streams[e].append(o)
        self.seg += 1

    def schedule(self):
        WINDOW = 48
        segs = {}
        for e in ENGS:
            for o in self.streams[e]:
                segs.setdefault(o.seg, {}).setdefault(e, []).append(o)
        final = {e: [] for e in ENGS}
        tnow = 0.0
        done = set()
        for sg in sorted(segs):
            per = segs[sg]
            if sg % 2 == 1:
                tails = [final[e2][-1 - i] for e2 in ENGS for i in range(min(len(final[e2]), 1))]
                tails = []
                for e2 in ENGS:
                    for o2 in reversed(final[e2]):
                        if not o2.is_dma:
                            tails.append(o2)
                            break
                for e in ENGS:
                    for o in per.get(e, []):
                        o.deps = list(o.deps) + tails
                        for d in tails:
                            d.needs_inc = True
                        o.fin = tnow
                        done.add(id(o))
                        final[e].append(o)
                continue
            et = {e: tnow for e in ENGS}
            pend = {e: list(per.get(e, [])) for e in ENGS}
            nleft = sum(len(v) for v in pend.values())
            while nleft:
                best = None
                for e in ENGS:
                    lst = pend[e]
                    for i in range(min(len(lst), WINDOW)):
                        o = lst[i]
                        st = et[e]
                        ok = True
                        for d in o.deps:
                            if id(d) not in done:
                                ok = False
                                break
                            if d.fin > st:
                                st = d.fin
                        if not ok:
                            continue
                        key = (st, o.lidx)
                        if best is None or key < best[0]:
                            best = (key, e, i, o, st)
                        if st <= et[e]:
                            break
                assert best is not None, "scheduler deadlock"
                _, e, i, o, st = best
                pend[e].pop(i)
                nleft -= 1
                if o.is_dma:
                    o.fin = st + o.cost
                    et[e] = st + DMA_ISSUE
                else:
                    o.fin = st + o.cost
                    et[e] = o.fin
                done.add(id(o))
                final[e].append(o)
            tnow = max([tnow] + [o.fin for e in ENGS for o in per.get(e, [])])
        self.streams = final
        self.est_us = tnow

    def finalize(self):
        nc = self.nc
        self.schedule()
        for e in RING:
            k = len(self.ring[e])
            i = 0
            for o in self.streams[e]:
                if not o.is_dma:
                    continue
                o.sem = self.ring[e][i % k]
                o.semval = 16 * (i // k + 1)
                if i >= k:
                    o.pre = (o.sem, 16 * (i // k))
                i += 1
        for e in ENGS:
            cnt = 0
            for o in self.streams[e]:
                if o.is_dma or not o.needs_inc:
                    continue
                ep = cnt // EPOCH
                while len(self.eng_sems[e]) <= ep:
                    self.eng_sems[e].append(self._sem(f"cs_{e}_{len(self.eng_sems[e])}"))
                o.sem = self.eng_sems[e][ep]
                o.semval = cnt % EPOCH + 1
                cnt += 1
        handles = {"pe": "tensor", "act": "scalar", "dve": "vector", "pool": "gpsimd", "sp": "sync"}
        stats = {}
        with nc.Block() as block:
            for e in ENGS:
                ops = self.streams[e]
                if not ops:
                    continue

                def body(eng, ops=ops, e=e):
                    waited = {}
                    nw = 0
                    for o in ops:
                        ws = []
                        if o.pre is not None:
                            ws.append(o.pre)
                        for d in o.deps:
                            if d.eng == e and e == "pe" and not d.is_dma:
                                continue
                            ws.append((d.sem, d.semval))
                        for (s, v) in ws:
                            key = id(s)
                            if waited.get(key, 0) >= v:
                                continue
                            waited[key] = v
                            eng.wait_ge(s, v)
                            nw += 1
                        ins = o.fn(eng)
                        if o.needs_inc:
                            ins.then_inc(o.sem, 16 if o.is_dma else 1)
                    for o in ops:
                        if o.is_dma:
                            key = id(o.sem)
                            if waited.get(key, 0) < o.semval:
                                waited[key] = o.semval
                                eng.wait_ge(o.sem, o.semval)
                    stats[e] = (len(ops), nw)

                getattr(block, handles[e])(body)
        self.stats = stats
        return stats


class Arena:
    def __init__(self, ap, n):
        self.ap = ap
        self.n = n
        self.off = 0
        self.top = n

    def top_reset(self):
        self.top = self.n

    def top_bf16(self, n):
        w = (n + 3) // 4 * 2
        self.top -= w
        assert self.off <= self.top, ("arena overflow (top)", self.off, self.top)
        return self.ap[:, self.top:self.top + w].bitcast(BF16)[:, 0:n]

    def mark(self):
        return self.off

    def release(self, m):
        self.off = m

    def f32(self, n):
        n2 = (n + 1) // 2 * 2
        assert self.off + n2 <= self.top, ("arena overflow", self.off, n2, self.top)
        a = self.ap[:, self.off:self.off + n]
        self.off += n2
        return a

    def bf16(self, n):
        w = (n + 3) // 4 * 2
        assert self.off + w <= self.top, ("arena overflow", self.off, w, self.top)
        a = self.ap[:, self.off:self.off + w].bitcast(BF16)[:, 0:n]
        self.off += w
        return a

    def i32(self, n):
        return self.f32(n).bitcast(I32)


def v3(ap, a, b):
    return ap.rearrange("p (a b) -> p a b", a=a, b=b)


def v4(ap, a, b, c):
    return ap.rearrange("p (a b c) -> p a b c", a=a, b=b, c=c)


def build_program(dbg=False, stop_after=None):
    nc = bass.Bass("TRN2", target_bir_lowering=False)
    P = Prog(nc)

    def din(name, shape, dt=F32):
        return nc.dram_tensor(name, list(shape), dt, kind="ExternalInput").ap()

    def dscr(name, shape, dt, out=False):
        if out:
            return nc.dram_tensor(name, list(shape), dt, kind="ExternalOutput").ap()
        return nc.dram_tensor(name, list(shape), dt).ap()

    x_d = din("x", [TPC, D])
    posT_d = din("posT", [128, NT], I32)
    nmix_d = din("norm_mix", [2, D])
    nffn_d = din("norm_ffn", [2, D])
    nfin_d = din("norm_final", [1, D])
    awin_d = din("attn_w_in", [D, 4608])
    awout_d = din("attn_w_out", [512, D])
    pwin_d = din("pool_w_in", [D, D])
    pwg_d = din("pool_w_group", [4, 256, 256])
    pscT_d = din("pool_scaleT", [128, 8])
    pwout_d = din("pool_w_out", [D, D])
    rgw_d = din("router_group_w", [2, D, 4])
    rgb_d = din("router_group_b", [2, 4])
    rew_d = din("router_expert_w", [2, 4, D, 4])
    reb_d = din("router_expert_b", [2, 16])
    w1_d = din("expert_w1", [2, NEXP, D, 512])
    w3_d = din("expert_w3", [2, NEXP, D, 512])
    w2_d = din("expert_w2", [2, NEXP, 512, D])
    c_identf_d = din("c_identf", [128, 128])
    c_mask_d = din("c_mask", [128, 512])
    c_triu_d = din("c_triu", [128, 128])
    c_ones_d = din("c_ones", [128, 128])
    c_invf_d = din("c_invf", [128, 8])
    c_eoff_d = din("c_eoff", [128, 16])
    c_rc_d = din("c_rc", [128, 16])
    c_tokp1_d = din("c_tokp1", [128, NT])
    c_slot_d = din("c_slot", [128, NEXP * CAP_T])
    out_d = nc.dram_tensor("out", [TPC, D], F32, kind="ExternalOutput").ap()

    H1 = dscr("H1", [TPC, D], F32, out=dbg)
    H2 = dscr("H2", [TPC, D], F32, out=dbg)
    H3 = dscr("H3", [TPC, D], F32, out=dbg)
    OGZ = [dscr(f"OGZ{g}", [TPC, 264], F32) for g in range(3)]
    XS = dscr("XS", [NEXP * CAP, 516], F32)
    YK = dscr("YK", [2 * TPC, D], BF16)
    bH = {id(h): P.bufs(NT) for h in (H1, H2, H3)}
    bOGZ = [P.bufs(NT) for _ in range(3)]
    bXS = P.buf()
    bYK = P.buf()
    bYKz = P.buf()
    bOUT = P.buf()

    ARENA_N = 46000
    arena_t = P.es.enter_context(nc.sbuf_tensor("arena", [128, ARENA_N], F32))
    A = Arena(arena_t, ARENA_N)
    ps_t = P.es.enter_context(nc.psum_tensor("ps", [128, 4096], F32))

    def bank(i, n=1):
        return ps_t[:, i * 512:(i + n) * 512]

    def bank_bf(i):
        return ps_t[:, i * 512:(i + 1) * 512].bitcast(BF16)

    pb = P.bufs(8)
    PF = {}
    for b_ in pb:
        b_.excl = True
    _bc = {}

    def bc_reg(e, val=None):
        val = NEXP * CAP - 1 if val is None else val
        if val not in _bc:
            _bc[val] = e.to_reg(val)
        return _bc[val]

    identf = A.f32(128)
    identb = A.bf16(128)
    maskb = A.bf16(512)
    bconst = P.buf()
    P.dma("sp", lambda e: e.dma_start(out=identf, in_=c_identf_d), writes=[bconst])
    zt = A.f32(516)
    bzt = P.buf()
    bXSz = P.buf()
    tmpm = zt[:, 0:512]
    P.dma("sp", lambda e: e.dma_start(out=tmpm, in_=c_mask_d), writes=[bzt])
    P.op("dve", lambda e: e.tensor_copy(out=identb, in_=identf), reads=[bconst], writes=[bconst])
    P.op("dve", lambda e: e.tensor_copy(out=maskb, in_=tmpm), reads=[bconst, bzt], writes=[bconst])
    P.op("pool", lambda e: e.memset(zt, 0.0), writes=[bzt])
    XSv = XS.rearrange("(n p) d -> n p d", p=128)
    zero_state = {"jobs": []}

    def xs_zero_begin():
        zero_state["jobs"] = [ex_ * CAP_T + j_ for ex_ in range(NEXP) for j_ in range(CAP_T) if j_ >= ZERO_FROM_TILE]

    def xs_zero_some(n, after=()):
        for _ in range(n):
            if zero_state["jobs"]:
                n_ = zero_state["jobs"].pop(0)
                P.dma("sp", lambda e, n_=n_: e.dma_start(out=XSv[n_], in_=zt), reads=[bzt, bXS] + list(after), writes=[bXSz], c=4.0)
    persist_mark = A.mark()

    def rmsnorm_tile(xt, bx, gbt, bg, outs, junk, bjunk, ss, rs, bss):
        ss = ss[:, 0:1]
        rs = rs[:, 0:1]
        P.op("act", lambda e: e.activation(out=junk, in_=xt, func=AF.Square, accum_out=ss),
             reads=[bx], writes=[bjunk, bss])
        P.op("act", lambda e: e.activation(out=rs, in_=ss, func=AF.Sqrt, scale=1.0 / D, bias=EPS),
             reads=[bss], writes=[bss])
        P.op("dve", lambda e: e.reciprocal(out=rs, in_=rs), reads=[bss], writes=[bss])
        for (o_ap, o_b) in outs:
            P.op("dve", lambda e, o_ap=o_ap: e.scalar_tensor_tensor(
                out=o_ap, in0=xt, scalar=rs[:, 0:1], in1=gbt, op0=ALU.mult, op1=ALU.mult),
                reads=[bx, bss, bg], writes=[o_b])

    def seq_norm_transpose(src, bsrc_tiles, s, gbt, bg, yT, byT, tl):
        for t in range(16):
            gt = s * 16 + t
            xt, bx = tl["xt"][t % 2], tl["bxt"][t % 2]
            yb, byb = tl["yb"][t % 2], tl["byb"][t % 2]
            P.dma("sp", lambda e, xt=xt, gt=gt: e.dma_start(out=xt, in_=src[gt * 128:(gt + 1) * 128, :]),
                  reads=[bsrc_tiles[gt]], writes=[bx])
            rmsnorm_tile(xt, bx, gbt, bg, [(yb, byb)], tl["junk"], tl["bjunk"], tl["ss"], tl["rs"], tl["bss"])
            bk = 4 + (t % 2)
            pbf = bank_bf(bk)
            for k in range(8):
                P.op("pe", lambda e, k=k, pbf=pbf, yb=yb: e.transpose(
                    out=pbf[:, k * 128:(k + 1) * 128], in_=yb[:, k * 128:(k + 1) * 128], identity=identb),
                    reads=[byb, bconst], writes=[pb[bk]])
            eng = "act" if t % 2 == 0 else "dve"
            if eng == "act":
                P.op("act", lambda e, pbf=pbf, t=t: e.copy(out=yT[:, :, t * 128:(t + 1) * 128], in_=v3(pbf, 8, 128)),
                     reads=[pb[bk]], writes=[byT[t]])
            else:
                P.op("dve", lambda e, pbf=pbf, t=t: e.tensor_copy(out=yT[:, :, t * 128:(t + 1) * 128], in_=v3(pbf, 8, 128)),
                     reads=[pb[bk]], writes=[byT[t]])

    def attn_phase():
        A.release(persist_mark)
        gbt = A.f32(D)
        bg = P.buf()
        P.dma("sp", lambda e: e.dma_start(out=gbt, in_=nmix_d[0:1, :].partition_broadcast(128)), writes=[bg])
        yT = v3(A.bf16(8 * SEQ), 8, SEQ)
        byT = P.bufs(16)
        wg = v3(A.bf16(8 * 1536), 8, 1536)
        bwg = P.buf()
        qT = v3(A.bf16(4 * SEQ), 4, SEQ)
        kT = v3(A.bf16(4 * SEQ), 4, SEQ)
        bqT, bkT = P.buf(), P.buf()
        Va = v4(A.bf16(16 * 520), 16, 8, 65)
        bVa = P.bufs(16)
        bVa1 = P.buf()
        wout = v3(A.bf16(4 * D), 4, D)
        bwout = P.buf()
        tl = dict(xt=[A.f32(D), A.f32(D)], bxt=P.bufs(2), yb=[A.bf16(D), A.bf16(D)], byb=P.bufs(2),
                  junk=A.f32(D), bjunk=P.buf(), ss=A.f32(2), rs=A.f32(2), bss=P.buf())
        qk = [A.bf16(D), A.bf16(D)]
        bqk = P.bufs(2)
        NE = ATT_CFG[0]
        Eb = [v4(A.bf16(4 * 512), 4, 2, 256) for _ in range(NE)]
        bE = [P.bufs(4) for _ in range(NE)]
        OZ = [A.f32(264) for _ in range(3)]
        bOZ = P.bufs(3)
        posi = A.i32(NT)
        posf = A.f32(NT)
        invf = A.f32(8)
        ang = A.f32(NT * 8)
        a2 = A.f32(NT * 8)
        nf = A.f32(NT * 8)
        ni = A.i32(NT * 8)
        mk = A.f32(NT * 8)
        cosT = v3(A.f32(NT * 8), NT, 8)
        sinT = v3(A.f32(NT * 8), NT, 8)
        brot = P.buf()
        rt = [A.f32(128) for _ in range(4)]
        brt = P.bufs(4)
        ozl = [[A.f32(264) for _ in range(3)] for _ in range(2)]
        bozl = [P.bufs(3) for _ in range(2)]
        zs = A.f32(8)
        us = A.f32(512)
        bmg = P.buf()
        ob = A.bf16(512)
        bob = P.buf()
        oT = v3(A.bf16(4 * 128), 4, 128)
        boT = P.buf()
        res = [tl["junk"], A.f32(D)]
        bres = [tl["bjunk"], P.buf()]

        P.dma("sp", lambda e: e.dma_start(out=posi, in_=posT_d), writes=[brot])
        P.dma("sp", lambda e: e.dma_start(out=invf, in_=c_invf_d), writes=[brot])
        P.op("dve", lambda e: e.tensor_copy(out=posf, in_=posi), reads=[brot], writes=[brot])
        P.op("dve", lambda e: e.tensor_tensor(
            out=v3(ang, NT, 8), in0=posf.unsqueeze(2).to_broadcast([128, NT, 8]),
            in1=invf.unsqueeze(1).to_broadcast([128, NT, 8]), op=ALU.mult), reads=[brot], writes=[brot])
        TWO_PI = 2.0 * math.pi
        C1 = 6.28125
        C2 = TWO_PI - C1
        for (tab, shift) in ((sinT, 0.0), (cosT, 0.5 * math.pi)):
            tabf = tab.rearrange("p a b -> p (a b)")
            P.op("dve", lambda e, shift=shift: e.tensor_scalar(out=a2, in0=ang, scalar1=shift, scalar2=None, op0=ALU.add),
                 reads=[brot], writes=[brot])
            P.op("dve", lambda e: e.tensor_scalar(out=ni, in0=a2, scalar1=1.0 / TWO_PI, scalar2=None, op0=ALU.mult),
                 reads=[brot], writes=[brot])
            P.op("dve", lambda e: e.tensor_copy(out=nf, in_=ni), reads=[brot], writes=[brot])
            P.op("dve", lambda e: e.scalar_tensor_tensor(out=a2, in0=nf, scalar=-C1, in1=a2, op0=ALU.mult, op1=ALU.add),
                 reads=[brot], writes=[brot])
            P.op("dve", lambda e: e.scalar_tensor_tensor(out=a2, in0=nf, scalar=-C2, in1=a2, op0=ALU.mult, op1=ALU.add),
                 reads=[brot], writes=[brot])
            P.op("dve", lambda e: e.tensor_scalar(out=mk, in0=a2, scalar1=math.pi, scalar2=None, op0=ALU.is_gt),
                 reads=[brot], writes=[brot])
            P.op("dve", lambda e: e.scalar_tensor_tensor(out=a2, in0=mk, scalar=-TWO_PI, in1=a2, op0=ALU.mult, op1=ALU.add),
                 reads=[brot], writes=[brot])
            P.op("dve", lambda e: e.tensor_scalar(out=mk, in0=a2, scalar1=-math.pi, scalar2=None, op0=ALU.is_lt),
                 reads=[brot], writes=[brot])
            P.op("dve", lambda e: e.scalar_tensor_tensor(out=a2, in0=mk, scalar=TWO_PI, in1=a2, op0=ALU.mult, op1=ALU.add),
                 reads=[brot], writes=[brot])
            P.op("dve", lambda e: e.tensor_scalar(out=a2, in0=a2, scalar1=math.pi, scalar2=-math.pi, op0=ALU.min, op1=ALU.max),
                 reads=[brot], writes=[brot])
            P.op("act", lambda e, tabf=tabf: e.activation(out=tabf, in_=a2, func=AF.Sin), reads=[brot], writes=[brot])

        if CUT[0] == 1:
            P.barrier()
            return
        P.op("pool", lambda e: e.memset(Va[:, :, :, 64:65], 1.0), writes=[bVa1])
        P.dma("pool", lambda e: e.dma_start(out=wout, in_=awout_d.rearrange("(k p) n -> p k n", p=128)), writes=[bwout])
        for i in range(3):
            P.op("pool", lambda e, i=i: e.memset(OZ[i], 0.0), writes=[bOZ[i]])

        x_tiles = [P.buf() for _ in range(NT)]
        xs_zero_begin()
        ei = 0
        ozi = 0
        for s in range(2):
            seq_norm_transpose(x_d, x_tiles, s, gbt, bg, yT, byT, tl)
            if CUT[0] == 2:
                P.barrier()
                return
            for g in range(3):
                d = DILS[g]
                nb = 16 // d
                P.dma("pool", lambda e, g=g: e.dma_start(
                    out=wg, in_=awin_d[:, g * 1536:(g + 1) * 1536].rearrange("(k p) n -> p k n", p=128)),
                    writes=[bwg])
                if CUT[0] == 31:
                    P.barrier()
                    return
                for t in range(16):
                    gt = s * 16 + t
                    if CUT[0] in (32, 33, 34) and t == 1:
                        P.barrier()
                        return
                    b0 = 0 if t % 2 == 0 else 2
                    for j in range(2):
                        for k in range(8):
                            P.op("pe", lambda e, j=j, k=k, b0=b0, t=t: e.matmul(
                                bank(b0 + j), lhsT=yT[:, k, t * 128:(t + 1) * 128], rhs=wg[:, k, j * 512:(j + 1) * 512],
                                start=(k == 0), stop=(k == 7)), reads=[byT[t], bwg], writes=[pb[b0 + j]])
                    qkt, bq = qk[t % 2], bqk[t % 2]
                    xs_zero_some(1, after=[bq])
                    psq = bank(b0, 2)
                    P.op("act", lambda e, qkt=qkt, psq=psq: e.copy(out=qkt, in_=psq),
                         reads=[pb[b0], pb[b0 + 1]], writes=[bq])
                    if CUT[0] == 32:
                        continue
                    psv = v3(psq, 16, 64)
                    qkv = v3(qkt, 16, 64)
                    cb = cosT[:, gt:gt + 1, :].to_broadcast([128, 16, 8])
                    sb = sinT[:, gt:gt + 1, :].to_broadcast([128, 16, 8])
                    t1 = psv[:, :, 0:8]
                    t2 = psv[:, :, 8:16]
                    r = [v3(x_, 16, 8) for x_ in rt]
                    rd = [pb[b0], pb[b0 + 1], brot]
                    P.op("dve", lambda e, t1=t1, cb=cb, r=r: e.tensor_tensor(out=r[0], in0=t1, in1=cb, op=ALU.mult), reads=rd, writes=[brt[0]])
                    P.op("dve", lambda e, t2=t2, sb=sb, r=r: e.tensor_tensor(out=r[1], in0=t2, in1=sb, op=ALU.mult), reads=rd, writes=[brt[1]])
                    P.op("dve", lambda e, t2=t2, cb=cb, r=r: e.tensor_tensor(out=r[2], in0=t2, in1=cb, op=ALU.mult), reads=rd, writes=[brt[2]])
                    P.op("dve", lambda e, t1=t1, sb=sb, r=r: e.tensor_tensor(out=r[3], in0=t1, in1=sb, op=ALU.mult), reads=rd, writes=[brt[3]])
                    P.op("dve", lambda e, qkv=qkv, r=r: e.tensor_tensor(out=qkv[:, :, 0:8], in0=r[0], in1=r[1], op=ALU.subtract),
                         reads=[brt[0], brt[1]], writes=[bq])
                    P.op("dve", lambda e, qkv=qkv, r=r: e.tensor_tensor(out=qkv[:, :, 8:16], in0=r[2], in1=r[3], op=ALU.add),
                         reads=[brt[2], brt[3]], writes=[bq])
                    if CUT[0] == 33:
                        continue
                    bk = 4 + (t % 2)
                    pbf = bank_bf(bk)
                    for c in range(8):
                        P.op("pe", lambda e, c=c, pbf=pbf, qkt=qkt: e.transpose(
                            out=pbf[:, c * 128:(c + 1) * 128], in_=qkt[:, c * 128:(c + 1) * 128], identity=identb),
                            reads=[bq, bconst], writes=[pb[bk]])
                    P.op("act", lambda e, pbf=pbf, t=t: e.copy(out=qT[:, :, t * 128:(t + 1) * 128], in_=v3(pbf[:, 0:512], 4, 128)),
                         reads=[pb[bk]], writes=[bqT])
                    P.op("dve", lambda e, pbf=pbf, t=t: e.tensor_copy(out=kT[:, :, t * 128:(t + 1) * 128], in_=v3(pbf[:, 512:1024], 4, 128)),
                         reads=[pb[bk]], writes=[bkT])
                if CUT[0] == 3:
                    P.barrier()
                    return
                for blk in range(16):
                    ph, b = blk // nb, blk % nb
                    st = b * 128 * d + ph
                    bk = 6 + (blk % 2)
                    for k in range(8):
                        P.op("pe", lambda e, k=k, st=st, d=d, bk=bk: e.matmul(
                            bank(bk), lhsT=yT[:, k, st:st + 127 * d + 1:d], rhs=wg[:, k, 1024:1536],
                            start=(k == 0), stop=(k == 7)), reads=byT[st // 128:(st + 127 * d) // 128 + 1] + [bwg], writes=[pb[bk]])
                    eng = "act" if blk % 2 == 0 else "dve"
                    if eng == "act":
                        P.op("act", lambda e, blk=blk, bk=bk: e.copy(out=Va[:, blk, :, 0:64], in_=v3(bank(bk), 8, 64)),
                             reads=[pb[bk]], writes=[bVa[blk]])
                    else:
                        P.op("dve", lambda e, blk=blk, bk=bk: e.tensor_copy(out=Va[:, blk, :, 0:64], in_=v3(bank(bk), 8, 64)),
                             reads=[pb[bk]], writes=[bVa[blk]])
                if CUT[0] == 4:
                    P.barrier()
                    return
                sbi = 0
                pvi = 0
                for ph in range(d):
                    prevE = None
                    for b in range(nb):
                        blk = ph * nb + b
                        nq = 256 if b + 1 < nb else 128
                        st = b * 128 * d + ph
                        Ec, bEc = Eb[ei % NE], bE[ei % NE]
                        ei += 1
                        for c in range(4):
                            sbk = 2 * (sbi % ATT_CFG[1])
                            sbi += 1
                            for hh in range(2):
                                P.op("pe", lambda e, c=c, hh=hh, st=st, d=d, nq=nq, sbk=sbk: e.matmul(
                                    bank(sbk + hh)[:, 0:nq],
                                    lhsT=kT[hh * 64:(hh + 1) * 64, c, st:st + 127 * d + 1:d],
                                    rhs=qT[hh * 64:(hh + 1) * 64, c, st:st + (nq - 1) * d + 1:d],
                                    start=True, stop=True), reads=[bqT, bkT], writes=[pb[sbk + hh]])
                            P.op("act", lambda e, Ec=Ec, c=c, nq=nq, sbk=sbk: e.activation(
                                out=Ec[:, c, :, 0:nq], in_=v3(bank(sbk, 2), 2, 512)[:, :, 0:nq], func=AF.Exp, scale=0.125),
                                reads=[pb[sbk], pb[sbk + 1]], writes=[bEc[c]])
                            P.op("dve", lambda e, Ec=Ec, c=c, nq=nq: e.tensor_tensor(
                                out=Ec[:, c, :, 0:nq], in0=Ec[:, c, :, 0:nq], in1=v3(maskb, 2, 256)[:, :, 0:nq], op=ALU.mult),
                                reads=[bEc[c], bconst], writes=[bEc[c]])
                        pvb = (4 + 2 * (pvi % 2)) if ATT_CFG[1] == 2 else 6
                        pvi += 1
                        for h in range(8):
                            c, hh = h // 2, h % 2
                            o_ap = bank(pvb + h // 4)[:, (h % 4) * 65:(h % 4) * 65 + 65]
                            if b > 0:
                                Ep, bEp = prevE
                                P.op("pe", lambda e, o_ap=o_ap, Ep=Ep, c=c, hh=hh, blk=blk, h=h: e.matmul(
                                    o_ap, lhsT=Ep[:, c, hh, 128:256], rhs=Va[:, blk - 1, h, :], start=True, stop=False),
                                    reads=[bEp[c], bVa[blk - 1], bVa1], writes=[pb[pvb + h // 4]])
                            P.op("pe", lambda e, o_ap=o_ap, Ec=Ec, c=c, hh=hh, blk=blk, h=h, b=b: e.matmul(
                                o_ap, lhsT=Ec[:, c, hh, 0:128], rhs=Va[:, blk, h, :], start=(b == 0), stop=True),
                                reads=[bEc[c], bVa[blk], bVa1], writes=[pb[pvb + h // 4]])
                        prevE = (Ec, bEc)
                        oz, boz = OZ[ozi % 3], bOZ[ozi % 3]
                        ozi += 1
                        pv = v3(bank(pvb, 2), 2, 512)[:, :, 0:260].rearrange("p a (h e) -> p a h e", h=4, e=65)
                        P.op("act", lambda e, oz=oz, pv=pv: e.copy(
                            out=v4(oz[:, 0:256].bitcast(BF16), 2, 4, 64), in_=pv[:, :, :, 0:64]),
                            reads=[pb[pvb], pb[pvb + 1]], writes=[boz])
                        P.op("dve", lambda e, oz=oz, pv=pv: e.tensor_copy(
                            out=v4(oz[:, 256:264], 2, 4, 1), in_=pv[:, :, :, 64:65]),
                            reads=[pb[pvb], pb[pvb + 1]], writes=[boz])
                        r0 = s * SEQ + st
                        touched = sorted({(r0 + d * i) // 128 for i in (0, 127)})
                        tb = [bOGZ[g][ti] for ti in range(touched[0], touched[-1] + 1)]
                        P.dma("sp", lambda e, oz=oz, g=g, r0=r0, d=d: e.dma_start(
                            out=OGZ[g][r0:r0 + 127 * d + 1:d, :], in_=oz), reads=[boz], writes=tb)
                if CUT[0] == 5:
                    P.barrier()
                    return
            if CUT[0] == 6:
                P.barrier()
                return
            for t in range(16):
                gt = s * 16 + t
                ol, bol = ozl[t % 2], bozl[t % 2]
                for g in range(3):
                    P.dma("sp", lambda e, g=g, ol=ol, gt=gt: e.dma_start(out=ol[g], in_=OGZ[g][gt * 128:(gt + 1) * 128, :]),
                          reads=[bOGZ[g][gt]], writes=[bol[g]])
                zv = [ol[g][:, 256:264] for g in range(3)]
                uv = [ol[g][:, 0:256].bitcast(BF16) for g in range(3)]
                P.op("dve", lambda e, zv=zv: e.tensor_tensor(out=zs, in0=zv[0], in1=zv[1], op=ALU.add), reads=[bol[0], bol[1]], writes=[bmg])
                P.op("dve", lambda e, zv=zv: e.tensor_tensor(out=zs, in0=zs, in1=zv[2], op=ALU.add), reads=[bol[2], bmg], writes=[bmg])
                P.op("dve", lambda e: e.reciprocal(out=zs, in_=zs), reads=[bmg], writes=[bmg])
                P.op("dve", lambda e, uv=uv: e.tensor_tensor(out=us, in0=uv[0], in1=uv[1], op=ALU.add), reads=[bol[0], bol[1], bmg], writes=[bmg])
                P.op("dve", lambda e, uv=uv: e.tensor_tensor(out=us, in0=us, in1=uv[2], op=ALU.add), reads=[bol[2], bmg], writes=[bmg])
                P.op("dve", lambda e: e.tensor_tensor(out=v3(ob, 8, 64), in0=v3(us, 8, 64),
                                                      in1=zs.unsqueeze(2).to_broadcast([128, 8, 64]), op=ALU.mult),
                     reads=[bmg], writes=[bob])
                bk = 4 + (t % 2)
                pbf = bank_bf(bk)
                for k in range(4):
                    P.op("pe", lambda e, k=k, pbf=pbf: e.transpose(out=pbf[:, k * 128:(k + 1) * 128], in_=ob[:, k * 128:(k + 1) * 128], identity=identb),
                         reads=[bob, bconst], writes=[pb[bk]])
                P.op("act", lambda e, pbf=pbf: e.copy(out=oT, in_=v3(pbf[:, 0:512], 4, 128)), reads=[pb[bk]], writes=[boT])
                b0 = 0 if t % 2 == 0 else 2
                for hf in range(2):
                    for k in range(4):
                        P.op("pe", lambda e, hf=hf, k=k, b0=b0: e.matmul(
                            bank(b0 + hf), lhsT=oT[:, k, :], rhs=wout[:, k, hf * 512:(hf + 1) * 512],
                            start=(k == 0), stop=(k == 3)), reads=[boT, bwout], writes=[pb[b0 + hf]])
                xt, bx = tl["xt"][t % 2], tl["bxt"][t % 2]
                P.dma("sp", lambda e, xt=xt, gt=gt: e.dma_start(out=xt, in_=x_d[gt * 128:(gt + 1) * 128, :]), writes=[bx])
                rs_, brs_ = res[t % 2], bres[t % 2]
                P.op("dve", lambda e, rs_=rs_, xt=xt, b0=b0: e.tensor_tensor(out=rs_, in0=xt, in1=bank(b0, 2), op=ALU.add),
                     reads=[bx, pb[b0], pb[b0 + 1]], writes=[brs_])
                P.dma("sp", lambda e, rs_=rs_, gt=gt: e.dma_start(out=H1[gt * 128:(gt + 1) * 128, :], in_=rs_),
                      reads=[brs_], writes=[bH[id(H1)][gt]])
        P.barrier()

    def moe_phase(li, Hin, Hout, final):
        A.release(persist_mark)
        bHin = bH[id(Hin)]
        gbt = A.f32(D)
        bg = P.buf()
        P.dma("sp", lambda e: e.dma_start(out=gbt, in_=nffn_d[li:li + 1, :].partition_broadcast(128)), writes=[bg])
        idx = A.i32(2 * NT)
        gat = A.f32(2 * NT)
        broute = P.buf()
        m_phase = A.mark()
        A.top_reset()
        w1b = [v3(A.top_bf16(8 * 512), 8, 512), None]
        w3b = [v3(A.top_bf16(8 * 512), 8, 512), None]
        w2b = [v3(A.top_bf16(4 * D), 4, D), None]
        bw = [P.bufs(3) for _ in range(2)]
        pre_w = {}

        def prefetch_expert0(after):
            if pre_w:
                return
            pre_w["done"] = True
            for (wt_, src_, i_) in ((w1b[0], w1_d, 0), (w3b[0], w3_d, 1), (w2b[0], w2_d, 2)):
                P.dma("pool", lambda e, wt_=wt_, src_=src_: e.dma_start(out=wt_, in_=src_[li, 0].rearrange("(k p) n -> p k n", p=128)),
                      reads=list(after), writes=[bw[0][i_]], c=22.0)

        xs_zero_some(1000)
        ybA = v3(A.f32(NT * 516), NT, 516)

        def yb_t(t):
            return ybA[:, t, 0:512].bitcast(BF16)
        bybA = P.bufs(NT)
        wr = v3(A.f32(8 * 20), 8, 20)
        bwr = P.buf()
        rb = A.f32(20)
        triu = A.f32(128)
        onesm = A.f32(128)
        eoff = A.f32(16)
        P.dma("sp", lambda e: e.dma_start(out=wr[:, :, 0:4], in_=rgw_d[li].rearrange("(k p) n -> p k n", p=128)), writes=[bwr])
        for g in range(4):
            P.dma("sp", lambda e, g=g: e.dma_start(out=wr[:, :, 4 + 4 * g:8 + 4 * g],
                                                    in_=rew_d[li, g].rearrange("(k p) n -> p k n", p=128)), writes=[bwr])
        P.dma("sp", lambda e: e.dma_start(out=rb[:, 0:4], in_=rgb_d[li:li + 1, :].partition_broadcast(128)), writes=[bwr])
        P.dma("sp", lambda e: e.dma_start(out=rb[:, 4:20], in_=reb_d[li:li + 1, :].partition_broadcast(128)), writes=[bwr])
        P.dma("sp", lambda e: e.dma_start(out=triu, in_=c_triu_d), writes=[bwr])
        P.dma("sp", lambda e: e.dma_start(out=onesm, in_=c_ones_d), writes=[bwr])
        P.dma("sp", lambda e: e.dma_start(out=eoff, in_=c_eoff_d), writes=[bwr])
        NB3 = 3
        xt2 = [A.f32(D) for _ in range(NB3)]
        bxt2 = P.bufs(NB3)
        yf = [A.f32(D) for _ in range(NB3)]
        byf = P.bufs(NB3)
        junk = A.f32(D)
        bjunk = P.buf()
        ss, rs = A.f32(2), A.f32(2)
        bss = P.buf()
        whl = v3(A.bf16(8 * 40), 8, 40)
        wtmp = v3(A.f32(8 * 20), 8, 20)
        bwhl = P.buf()
        P.op("dve", lambda e: e.tensor_copy(out=whl[:, :, 0:20], in_=wr), reads=[bwr], writes=[bwhl])
        P.op("dve", lambda e: e.tensor_tensor(out=wtmp, in0=wr, in1=whl[:, :, 0:20], op=ALU.subtract), reads=[bwr, bwhl], writes=[bwhl])
        P.op("dve", lambda e: e.tensor_copy(out=whl[:, :, 20:40], in_=wtmp), reads=[bwhl], writes=[bwhl])
        yl = [A.bf16(D) for _ in range(NB3)]
        byl = P.bufs(NB3)
        yhT = [v3(A.bf16(8 * 128), 8, 128) for _ in range(NB3)]
        ylT = [v3(A.bf16(8 * 128), 8, 128) for _ in range(NB3)]
        byhT, bylT = P.bufs(NB3), P.bufs(NB3)
        L = v3(A.f32(NT * 20), NT, 20)
        NG = 4
        GN = NT // NG
        bLg = P.bufs(NG)

        def tile_step(t):
            xt, bx = xt2[t % NB3], bxt2[t % NB3]
            P.dma("sp", lambda e, xt=xt, t=t: e.dma_start(out=xt, in_=Hin[t * 128:(t + 1) * 128, :]),
                  reads=[bHin[t]], writes=[bx])
            yft, byft = yf[t % NB3], byf[t % NB3]
            rmsnorm_tile(xt, bx, gbt, bg, [(yft, byft)], junk, bjunk, ss, rs, bss)
            P.op("act", lambda e, yft=yft, t=t: e.copy(out=yb_t(t), in_=yft), reads=[byft], writes=[bybA[t]], c=1.1)
            if t == 6:
                prefetch_expert0([bybA[t]])
            ylt, bylt = yl[t % NB3], byl[t % NB3]
            P.op("dve", lambda e, yft=yft, ylt=ylt, t=t: e.tensor_tensor(out=ylt, in0=yft, in1=yb_t(t), op=ALU.subtract),
                 reads=[byft, bybA[t]], writes=[bylt], c=1.1)
            b0 = 0 if t % 2 == 0 else 2
            ph_, pl_ = bank_bf(b0), bank_bf(b0 + 1)
            for k in range(8):
                P.op("pe", lambda e, k=k, t=t, ph_=ph_: e.transpose(
                    out=ph_[:, k * 128:(k + 1) * 128], in_=yb_t(t)[:, k * 128:(k + 1) * 128], identity=identb),
                    reads=[bybA[t], bconst], writes=[pb[b0]], c=0.08)
            for k in range(8):
                P.op("pe", lambda e, k=k, ylt=ylt, pl_=pl_: e.transpose(
                    out=pl_[:, k * 128:(k + 1) * 128], in_=ylt[:, k * 128:(k + 1) * 128], identity=identb),
                    reads=[bylt, bconst], writes=[pb[b0 + 1]], c=0.08)
            yh_, byh_ = yhT[t % NB3], byhT[t % NB3]
            yl_, byl_ = ylT[t % NB3], bylT[t % NB3]
            P.op("act", lambda e, yh_=yh_, ph_=ph_: e.copy(out=yh_, in_=v3(ph_, 8, 128)), reads=[pb[b0]], writes=[byh_], c=1.0)
            if t % 2 == 1:
                P.op("act", lambda e, yl_=yl_, pl_=pl_: e.copy(out=yl_, in_=v3(pl_, 8, 128)), reads=[pb[b0 + 1]], writes=[byl_], c=1.0)
            else:
                P.op("dve", lambda e, yl_=yl_, pl_=pl_: e.tensor_copy(out=yl_, in_=v3(pl_, 8, 128)), reads=[pb[b0 + 1]], writes=[byl_], c=1.0)
            lb = 4 + (t // 8) % 2
            c0 = (t % 8) * 60
            for k in range(8):
                P.op("pe", lambda e, k=k, yh_=yh_, lb=lb, c0=c0: e.matmul(
                    bank(lb)[:, c0:c0 + 40], lhsT=yh_[:, k, :], rhs=whl[:, k, :],
                    start=(k == 0), stop=(k == 7)), reads=[byh_, bwhl], writes=[pb[lb]], c=0.08)
            for k in range(8):
                P.op("pe", lambda e, k=k, yl_=yl_, lb=lb, c0=c0: e.matmul(
                    bank(lb)[:, c0 + 40:c0 + 60], lhsT=yl_[:, k, :], rhs=whl[:, k, 0:20],
                    start=(k == 0), stop=(k == 7)), reads=[byl_, bwhl], writes=[pb[lb]], c=0.08)
            if t % 8 == 7:
                hb = t // 8
                pv_ = v3(bank(lb)[:, 0:480], 8, 60)
                Lh = L[:, hb * 8:(hb + 1) * 8, :]
                bL_ = bLg[t // GN]
                P.op("dve", lambda e, pv_=pv_, Lh=Lh: e.tensor_tensor(
                    out=Lh, in0=pv_[:, :, 0:20], in1=rb.unsqueeze(1).to_broadcast([128, 8, 20]), op=ALU.add),
                    reads=[pb[lb], bwr], writes=[bL_])
                P.op("dve", lambda e, pv_=pv_, Lh=Lh: e.tensor_tensor(out=Lh, in0=Lh, in1=pv_[:, :, 20:40], op=ALU.add),
                     reads=[pb[lb], bL_], writes=[bL_])
                P.op("dve", lambda e, pv_=pv_, Lh=Lh: e.tensor_tensor(out=Lh, in0=Lh, in1=pv_[:, :, 40:60], op=ALU.add),
                     reads=[pb[lb], bL_], writes=[bL_])

        def T(n):
            return A.f32(NT * n)
        mg = T(1)
        G = v3(T(4), NT, 4)
        ex4 = v3(T(4), NT, 4)
        se = T(1)
        g1 = T(1)
        tmp16 = v3(T(16), NT, 16)
        sel = v3(T(4), NT, 4)
        m1, m2 = T(1), T(1)
        o1 = v3(T(4), NT, 4)
        o2 = v3(T(4), NT, 4)
        sel2 = v3(T(4), NT, 4)
        dl, exd, w1g, w2g = T(1), T(1), T(1), T(1)
        E1 = v3(T(16), NT, 16)
        E2 = v3(T(16), NT, 16)
        S16 = v3(T(16), NT, 16)
        Bc = [v3(T(16), NT, 16), v3(T(16), NT, 16)]
        rank = v3(T(16), NT, 16)
        valid = v3(T(16), NT, 16)
        pos16 = v3(T(16), NT, 16)
        idf = v3(T(2), 2, NT)
        vld = v3(T(2), 2, NT)
        tokp1 = A.f32(NT)
        btk = P.buf()
        P.dma("sp", lambda e: e.dma_start(out=tokp1, in_=c_tokp1_d), writes=[btk])
        BIG = float(NEXP * CAP + 64)
        idx3 = v3(idx, 2, NT)
        gat3 = v3(gat, 2, NT)
        brg = P.bufs(NG)
        broute_g = P.bufs(NG)
        bexg = P.bufs(NG)
        incs = []

        def route(gi):
            t0, t1 = gi * GN, (gi + 1) * GN
            sl = slice(t0, t1)
            br = brg[gi]
            bL_ = bLg[gi]
            deps_prev = [brg[gi - 1]] if gi > 0 else []

            def dv(fn, reads=(), writes=()):
                P.op("dve", fn, reads=[br, bL_, bwr] + list(reads), writes=[br] + list(writes), c=0.3)

            def bc1(ap1):
                return ap1.unsqueeze(2).to_broadcast([128, GN, 4])

            def g4(ap3):
                return ap3.rearrange("p a (g e) -> p a g e", g=4, e=4)
            lgv = L[:, sl, 0:4]
            lev = L[:, sl, 4:20]
            mg_, se_, g1_, m1_, m2_, dl_, exd_, w1_, w2_ = [x_[:, sl] for x_ in (mg, se, g1, m1, m2, dl, exd, w1g, w2g)]
            G_, ex4_, sel_, o1_, o2_, sel2_ = [x_[:, sl, :] for x_ in (G, ex4, sel, o1, o2, sel2)]
            tmp_, E1_, E2_, S_, rank_, valid_, pos_ = [x_[:, sl, :] for x_ in (tmp16, E1, E2, S16, rank, valid, pos16)]
            B0, B1 = Bc[0][:, sl, :], Bc[1][:, sl, :]
            dv(lambda e: e.tensor_reduce(out=mg_, in_=lgv, axis=AX.X, op=ALU.max))
            dv(lambda e: e.tensor_tensor(out=G_, in0=lgv, in1=bc1(mg_), op=ALU.is_equal))
            dv(lambda e: e.tensor_tensor(out=ex4_, in0=lgv, in1=bc1(mg_), op=ALU.subtract))
            P.op("act", lambda e: e.activation(out=ex4_, in_=ex4_, func=AF.Exp), reads=[br], writes=[br], c=0.3)
            dv(lambda e: e.tensor_reduce(out=se_, in_=ex4_, axis=AX.X, op=ALU.add))
            dv(lambda e: e.reciprocal(out=g1_, in_=se_))
            dv(lambda e: e.tensor_tensor(out=g4(tmp_), in0=g4(lev), in1=G_.unsqueeze(3).to_broadcast([128, GN, 4, 4]), op=ALU.mult))
            dv(lambda e: e.tensor_reduce(out=sel_, in_=tmp_.rearrange("p a (g e) -> p a e g", g=4, e=4), axis=AX.X, op=ALU.add))
            dv(lambda e: e.tensor_reduce(out=m1_, in_=sel_, axis=AX.X, op=ALU.max))
            dv(lambda e: e.tensor_tensor(out=o1_, in0=sel_, in1=bc1(m1_), op=ALU.is_equal))
            dv(lambda e: e.tensor_scalar(out=sel2_, in0=o1_, scalar1=-1e30, scalar2=None, op0=ALU.mult))
            dv(lambda e: e.tensor_tensor(out=sel2_, in0=sel2_, in1=sel_, op=ALU.add))
            dv(lambda e: e.tensor_reduce(out=m2_, in_=sel2_, axis=AX.X, op=ALU.max))
            dv(lambda e: e.tensor_tensor(out=o2_, in0=sel2_, in1=bc1(m2_), op=ALU.is_equal))
            dv(lambda e: e.tensor_tensor(out=dl_, in0=m2_, in1=m1_, op=ALU.subtract))
            P.op("act", lambda e: e.activation(out=exd_, in_=dl_, func=AF.Exp), reads=[br], writes=[br], c=0.3)
            dv(lambda e: e.tensor_scalar(out=w1_, in0=exd_, scalar1=1.0, scalar2=None, op0=ALU.add))
            dv(lambda e: e.reciprocal(out=w1_, in_=w1_))
            dv(lambda e: e.tensor_tensor(out=w2_, in0=exd_, in1=w1_, op=ALU.mult))
            dv(lambda e: e.tensor_tensor(out=w1_, in0=w1_, in1=g1_, op=ALU.mult))
            dv(lambda e: e.tensor_tensor(out=w2_, in0=w2_, in1=g1_, op=ALU.mult))
            for (Ek, ok) in ((E1_, o1_), (E2_, o2_)):
                dv(lambda e, Ek=Ek, ok=ok: e.tensor_tensor(
                    out=g4(Ek), in0=G_.unsqueeze(3).to_broadcast([128, GN, 4, 4]),
                    in1=ok.unsqueeze(2).to_broadcast([128, GN, 4, 4]), op=ALU.mult))
            dv(lambda e: e.tensor_tensor(out=S_, in0=E1_, in1=E2_, op=ALU.add))
            Sf = S16.rearrange("p a b -> p (a b)")[:, t0 * 16:t1 * 16]
            cs = slice(gi * GN * 16, (gi + 1) * GN * 16)
            P.op("pe", lambda e: e.matmul(bank(6)[:, cs], lhsT=triu, rhs=Sf, start=True, stop=True), reads=[br, bwr], writes=[pb[6]])
            P.op("pe", lambda e: e.matmul(bank(7)[:, cs], lhsT=onesm, rhs=Sf, start=True, stop=True), reads=[br, bwr], writes=[pb[7]])
            psA = v3(bank(6)[:, cs], GN, 16)
            psB = v3(bank(7)[:, cs], GN, 16)
            P.op("dve", lambda e: e.tensor_copy(out=B0, in_=psB), reads=[pb[7], br], writes=[br])
            cur = [B0, B1]
            sft = 1
            while sft < GN:
                a_, b_ = cur
                dv(lambda e, a_=a_, b_=b_, sft=sft: e.tensor_copy(out=b_[:, 0:sft, :], in_=a_[:, 0:sft, :]))
                dv(lambda e, a_=a_, b_=b_, sft=sft: e.tensor_tensor(out=b_[:, sft:GN, :], in0=a_[:, sft:GN, :], in1=a_[:, 0:GN - sft, :], op=ALU.add))
                cur = [b_, a_]
                sft *= 2
            inc = cur[0]
            if gi > 0:
                pin = incs[gi - 1]
                dv(lambda e, inc=inc, pin=pin: e.tensor_tensor(out=inc, in0=inc, in1=pin[:, GN - 1:GN, :].to_broadcast([128, GN, 16]), op=ALU.add),
                   reads=deps_prev)
            incs.append(inc)
            P.op("dve", lambda e, inc=inc: e.tensor_tensor(out=rank_, in0=inc, in1=psB, op=ALU.subtract), reads=[pb[7], br], writes=[br])
            P.op("dve", lambda e: e.tensor_tensor(out=rank_, in0=rank_, in1=psA, op=ALU.add), reads=[pb[6], br], writes=[br])
            dv(lambda e: e.tensor_scalar(out=valid_, in0=rank_, scalar1=float(CAP), scalar2=None, op0=ALU.is_lt))
            dv(lambda e: e.tensor_tensor(out=pos_, in0=rank_, in1=eoff.unsqueeze(1).to_broadcast([128, GN, 16]), op=ALU.add))
            dv(lambda e: e.tensor_scalar(out=pos_, in0=pos_, scalar1=-BIG, scalar2=None, op0=ALU.add))
            dv(lambda e: e.tensor_tensor(out=pos_, in0=pos_, in1=valid_, op=ALU.mult))
            dv(lambda e: e.tensor_scalar(out=pos_, in0=pos_, scalar1=BIG, scalar2=None, op0=ALU.add))
            brt = broute_g[gi]
            for k, (Ek, wk) in enumerate(((E1_, w1_), (E2_, w2_))):
                dv(lambda e, Ek=Ek: e.tensor_tensor(out=tmp_, in0=Ek, in1=pos_, op=ALU.mult))
                dv(lambda e, k=k: e.tensor_reduce(out=idf[:, k, sl], in_=tmp_, axis=AX.X, op=ALU.add))
                dv(lambda e, Ek=Ek: e.tensor_tensor(out=tmp_, in0=Ek, in1=valid_, op=ALU.mult))
                dv(lambda e, k=k: e.tensor_reduce(out=vld[:, k, sl], in_=tmp_, axis=AX.X, op=ALU.add))
                P.op("dve", lambda e, k=k, wk=wk: e.tensor_tensor(out=gat3[:, k, sl], in0=wk, in1=vld[:, k, sl], op=ALU.mult),
                     reads=[br], writes=[brt], c=0.3)
            P.op("dve", lambda e: e.tensor_copy(out=idx3[:, :, sl], in_=idf[:, :, sl]), reads=[br], writes=[brt], c=0.3)
            bex = bexg[gi]
            P.op("dve", lambda e: e.tensor_copy(out=ybA[:, sl, 512:513], in_=tokp1[:, sl].unsqueeze(2)), reads=[btk], writes=[bex], c=0.3)
            P.op("dve", lambda e: e.tensor_copy(out=ybA[:, sl, 513:514], in_=gat3[:, 0, sl].unsqueeze(2)), reads=[bex, brt], writes=[bex], c=0.3)
            P.op("dve", lambda e: e.tensor_copy(out=ybA[:, sl, 514:515], in_=gat3[:, 1, sl].unsqueeze(2)), reads=[bex, brt], writes=[bex], c=0.3)
            P.op("dve", lambda e: e.tensor_copy(out=ybA[:, sl, 515:516], in_=idf[:, 1, sl].unsqueeze(2)), reads=[bex, br], writes=[bex], c=0.3)
            for t in range(t0, t1):
                for k in range(2):
                    P.dma("pool", lambda e, t=t, k=k: e.indirect_dma_start(
                        out=XS, out_offset=bass.IndirectOffsetOnAxis(ap=idx[:, k * NT + t:k * NT + t + 1], axis=0),
                        in_=ybA[:, t, :], in_offset=None, bounds_check=bc_reg(e), oob_is_err=False),
                        reads=[bybA[t], brt, bXSz, bex], writes=[bXS], c=5.0)

        for gi in range(NG):
            for t in range(gi * GN, (gi + 1) * GN):
                tile_step(t)
            route(gi)
        P.barrier()
        if CUT[0] == 101:
            return

        A.release(m_phase)
        w1b[1] = v3(A.bf16(8 * 512), 8, 512)
        w3b[1] = v3(A.bf16(8 * 512), 8, 512)
        w2b[1] = v3(A.bf16(4 * D), 4, D)
        NXS = 8
        xs = [A.f32(516) for _ in range(NXS)]
        bxs = P.bufs(NXS)
        xsT = [v3(A.bf16(8 * 128), 8, 128) for _ in range(2)]
        bxsT = P.bufs(2)
        s1 = [A.f32(512) for _ in range(2)]
        bs1 = P.bufs(2)
        hm = [A.bf16(512) for _ in range(2)]
        bhm = P.bufs(2)
        hmT = [v3(A.bf16(4 * 128), 4, 128) for _ in range(2)]
        bhmT = P.bufs(2)
        ys = [A.bf16(D) for _ in range(6)]
        bys = P.bufs(6)
        cslot = A.f32(NEXP * CAP_T)
        bcs = P.buf()
        P.dma("sp", lambda e: e.dma_start(out=cslot, in_=c_slot_d), writes=[bcs])
        BIG2 = 20000.0
        NST = NEXP * CAP_T
        ev = v3(A.f32(NST * 4), NST, 4)
        bev = P.buf()
        for q4 in range(4):
            n0, n1 = q4 * NST // 4, (q4 + 1) * NST // 4
            P.dma("sp", lambda e, n0=n0, n1=n1: e.dma_start(
                out=ev[:, n0:n1, :], in_=XS[n0 * 128:n1 * 128, 512:516].rearrange("(j p) c -> p j c", p=128)),
                reads=[bXS], writes=[bev], c=20.0)
        kf, dg, gate, dest, inv = [A.f32(NST) for _ in range(5)]
        di = A.i32(NST)
        brt_ = P.buf()

        def dv2(fn):
            P.op("dve", fn, reads=[bev, brt_, bcs], writes=[brt_], c=0.3)
        dv2(lambda e: e.tensor_tensor(out=kf, in0=ev[:, :, 3], in1=cslot, op=ALU.is_equal))
        dv2(lambda e: e.tensor_tensor(out=dg, in0=ev[:, :, 2], in1=ev[:, :, 1], op=ALU.subtract))
        dv2(lambda e: e.tensor_tensor(out=dg, in0=dg, in1=kf, op=ALU.mult))
        dv2(lambda e: e.tensor_tensor(out=gate, in0=dg, in1=ev[:, :, 1], op=ALU.add))
        dv2(lambda e: e.scalar_tensor_tensor(out=dest, in0=kf, scalar=float(TPC), in1=ev[:, :, 0], op0=ALU.mult, op1=ALU.add))
        dv2(lambda e: e.tensor_scalar(out=inv, in0=ev[:, :, 0], scalar1=0.0, scalar2=None, op0=ALU.is_equal))
        dv2(lambda e: e.scalar_tensor_tensor(out=dest, in0=inv, scalar=BIG2, in1=dest, op0=ALU.mult, op1=ALU.add))
        dv2(lambda e: e.tensor_scalar(out=dest, in0=dest, scalar1=-1.0, scalar2=None, op0=ALU.add))
        dv2(lambda e: e.tensor_copy(out=di, in_=dest))
        BK = M2_BANKS[M2_LAYOUT[0]]
        yb0 = BK['y']
        it = 0
        for ex in range(NEXP):
            wb_ = ex % 2
            if ex > 0:
                P.dma("pool", lambda e, ex=ex, w_=w1b[wb_]: e.dma_start(out=w_, in_=w1_d[li, ex].rearrange("(k p) n -> p k n", p=128)), writes=[bw[wb_][0]], c=22.0)
                P.dma("pool", lambda e, ex=ex, w_=w3b[wb_]: e.dma_start(out=w_, in_=w3_d[li, ex].rearrange("(k p) n -> p k n", p=128)), writes=[bw[wb_][1]], c=22.0)
                P.dma("pool", lambda e, ex=ex, w_=w2b[wb_]: e.dma_start(out=w_, in_=w2_d[li, ex].rearrange("(k p) n -> p k n", p=128)), writes=[bw[wb_][2]], c=22.0)
            for j in range(CAP_T):
                r0 = ex * CAP + j * 128
                x_, bx_ = xs[it % NXS], bxs[it % NXS]
                P.dma("sp", lambda e, x_=x_, r0=r0: e.dma_start(out=x_, in_=XS[r0:r0 + 128, :]), reads=[bXS], writes=[bx_])
                xb_ = x_[:, 0:512].bitcast(BF16)
                tbx = BK['xt'][it % len(BK['xt'])]
                pbf = bank_bf(tbx)
                for k in range(8):
                    P.op("pe", lambda e, k=k, pbf=pbf, xb_=xb_: e.transpose(out=pbf[:, k * 128:(k + 1) * 128], in_=xb_[:, k * 128:(k + 1) * 128], identity=identb),
                         reads=[bx_, bconst], writes=[pb[tbx]], c=0.08)
                xT_, bxT_ = xsT[it % 2], bxsT[it % 2]
                P.op("act", lambda e, xT_=xT_, pbf=pbf: e.copy(out=xT_, in_=v3(pbf, 8, 128)), reads=[pb[tbx]], writes=[bxT_], c=1.0)
                hb = BK['h'][it % len(BK['h'])]
                for (wi, wt, bk) in ((0, w1b[wb_], hb), (1, w3b[wb_], hb + 1)):
                    for k in range(8):
                        P.op("pe", lambda e, k=k, wt=wt, bk=bk, xT_=xT_: e.matmul(bank(bk), lhsT=xT_[:, k, :], rhs=wt[:, k, :], start=(k == 0), stop=(k == 7)),
                             reads=[bxT_, bw[wb_][wi]], writes=[pb[bk]])
                s1_, bs1_ = s1[it % 2], bs1[it % 2]
                P.op("act", lambda e, s1_=s1_, hb=hb: e.activation(out=s1_, in_=bank(hb), func=AF.Silu), reads=[pb[hb]], writes=[bs1_])
                hm_, bhm_ = hm[it % 2], bhm[it % 2]
                P.op("dve", lambda e, hm_=hm_, s1_=s1_, hb=hb: e.tensor_tensor(out=hm_, in0=s1_, in1=bank(hb + 1), op=ALU.mult), reads=[bs1_, pb[hb + 1]], writes=[bhm_])
                tbh = BK['ht'][it % len(BK['ht'])]
                pbf2 = bank_bf(tbh)
                for k in range(4):
                    P.op("pe", lambda e, k=k, pbf2=pbf2, hm_=hm_: e.transpose(out=pbf2[:, k * 128:(k + 1) * 128], in_=hm_[:, k * 128:(k + 1) * 128], identity=identb),
                         reads=[bhm_, bconst], writes=[pb[tbh]], c=0.08)
                hT_, bhT_ = hmT[it % 2], bhmT[it % 2]
                P.op("dve", lambda e, hT_=hT_, pbf2=pbf2: e.tensor_copy(out=hT_, in_=v3(pbf2[:, 0:512], 4, 128)), reads=[pb[tbh]], writes=[bhT_])
                for hf in range(2):
                    for k in range(4):
                        P.op("pe", lambda e, k=k, hf=hf, hT_=hT_, w2_=w2b[wb_]: e.matmul(bank(yb0 + hf), lhsT=hT_[:, k, :], rhs=w2_[:, k, hf * 512:(hf + 1) * 512],
                                                                      start=(k == 0), stop=(k == 3)),
                             reads=[bhT_, bw[wb_][2]], writes=[pb[yb0 + hf]])
                ys_, bys_ = ys[it % 6], bys[it % 6]
                P.op("act", lambda e, ys_=ys_, it=it: e.activation(out=ys_, in_=bank(yb0, 2), func=AF.Copy, scale=gate[:, it:it + 1]),
                     reads=[pb[yb0], pb[yb0 + 1], brt_], writes=[bys_], c=1.1)
                P.dma("pool", lambda e, ys_=ys_, it=it: e.indirect_dma_start(
                    out=YK, out_offset=bass.IndirectOffsetOnAxis(ap=di[:, it:it + 1], axis=0),
                    in_=ys_, in_offset=None, bounds_check=bc_reg(e, 2 * TPC - 1), oob_is_err=False),
                    reads=[bys_, brt_], writes=[bYK], c=5.0)
                it += 1
        P.barrier()
        if CUT[0] == 102:
            return

        A.release(m_phase)
        A.top_reset()
        if not final:
            pw = dict(win=v3(A.top_bf16(8 * D), 8, D), wou=v3(A.top_bf16(8 * D), 8, D), wgp=v4(A.top_bf16(4 * 2 * 256), 4, 2, 256),
                      bwin=P.buf(), bwou=P.buf(), bwgp=P.buf())
            PF["pool_w"] = pw
            P.dma("pool", lambda e: e.dma_start(out=pw["win"], in_=pwin_d.rearrange("(k p) n -> p k n", p=128)), writes=[pw["bwin"]], c=15.0)
            P.dma("pool", lambda e: e.dma_start(out=pw["wou"], in_=pwout_d.rearrange("(k p) n -> p k n", p=128)), writes=[pw["bwou"]], c=15.0)
            for g in range(4):
                P.dma("pool", lambda e, g=g: e.dma_start(out=pw["wgp"][:, g, :, :], in_=pwg_d[g].rearrange("(k p) n -> p k n", p=128)), writes=[pw["bwgp"]])
        y1 = [A.bf16(D) for _ in range(2)]
        y2 = [A.bf16(D) for _ in range(2)]
        by1, by2 = P.bufs(2), P.bufs(2)
        ht = [A.f32(D) for _ in range(2)]
        bht = P.bufs(2)
        acc = [A.f32(D) for _ in range(2)]
        bacc = P.bufs(2)
        hn = [A.f32(D) for _ in range(2)]
        bhn = P.bufs(2)
        gft = None
        if final:
            gft = A.f32(D)
            bgf = P.buf()
            P.dma("sp", lambda e: e.dma_start(out=gft, in_=nfin_d[0:1, :].partition_broadcast(128)), writes=[bgf])
            junk = A.f32(D)
            bjunk = P.buf()
            ss, rs = A.f32(2), A.f32(2)
            bss = P.buf()
            fo = [A.f32(D) for _ in range(2)]
            bfo = P.bufs(2)
        for t in range(NT):
            i = t % 2
            P.dma("sp", lambda e, t=t, i=i: e.dma_start(out=y1[i], in_=YK[t * 128:(t + 1) * 128, :]), reads=[bYK], writes=[by1[i]])
            P.dma("sp", lambda e, t=t, i=i: e.dma_start(out=y2[i], in_=YK[TPC + t * 128:TPC + (t + 1) * 128, :]), reads=[bYK], writes=[by2[i]])
            P.dma("sp", lambda e, t=t, i=i: e.dma_start(out=ht[i], in_=Hin[t * 128:(t + 1) * 128, :]), reads=[bHin[t]], writes=[bht[i]])
            P.op("dve", lambda e, i=i: e.tensor_tensor(out=acc[i], in0=ht[i], in1=y1[i], op=ALU.add),
                 reads=[by1[i], bht[i]], writes=[bacc[i]], c=1.1)
            P.op("dve", lambda e, i=i: e.tensor_tensor(out=hn[i], in0=acc[i], in1=y2[i], op=ALU.add),
                 reads=[by2[i], bacc[i]], writes=[bhn[i]], c=1.1)
            if final:
                rmsnorm_tile(hn[i], bhn[i], gft, bgf, [(fo[i], bfo[i])], junk, bjunk, ss, rs, bss)
                P.dma("sp", lambda e, t=t, i=i: e.dma_start(out=out_d[t * 128:(t + 1) * 128, :], in_=fo[i]), reads=[bfo[i]], writes=[bOUT])
            else:
                P.dma("sp", lambda e, t=t, i=i: e.dma_start(out=Hout[t * 128:(t + 1) * 128, :], in_=hn[i]), reads=[bhn[i]], writes=[bH[id(Hout)][t]])
        P.barrier()

    def pool_phase():
        A.release(persist_mark)
        gbt = A.f32(D)
        bg = P.buf()
        P.dma("sp", lambda e: e.dma_start(out=gbt, in_=nmix_d[1:2, :].partition_broadcast(128)), writes=[bg])
        yT = v3(A.bf16(8 * SEQ), 8, SEQ)
        byT = P.bufs(16)
        zT = v3(A.bf16(8 * SEQ), 8, SEQ)
        bzT = P.buf()
        plT = v3(A.bf16(4 * SEQ), 4, SEQ)
        bplT = P.bufs(4)
        pw = PF["pool_w"]
        win, wou, wgp = pw["win"], pw["wou"], pw["wgp"]
        bwin, bwou, bwgp = pw["bwin"], pw["bwou"], pw["bwgp"]
        psc = A.f32(8)
        rc = A.f32(16)
        bpc = P.buf()
        P.dma("sp", lambda e: e.dma_start(out=psc, in_=pscT_d), writes=[bpc])
        P.dma("sp", lambda e: e.dma_start(out=rc, in_=c_rc_d), writes=[bpc])
        tl = dict(xt=[A.f32(D), A.f32(D)], bxt=P.bufs(2), yb=[A.bf16(D), A.bf16(D)], byb=P.bufs(2),
                  junk=A.f32(D), bjunk=P.buf(), ss=A.f32(2), rs=A.f32(2), bss=P.buf())
        uT = [A.f32(SEQ) for _ in range(2)]
        buT = P.bufs(2)
        sA = [A.f32(SEQ) for _ in range(2)]
        bsA = P.bufs(2)
        res = [tl["junk"], A.f32(D)]
        bres = [tl["bjunk"], P.buf()]
        h2_tiles = bH[id(H2)]
        xs_zero_begin()
        for s in range(2):
            seq_norm_transpose(H2, h2_tiles, s, gbt, bg, yT, byT, tl)
            for c in range(8):
                g = c // 2
                w = 2 << g
                u_, bu_ = uT[c % 2], buT[c % 2]
                xs_zero_some(5, after=[bu_])
                for q in range(4):
                    bk = (c * 4 + q) % 4
                    for k in range(8):
                        P.op("pe", lambda e, k=k, c=c, q=q, bk=bk: e.matmul(bank(bk), lhsT=win[:, k, c * 128:(c + 1) * 128], rhs=yT[:, k, q * 512:(q + 1) * 512],
                                                                         start=(k == 0), stop=(k == 7)), reads=[bwin] + byT[4 * q:4 * q + 4], writes=[pb[bk]])
                    P.op("act", lambda e, u_=u_, q=q, bk=bk: e.copy(out=u_[:, q * 512:(q + 1) * 512], in_=bank(bk)), reads=[pb[bk]], writes=[bu_])
                cur, bcur = u_, bu_
                sft = 1
                pp = 0
                while sft < w:
                    nx, bnx = sA[pp], bsA[pp]
                    P.op("dve", lambda e, nx=nx, cur=cur, sft=sft: e.tensor_copy(out=nx[:, 0:sft], in_=cur[:, 0:sft]), reads=[bcur], writes=[bnx])
                    P.op("dve", lambda e, nx=nx, cur=cur, sft=sft: e.tensor_tensor(out=nx[:, sft:SEQ], in0=cur[:, sft:SEQ], in1=cur[:, 0:SEQ - sft], op=ALU.add),
                         reads=[bcur], writes=[bnx])
                    cur, bcur = nx, bnx
                    pp = 1 - pp
                    sft *= 2
                P.op("dve", lambda e, cur=cur, u_=u_, c=c, w=w: e.scalar_tensor_tensor(out=plT[:, c % 4, :], in0=cur, scalar=1.0 / w, in1=u_, op0=ALU.mult, op1=ALU.subtract),
                     reads=[bcur, bu_], writes=[bplT[c % 4]])
                tmpc = sA[pp]
                btmpc = bsA[pp]
                P.op("dve", lambda e, cur=cur, tmpc=tmpc, w=w: e.tensor_tensor(out=tmpc[:, 0:w - 1], in0=cur[:, 0:w - 1], in1=rc[:, 0:w - 1], op=ALU.mult),
                     reads=[bcur, bpc], writes=[btmpc])
                P.op("dve", lambda e, tmpc=tmpc, u_=u_, c=c, w=w: e.tensor_tensor(out=plT[:, c % 4, 0:w - 1], in0=tmpc[:, 0:w - 1], in1=u_[:, 0:w - 1], op=ALU.subtract),
                     reads=[btmpc, bu_], writes=[bplT[c % 4]])
                if c % 2 == 1:
                    for eo in range(2):
                        co = 2 * g + eo
                        for q in range(4):
                            bk = 4 + (co * 4 + q) % 4
                            for ci in range(2):
                                P.op("pe", lambda e, g=g, eo=eo, ci=ci, q=q, bk=bk: e.matmul(
                                    bank(bk), lhsT=wgp[:, g, ci, eo * 128:(eo + 1) * 128], rhs=plT[:, (2 * g + ci) % 4, q * 512:(q + 1) * 512],
                                    start=(ci == 0), stop=(ci == 1)), reads=[bwgp, bplT[(2 * g + ci) % 4]], writes=[pb[bk]])
                            P.op("act", lambda e, co=co, q=q, bk=bk: e.activation(out=zT[:, co, q * 512:(q + 1) * 512], in_=bank(bk), func=AF.Copy, scale=psc[:, co:co + 1]),
                                 reads=[pb[bk], bpc], writes=[bzT])
            for t in range(16):
                gt = s * 16 + t
                b0 = 0 if t % 2 == 0 else 2
                for hf in range(2):
                    for k in range(8):
                        P.op("pe", lambda e, hf=hf, k=k, b0=b0, t=t: e.matmul(bank(b0 + hf), lhsT=zT[:, k, t * 128:(t + 1) * 128], rhs=wou[:, k, hf * 512:(hf + 1) * 512],
                                                                          start=(k == 0), stop=(k == 7)), reads=[bzT, bwou], writes=[pb[b0 + hf]])
                xt, bx = tl["xt"][t % 2], tl["bxt"][t % 2]
                P.dma("sp", lambda e, xt=xt, gt=gt: e.dma_start(out=xt, in_=H2[gt * 128:(gt + 1) * 128, :]), reads=[h2_tiles[gt]], writes=[bx])
                rs_, brs_ = res[t % 2], bres[t % 2]
                P.op("dve", lambda e, rs_=rs_, xt=xt, b0=b0: e.tensor_tensor(out=rs_, in0=xt, in1=bank(b0, 2), op=ALU.add),
                     reads=[bx, pb[b0], pb[b0 + 1]], writes=[brs_])
                P.dma("sp", lambda e, rs_=rs_, gt=gt: e.dma_start(out=H3[gt * 128:(gt + 1) * 128, :], in_=rs_), reads=[brs_], writes=[bH[id(H3)][gt]])
        P.barrier()

    phases = [("attn", attn_phase), ("moe0", lambda: moe_phase(0, H1, H2, False)),
              ("pool", pool_phase), ("moe1", lambda: moe_phase(1, H3, None, True))]
    for name, fn in phases:
        fn()
        if stop_after == name:
            break
    stats = P.finalize()
    P.es.close()
    return nc, stats


def make_constants():
    k = np.arange(128)[:, None]
    q = np.arange(128)[None, :]
    m = np.concatenate([(k <= q), (k >= q)], axis=1).astype(np.float32)
    mask = np.concatenate([m, m], axis=1)
    inv_freq = (500000.0 ** (-(np.arange(8, dtype=np.float32) * 2.0 / 16.0))).astype(np.float32)
    return dict(
        c_identf=np.eye(128, dtype=np.float32),
        c_mask=mask,
        c_triu=(k < q).astype(np.float32),
        c_ones=np.ones((128, 128), np.float32),
        c_invf=np.broadcast_to(inv_freq[None, :], (128, 8)).copy(),
        c_eoff=np.broadcast_to((np.arange(16, dtype=np.float32) * CAP)[None, :], (128, 16)).copy(),
        c_rc=np.broadcast_to((1.0 / np.arange(1, 17, dtype=np.float32))[None, :], (128, 16)).copy(),
        c_tokp1=(np.arange(NT, dtype=np.float32)[None, :] * 128 + np.arange(128, dtype=np.float32)[:, None] + 1.0).astype(np.float32),
        c_slot=(np.arange(NEXP * CAP_T, dtype=np.float32)[None, :] * 128 + np.arange(128, dtype=np.float32)[:, None]).astype(np.float32),
    )


def make_in_maps(inputs, ncores=NCORES):
    f = lambda a: np.ascontiguousarray(np.asarray(a, dtype=np.float32))
    x = f(inputs["x"])
    pos = np.asarray(inputs["positions"]).astype(np.int32)
    shared = dict(
        norm_mix=f(inputs["norm_mix"]), norm_ffn=f(inputs["norm_ffn"]),
        norm_final=f(inputs["norm_final"]).reshape(1, D),
        attn_w_in=f(inputs["attn_w_in"])[0], attn_w_out=f(inputs["attn_w_out"])[0],
        pool_w_in=f(inputs["pool_w_in"])[0], pool_w_group=f(inputs["pool_w_group"])[0],
        pool_scaleT=np.ascontiguousarray(f(inputs["pool_scale"])[0].reshape(8, 128).T),
        pool_w_out=f(inputs["pool_w_out"])[0],
        router_group_w=f(inputs["router_group_w"]), router_group_b=f(inputs["router_group_b"]),
        router_expert_w=f(inputs["router_expert_w"]),
        router_expert_b=f(inputs["router_expert_b"]).reshape(2, 16),
        expert_w1=f(inputs["expert_w1"]), expert_w3=f(inputs["expert_w3"]), expert_w2=f(inputs["expert_w2"]),
    )
    shared.update(make_constants())
    maps = []
    for c in range(ncores):
        xs = x[2 * c:2 * c + 2].reshape(TPC, D)
        p = pos[2 * c:2 * c + 2].reshape(NT, 128).T
        m = dict(shared)
        m["x"] = np.ascontiguousarray(xs)
        m["posT"] = np.ascontiguousarray(p)
        maps.append(m)
    return maps


_CACHE = {}


def kernel(**inputs):
    if "nc" not in _CACHE:
        _CACHE["nc"] = build_program()[0]
    nc = _CACHE["nc"]
    maps = make_in_maps(inputs)
    res = run_bass_kernel_spmd(nc, maps, core_ids=list(range(NCORES)))
    outs = [np.asarray(r["out"]).reshape(2, SEQ, D) for r in res.results]
    return np.concatenate(outs, axis=0).astype(np.float32)
```

```python
from contextlib import ExitStack
import math
import numpy as np
import ml_dtypes
import concourse.bass as bass
import concourse.mybir as mybir
from concourse.bass_utils import run_bass_kernel_spmd

F32 = mybir.dt.float32
BF16 = mybir.dt.bfloat16
I32 = mybir.dt.int32
ALU = mybir.AluOpType
AF = mybir.ActivationFunctionType
AX = mybir.AxisListType

NCORES = 8
SEQ = 2048
D = 1024
TPC = 4096
NT = 32
CAP_T = 5
CAP = CAP_T * 128
NEXP = 16
EPS = 1e-6
DILS = (1, 4, 16)
CUT = [0]
ZERO_FROM_TILE = 3
ATT_CFG = [3, 3]
M2_LAYOUT = [0]
M2_BANKS = [dict(h=[0], y=2, xt=[4, 5], ht=[6, 7]), dict(h=[0, 2], y=4, xt=[6], ht=[7])]

ENGS = ("pe", "act", "dve", "pool", "sp")
EPOCH = 12000
RING = {"sp": 40, "pool": 24, "act": 8}
DEF_COST = {"pe": 0.23, "act": 0.6, "dve": 0.8, "pool": 1.0, "sp": 0.1}
DEF_COST_DMA = 4.0
DMA_ISSUE = 0.25


class Buf:
    __slots__ = ("name", "w", "rs", "rd", "excl")

    def __init__(self, name):
        self.name = name
        self.excl = False
        self.w = []
        self.rs = []
        self.rd = []


class Op:
    __slots__ = ("eng", "fn", "deps", "needs_inc", "is_dma", "sem", "semval", "pre", "lidx", "seg", "cost", "fin")

    def __init__(self, eng, fn, is_dma):
        self.eng = eng
        self.fn = fn
        self.is_dma = is_dma
        self.lidx = 0
        self.seg = 0
        self.cost = 0.3
        self.fin = 0.0
        self.deps = []
        self.needs_inc = is_dma
        self.sem = None
        self.semval = None
        self.pre = None


class Prog:
    def __init__(self, nc):
        self.nc = nc
        self.es = ExitStack()
        self.streams = {e: [] for e in ENGS}
        self.ring = {}
        self.ring_n = {e: 0 for e in RING}
        for e, k in RING.items():
            self.ring[e] = [self._sem(f"dq_{e}_{i}") for i in range(k)]
        self.eng_sems = {e: [] for e in ENGS}
        self.nbuf = 0
        self.live_dma = []
        self.nops = 0
        self.seg = 0

    def _sem(self, name):
        return self.es.enter_context(self.nc.semaphore(name))

    def buf(self, name=None):
        self.nbuf += 1
        return Buf(name or f"b{self.nbuf}")

    def bufs(self, n):
        return [self.buf() for _ in range(n)]

    def _record(self, eng, fn, reads, writes, is_dma, c=None):
        o = Op(eng, fn, is_dma)
        self.nops += 1
        o.lidx = self.nops
        o.seg = self.seg
        o.cost = c if c is not None else (DEF_COST_DMA if is_dma else DEF_COST[eng])
        deps = {}
        ex = [b for b in reads if b.excl]
        if ex:
            reads = [b for b in reads if not b.excl]
            writes = list(writes) + [b for b in ex if b not in writes]
        for b in reads:
            for w_ in b.w:
                deps[id(w_)] = w_
        acc = []
        for b in writes:
            if is_dma and b.w and all(w_.is_dma for w_ in b.w) and not b.rs and not b.rd:
                acc.append(b)
                continue
            for w_ in b.w:
                deps[id(w_)] = w_
            for r in b.rs:
                deps[id(r)] = r
            for r in b.rd:
                deps[id(r)] = r
        o.deps = list(deps.values())
        for d in o.deps:
            d.needs_inc = True
        for b in reads:
            if is_dma:
                b.rd.append(o)
            else:
                b.rs.append(o)
        for b in writes:
            if b in acc:
                b.w.append(o)
            else:
                b.w = [o]
                b.rs = []
                b.rd = []
        if is_dma:
            self.live_dma.append(o)
        self.streams[eng].append(o)
        return o

    def op(self, eng, fn, reads=(), writes=(), c=None):
        return self._record(eng, fn, reads, writes, False, c)

    def dma(self, eng, fn, reads=(), writes=(), c=None):
        return self._record(eng, fn, reads, writes, True, c)

    def barrier(self):
        deps = list(self.live_dma)
        self.live_dma = []
        for d in deps:
            d.needs_inc = True
        self.seg += 1
        for e in ENGS:
            o = Op(e, lambda eng: eng.nop(), False)
            o.deps = list(deps)
            self.nops += 1
            o.lidx = self.nops
            o.seg = self.seg
            o.cost = 0.05
            self.streams[e].append(o)
        self.seg += 1

    def schedule(self):
        WINDOW = 48
        segs = {}
        for e in ENGS:
            for o in self.streams[e]:
                segs.setdefault(o.seg, {}).setdefault(e, []).append(o)
        final = {e: [] for e in ENGS}
        tnow = 0.0
        done = set()
        for sg in sorted(segs):
            per = segs[sg]
            if sg % 2 == 1:
                tails = [final[e2][-1 - i] for e2 in ENGS for i in range(min(len(final[e2]), 1))]
                tails = []
                for e2 in ENGS:
                    for o2 in reversed(final[e2]):
                        if not o2.is_dma:
                            tails.append(o2)
                            break
                for e in ENGS:
                    for o in per.get(e, []):
                        o.deps = list(o.deps) + tails
                        for d in tails:
                            d.needs_inc = True
                        o.fin = tnow
                        done.add(id(o))
                        final[e].append(o)
                continue
            et = {e: tnow for e in ENGS}
            pend = {e: list(per.get(e, [])) for e in ENGS}
            nleft = sum(len(v) for v in pend.values())
            while nleft:
                best = None
                for e in ENGS:
                    lst = pend[e]
                    for i in range(min(len(lst), WINDOW)):
                        o = lst[i]
                        st = et[e]
                        ok = True
                        for d in o.deps:
                            if id(d) not in done:
                                ok = False
                                break
                            if d.fin > st:
                                st = d.fin
                        if not ok:
                            continue
                        key = (st, o.lidx)
                        if best is None or key < best[0]:
                            best = (key, e, i, o, st)
                        if st <= et[e]:
                            break
                assert best is not None, "scheduler deadlock"
                _, e, i, o, st = best
                pend[e].pop(i)
                nleft -= 1
                if o.is_dma:
                    o.fin = st + o.cost
                    et[e] = st + DMA_ISSUE
                else:
                    o.fin = st + o.cost
                    et[e] = o.fin
                done.add(id(o))
                final[e].append(o)
            tnow = max([tnow] + [o.fin for e in ENGS for o in per.get(e, [])])
        self.streams = final
        self.est_us = tnow

    def finalize(self):
        nc = self.nc
        self.schedule()
        for e in RING:
            k = len(self.ring[e])
            i = 0
            for o in self.streams[e]:
                if not o.is_dma:
                    continue
                o.sem = self.ring[e][i % k]
                o.semval = 16 * (i // k + 1)
                if i >= k:
                    o.pre = (o.sem, 16 * (i // k))
                i += 1
        for e in ENGS:
            cnt = 0
            for o in self.streams[e]:
                if o.is_dma or not o.needs_inc:
                    continue
                ep = cnt // EPOCH
                while len(self.eng_sems[e]) <= ep:
                    self.eng_sems[e].append(self._sem(f"cs_{e}_{len(self.eng_sems[e])}"))
                o.sem = self.eng_sems[e][ep]
                o.semval = cnt % EPOCH + 1
                cnt += 1
        handles = {"pe": "tensor", "act": "scalar", "dve": "vector", "pool": "gpsimd", "sp": "sync"}
        stats = {}
        with nc.Block() as block:
            for e in ENGS:
                ops = self.streams[e]
                if not ops:
                    continue

                def body(eng, ops=ops, e=e):
                    waited = {}
                    nw = 0
                    for o in ops:
                        ws = []
                        if o.pre is not None:
                            ws.append(o.pre)
                        for d in o.deps:
                            if d.eng == e and e == "pe" and not d.is_dma:
                                continue
                            ws.append((d.sem, d.semval))
                        for (s, v) in ws:
                            key = id(s)
                            if waited.get(key, 0) >= v:
                                continue
                            waited[key] = v
                            eng.wait_ge(s, v)
                            nw += 1
                        ins = o.fn(eng)
                        if o.needs_inc:
                            ins.then_inc(o.sem, 16 if o.is_dma else 1)
                    for o in ops:
                        if o.is_dma:
                            key = id(o.sem)
                            if waited.get(key, 0) < o.semval:
                                waited[key] = o.semval
                                eng.wait_ge(o.sem, o.semval)
                    stats[e] = (len(ops), nw)

                getattr(block, handles[e])(body)
        self.stats = stats
        return stats


class Arena:
    def __init__(self, ap, n):
        self.ap = ap
        self.n = n
        self.off = 0
        self.top = n

    def top_reset(self):
        self.top = self.n

    def top_bf16(self, n):
        w = (n + 3) // 4 * 2
        self.top -= w
        assert self.off <= self.top, ("arena overflow (top)", self.off, self.top)
        return self.ap[:, self.top:self.top + w].bitcast(BF16)[:, 0:n]

    def mark(self):
        return self.off

    def release(self, m):
        self.off = m

    def f32(self, n):
        n2 = (n + 1) // 2 * 2
        assert self.off + n2 <= self.top, ("arena overflow", self.off, n2, self.top)
        a = self.ap[:, self.off:self.off + n]
        self.off += n2
        return a

    def bf16(self, n):
        w = (n + 3) // 4 * 2
        assert self.off + w <= self.top, ("arena overflow", self.off, w, self.top)
        a = self.ap[:, self.off:self.off + w].bitcast(BF16)[:, 0:n]
        self.off += w
        return a

    def i32(self, n):
        return self.f32(n).bitcast(I32)


def v3(ap, a, b):
    return ap.rearrange("p (a b) -> p a b", a=a, b=b)


def v4(ap, a, b, c):
    return ap.rearrange("p (a b c) -> p a b c", a=a, b=b, c=c)


def build_program(dbg=False, stop_after=None):
    nc = bass.Bass("TRN2", target_bir_lowering=False)
    P = Prog(nc)

    def din(name, shape, dt=F32):
        return nc.dram_tensor(name, list(shape), dt, kind="ExternalInput").ap()

    def dscr(name, shape, dt, out=False):
        if out:
            return nc.dram_tensor(name, list(shape), dt, kind="ExternalOutput").ap()
        return nc.dram_tensor(name, list(shape), dt).ap()

    x_d = din("x", [TPC, D])
    posT_d = din("posT", [128, NT], I32)
    nmix_d = din("norm_mix", [2, D])
    nffn_d = din("norm_ffn", [2, D])
    nfin_d = din("norm_final", [1, D])
    awin_d = din("attn_w_in", [D, 4608])
    awout_d = din("attn_w_out", [512, D])
    pwin_d = din("pool_w_in", [D, D])
    pwg_d = din("pool_w_group", [4, 256, 256])
    pscT_d = din("pool_scaleT", [128, 8])
    pwout_d = din("pool_w_out", [D, D])
    rgw_d = din("router_group_w", [2, D, 4])
    rgb_d = din("router_group_b", [2, 4])
    rew_d = din("router_expert_w", [2, 4, D, 4])
    reb_d = din("router_expert_b", [2, 16])
    w1_d = din("expert_w1", [2, NEXP, D, 512])
    w3_d = din("expert_w3", [2, NEXP, D, 512])
    w2_d = din("expert_w2", [2, NEXP, 512, D])
    c_identf_d = din("c_identf", [128, 128])
    c_mask_d = din("c_mask", [128, 512])
    c_triu_d = din("c_triu", [128, 128])
    c_ones_d = din("c_ones", [128, 128])
    c_invf_d = din("c_invf", [128, 8])
    c_eoff_d = din("c_eoff", [128, 16])
    c_rc_d = din("c_rc", [128, 16])
    c_tokp1_d = din("c_tokp1", [128, NT])
    c_slot_d = din("c_slot", [128, NEXP * CAP_T])
    out_d = nc.dram_tensor("out", [TPC, D], F32, kind="ExternalOutput").ap()

    H1 = dscr("H1", [TPC, D], F32, out=dbg)
    H2 = dscr("H2", [TPC, D], F32, out=dbg)
    H3 = dscr("H3", [TPC, D], F32, out=dbg)
    OGZ = [dscr(f"OGZ{g}", [TPC, 264], F32) for g in range(3)]
    XS = dscr("XS", [NEXP * CAP, 516], F32)
    YK = dscr("YK", [2 * TPC, D], BF16)
    bH = {id(h): P.bufs(NT) for h in (H1, H2, H3)}
    bOGZ = [P.bufs(NT) for _ in range(3)]
    bXS = P.buf()
    bYK = P.buf()
    bYKz = P.buf()
    bOUT = P.buf()

    ARENA_N = 46000
    arena_t = P.es.enter_context(nc.sbuf_tensor("arena", [128, ARENA_N], F32))
    A = Arena(arena_t, ARENA_N)
    ps_t = P.es.enter_context(nc.psum_tensor("ps", [128, 4096], F32))

    def bank(i, n=1):
        return ps_t[:, i * 512:(i + n) * 512]

    def bank_bf(i):
        return ps_t[:, i * 512:(i + 1) * 512].bitcast(BF16)

    pb = P.bufs(8)
    PF = {}
    for b_ in pb:
        b_.excl = True
    _bc = {}

    def bc_reg(e, val=None):
        val = NEXP * CAP - 1 if val is None else val
        if val not in _bc:
            _bc[val] = e.to_reg(val)
        return _bc[val]

    identf = A.f32(128)
    identb = A.bf16(128)
    maskb = A.bf16(512)
    bconst = P.buf()
    P.dma("sp", lambda e: e.dma_start(out=identf, in_=c_identf_d), writes=[bconst])
    zt = A.f32(516)
    bzt = P.buf()
    bXSz = P.buf()
    tmpm = zt[:, 0:512]
    P.dma("sp", lambda e: e.dma_start(out=tmpm, in_=c_mask_d), writes=[bzt])
    P.op("dve", lambda e: e.tensor_copy(out=identb, in_=identf), reads=[bconst], writes=[bconst])
    P.op("dve", lambda e: e.tensor_copy(out=maskb, in_=tmpm), reads=[bconst, bzt], writes=[bconst])
    P.op("pool", lambda e: e.memset(zt, 0.0), writes=[bzt])
    XSv = XS.rearrange("(n p) d -> n p d", p=128)
    zero_state = {"jobs": []}

    def xs_zero_begin():
        zero_state["jobs"] = [ex_ * CAP_T + j_ for ex_ in range(NEXP) for j_ in range(CAP_T) if j_ >= ZERO_FROM_TILE]

    def xs_zero_some(n, after=()):
        for _ in range(n):
            if zero_state["jobs"]:
                n_ = zero_state["jobs"].pop(0)
                P.dma("sp", lambda e, n_=n_: e.dma_start(out=XSv[n_], in_=zt), reads=[bzt, bXS] + list(after), writes=[bXSz], c=4.0)
    persist_mark = A.mark()

    def rmsnorm_tile(xt, bx, gbt, bg, outs, junk, bjunk, ss, rs, bss):
        ss = ss[:, 0:1]
        rs = rs[:, 0:1]
        P.op("act", lambda e: e.activation(out=junk, in_=xt, func=AF.Square, accum_out=ss),
             reads=[bx], writes=[bjunk, bss])
        P.op("act", lambda e: e.activation(out=rs, in_=ss, func=AF.Sqrt, scale=1.0 / D, bias=EPS),
             reads=[bss], writes=[bss])
        P.op("dve", lambda e: e.reciprocal(out=rs, in_=rs), reads=[bss], writes=[bss])
        for (o_ap, o_b) in outs:
            P.op("dve", lambda e, o_ap=o_ap: e.scalar_tensor_tensor(
                out=o_ap, in0=xt, scalar=rs[:, 0:1], in1=gbt, op0=ALU.mult, op1=ALU.mult),
                reads=[bx, bss, bg], writes=[o_b])

    def seq_norm_transpose(src, bsrc_tiles, s, gbt, bg, yT, byT, tl):
        for t in range(16):
            gt = s * 16 + t
            xt, bx = tl["xt"][t % 2], tl["bxt"][t % 2]
            yb, byb = tl["yb"][t % 2], tl["byb"][t % 2]
            P.dma("sp", lambda e, xt=xt, gt=gt: e.dma_start(out=xt, in_=src[gt * 128:(gt + 1) * 128, :]),
                  reads=[bsrc_tiles[gt]], writes=[bx])
            rmsnorm_tile(xt, bx, gbt, bg, [(yb, byb)], tl["junk"], tl["bjunk"], tl["ss"], tl["rs"], tl["bss"])
            bk = 4 + (t % 2)
            pbf = bank_bf(bk)
            for k in range(8):
                P.op("pe", lambda e, k=k, pbf=pbf, yb=yb: e.transpose(
                    out=pbf[:, k * 128:(k + 1) * 128], in_=yb[:, k * 128:(k + 1) * 128], identity=identb),
                    reads=[byb, bconst], writes=[pb[bk]])
            eng = "act" if t % 2 == 0 else "dve"
            if eng == "act":
                P.op("act", lambda e, pbf=pbf, t=t: e.copy(out=yT[:, :, t * 128:(t + 1) * 128], in_=v3(pbf, 8, 128)),
                     reads=[pb[bk]], writes=[byT[t]])
            else:
                P.op("dve", lambda e, pbf=pbf, t=t: e.tensor_copy(out=yT[:, :, t * 128:(t + 1) * 128], in_=v3(pbf, 8, 128)),
                     reads=[pb[bk]], writes=[byT[t]])

    def attn_phase():
        A.release(persist_mark)
        gbt = A.f32(D)
        bg = P.buf()
        P.dma("sp", lambda e: e.dma_start(out=gbt, in_=nmix_d[0:1, :].partition_broadcast(128)), writes=[bg])
        yT = v3(A.bf16(8 * SEQ), 8, SEQ)
        byT = P.bufs(16)
        wg = v3(A.bf16(8 * 1536), 8, 1536)
        bwg = P.buf()
        qT = v3(A.bf16(4 * SEQ), 4, SEQ)
        kT = v3(A.bf16(4 * SEQ), 4, SEQ)
        bqT, bkT = P.buf(), P.buf()
        Va = v4(A.bf16(16 * 520), 16, 8, 65)
        bVa = P.bufs(16)
        bVa1 = P.buf()
        wout = v3(A.bf16(4 * D), 4, D)
        bwout = P.buf()
        tl = dict(xt=[A.f32(D), A.f32(D)], bxt=P.bufs(2), yb=[A.bf16(D), A.bf16(D)], byb=P.bufs(2),
                  junk=A.f32(D), bjunk=P.buf(), ss=A.f32(2), rs=A.f32(2), bss=P.buf())
        qk = [A.bf16(D), A.bf16(D)]
        bqk = P.bufs(2)
        NE = ATT_CFG[0]
        Eb = [v4(A.bf16(4 * 512), 4, 2, 256) for _ in range(NE)]
        bE = [P.bufs(4) for _ in range(NE)]
        OZ = [A.f32(264) for _ in range(3)]
        bOZ = P.bufs(3)
        posi = A.i32(NT)
        posf = A.f32(NT)
        invf = A.f32(8)
        ang = A.f32(NT * 8)
        a2 = A.f32(NT * 8)
        nf = A.f32(NT * 8)
        ni = A.i32(NT * 8)
        mk = A.f32(NT * 8)
        cosT = v3(A.f32(NT * 8), NT, 8)
        sinT = v3(A.f32(NT * 8), NT, 8)
        brot = P.buf()
        rt = [A.f32(128) for _ in range(4)]
        brt = P.bufs(4)
        ozl = [[A.f32(264) for _ in range(3)] for _ in range(2)]
        bozl = [P.bufs(3) for _ in range(2)]
        zs = A.f32(8)
        us = A.f32(512)
        bmg = P.buf()
        ob = A.bf16(512)
        bob = P.buf()
        oT = v3(A.bf16(4 * 128), 4, 128)
        boT = P.buf()
        res = [tl["junk"], A.f32(D)]
        bres = [tl["bjunk"], P.buf()]

        P.dma("sp", lambda e: e.dma_start(out=posi, in_=posT_d), writes=[brot])
        P.dma("sp", lambda e: e.dma_start(out=invf, in_=c_invf_d), writes=[brot])
        P.op("dve", lambda e: e.tensor_copy(out=posf, in_=posi), reads=[brot], writes=[brot])
        P.op("dve", lambda e: e.tensor_tensor(
            out=v3(ang, NT, 8), in0=posf.unsqueeze(2).to_broadcast([128, NT, 8]),
            in1=invf.unsqueeze(1).to_broadcast([128, NT, 8]), op=ALU.mult), reads=[brot], writes=[brot])
        TWO_PI = 2.0 * math.pi
        C1 = 6.28125
        C2 = TWO_PI - C1
        for (tab, shift) in ((sinT, 0.0), (cosT, 0.5 * math.pi)):
            tabf = tab.rearrange("p a b -> p (a b)")
            P.op("dve", lambda e, shift=shift: e.tensor_scalar(out=a2, in0=ang, scalar1=shift, scalar2=None, op0=ALU.add),
                 reads=[brot], writes=[brot])
            P.op("dve", lambda e: e.tensor_scalar(out=ni, in0=a2, scalar1=1.0 / TWO_PI, scalar2=None, op0=ALU.mult),
                 reads=[brot], writes=[brot])
            P.op("dve", lambda e: e.tensor_copy(out=nf, in_=ni), reads=[brot], writes=[brot])
            P.op("dve", lambda e: e.scalar_tensor_tensor(out=a2, in0=nf, scalar=-C1, in1=a2, op0=ALU.mult, op1=ALU.add),
                 reads=[brot], writes=[brot])
            P.op("dve", lambda e: e.scalar_tensor_tensor(out=a2, in0=nf, scalar=-C2, in1=a2, op0=ALU.mult, op1=ALU.add),
                 reads=[brot], writes=[brot])
            P.op("dve", lambda e: e.tensor_scalar(out=mk, in0=a2, scalar1=math.pi, scalar2=None, op0=ALU.is_gt),
                 reads=[brot], writes=[brot])
            P.op("dve", lambda e: e.scalar_tensor_tensor(out=a2, in0=mk, scalar=-TWO_PI, in1=a2, op0=ALU.mult, op1=ALU.add),
                 reads=[brot], writes=[brot])
            P.op("dve", lambda e: e.tensor_scalar(out=mk, in0=a2, scalar1=-math.pi, scalar2=None, op0=ALU.is_lt),
                 reads=[brot], writes=[brot])
            P.op("dve", lambda e: e.scalar_tensor_tensor(out=a2, in0=mk, scalar=TWO_PI, in1=a2, op0=ALU.mult, op1=ALU.add),
                 reads=[brot], writes=[brot])
            P.op("dve", lambda e: e.tensor_scalar(out=a2, in0=a2, scalar1=math.pi, scalar2=-math.pi, op0=ALU.min, op1=ALU.max),
                 reads=[brot], writes=[brot])
            P.op("act", lambda e, tabf=tabf: e.activation(out=tabf, in_=a2, func=AF.Sin), reads=[brot], writes=[brot])

        if CUT[0] == 1:
            P.barrier()
            return
        P.op("pool", lambda e: e.memset(Va[:, :, :, 64:65], 1.0), writes=[bVa1])
        P.dma("pool", lambda e: e.dma_start(out=wout, in_=awout_d.rearrange("(k p) n -> p k n", p=128)), writes=[bwout])
        for i in range(3):
            P.op("pool", lambda e, i=i: e.memset(OZ[i], 0.0), writes=[bOZ[i]])

        x_tiles = [P.buf() for _ in range(NT)]
        xs_zero_begin()
        ei = 0
        ozi = 0
        for s in range(2):
            seq_norm_transpose(x_d, x_tiles, s, gbt, bg, yT, byT, tl)
            if CUT[0] == 2:
                P.barrier()
                return
            for g in range(3):
                d = DILS[g]
                nb = 16 // d
                P.dma("pool", lambda e, g=g: e.dma_start(
                    out=wg, in_=awin_d[:, g * 1536:(g + 1) * 1536].rearrange("(k p) n -> p k n", p=128)),
                    writes=[bwg])
                if CUT[0] == 31:
                    P.barrier()
                    return
                for t in range(16):
                    gt = s * 16 + t
                    if CUT[0] in (32, 33, 34) and t == 1:
                        P.barrier()
                        return
                    b0 = 0 if t % 2 == 0 else 2
                    for j in range(2):
                        for k in range(8):
                            P.op("pe", lambda e, j=j, k=k, b0=b0, t=t: e.matmul(
                                bank(b0 + j), lhsT=yT[:, k, t * 128:(t + 1) * 128], rhs=wg[:, k, j * 512:(j + 1) * 512],
                                start=(k == 0), stop=(k == 7)), reads=[byT[t], bwg], writes=[pb[b0 + j]])
                    qkt, bq = qk[t % 2], bqk[t % 2]
                    xs_zero_some(1, after=[bq])
                    psq = bank(b0, 2)
                    P.op("act", lambda e, qkt=qkt, psq=psq: e.copy(out=qkt, in_=psq),
                         reads=[pb[b0], pb[b0 + 1]], writes=[bq])
                    if CUT[0] == 32:
                        continue
                    psv = v3(psq, 16, 64)
                    qkv = v3(qkt, 16, 64)
                    cb = cosT[:, gt:gt + 1, :].to_broadcast([128, 16, 8])
                    sb = sinT[:, gt:gt + 1, :].to_broadcast([128, 16, 8])
                    t1 = psv[:, :, 0:8]
                    t2 = psv[:, :, 8:16]
                    r = [v3(x_, 16, 8) for x_ in rt]
                    rd = [pb[b0], pb[b0 + 1], brot]
                    P.op("dve", lambda e, t1=t1, cb=cb, r=r: e.tensor_tensor(out=r[0], in0=t1, in1=cb, op=ALU.mult), reads=rd, writes=[brt[0]])
                    P.op("dve", lambda e, t2=t2, sb=sb, r=r: e.tensor_tensor(out=r[1], in0=t2, in1=sb, op=ALU.mult), reads=rd, writes=[brt[1]])
                    P.op("dve", lambda e, t2=t2, cb=cb, r=r: e.tensor_tensor(out=r[2], in0=t2, in1=cb, op=ALU.mult), reads=rd, writes=[brt[2]])
                    P.op("dve", lambda e, t1=t1, sb=sb, r=r: e.tensor_tensor(out=r[3], in0=t1, in1=sb, op=ALU.mult), reads=rd, writes=[brt[3]])
                    P.op("dve", lambda e, qkv=qkv, r=r: e.tensor_tensor(out=qkv[:, :, 0:8], in0=r[0], in1=r[1], op=ALU.subtract),
                         reads=[brt[0], brt[1]], writes=[bq])
                    P.op("dve", lambda e, qkv=qkv, r=r: e.tensor_tensor(out=qkv[:, :, 8:16], in0=r[2], in1=r[3], op=ALU.add),
                         reads=[brt[2], brt[3]], writes=[bq])
                    if CUT[0] == 33:
                        continue
                    bk = 4 + (t % 2)
                    pbf = bank_bf(bk)
                    for c in range(8):
                        P.op("pe", lambda e, c=c, pbf=pbf, qkt=qkt: e.transpose(
                            out=pbf[:, c * 128:(c + 1) * 128], in_=qkt[:, c * 128:(c + 1) * 128], identity=identb),
                            reads=[bq, bconst], writes=[pb[bk]])
                    P.op("act", lambda e, pbf=pbf, t=t: e.copy(out=qT[:, :, t * 128:(t + 1) * 128], in_=v3(pbf[:, 0:512], 4, 128)),
                         reads=[pb[bk]], writes=[bqT])
                    P.op("dve", lambda e, pbf=pbf, t=t: e.tensor_copy(out=kT[:, :, t * 128:(t + 1) * 128], in_=v3(pbf[:, 512:1024], 4, 128)),
                         reads=[pb[bk]], writes=[bkT])
                if CUT[0] == 3:
                    P.barrier()
                    return
                for blk in range(16):
                    ph, b = blk // nb, blk % nb
                    st = b * 128 * d + ph
                    bk = 6 + (blk % 2)
                    for k in range(8):
                        P.op("pe", lambda e, k=k, st=st, d=d, bk=bk: e.matmul(
                            bank(bk), lhsT=yT[:, k, st:st + 127 * d + 1:d], rhs=wg[:, k, 1024:1536],
                            start=(k == 0), stop=(k == 7)), reads=byT[st // 128:(st + 127 * d) // 128 + 1] + [bwg], writes=[pb[bk]])
                    eng = "act" if blk % 2 == 0 else "dve"
                    if eng == "act":
                        P.op("act", lambda e, blk=blk, bk=bk: e.copy(out=Va[:, blk, :, 0:64], in_=v3(bank(bk), 8, 64)),
                             reads=[pb[bk]], writes=[bVa[blk]])
                    else:
                        P.op("dve", lambda e, blk=blk, bk=bk: e.tensor_copy(out=Va[:, blk, :, 0:64], in_=v3(bank(bk), 8, 64)),
                             reads=[pb[bk]], writes=[bVa[blk]])
                if CUT[0] == 4:
                    P.barrier()
                    return
                sbi = 0
                pvi = 0
                for ph in range(d):
                    prevE = None
                    for b in range(nb):
                        blk = ph * nb + b
                        nq = 256 if b + 1 < nb else 128
                        st = b * 128 * d + ph
                        Ec, bEc = Eb[ei % NE], bE[ei % NE]
                        ei += 1
                        for c in range(4):
                            sbk = 2 * (sbi % ATT_CFG[1])
                            sbi += 1
                            for hh in range(2):
                                P.op("pe", lambda e, c=c, hh=hh, st=st, d=d, nq=nq, sbk=sbk: e.matmul(
                                    bank(sbk + hh)[:, 0:nq],
                                    lhsT=kT[hh * 64:(hh + 1) * 64, c, st:st + 127 * d + 1:d],
                                    rhs=qT[hh * 64:(hh + 1) * 64, c, st:st + (nq - 1) * d + 1:d],
                                    start=True, stop=True), reads=[bqT, bkT], writes=[pb[sbk + hh]])
                            P.op("act", lambda e, Ec=Ec, c=c, nq=nq, sbk=sbk: e.activation(
                                out=Ec[:, c, :, 0:nq], in_=v3(bank(sbk, 2), 2, 512)[:, :, 0:nq], func=AF.Exp, scale=0.125),
                                reads=[pb[sbk], pb[sbk + 1]], writes=[bEc[c]])
                            P.op("dve", lambda e, Ec=Ec, c=c, nq=nq: e.tensor_tensor(
                                out=Ec[:, c, :, 0:nq], in0=Ec[:, c, :, 0:nq], in1=v3(maskb, 2, 256)[:, :, 0:nq], op=ALU.mult),
                                reads=[bEc[c], bconst], writes=[bEc[c]])
                        pvb = (4 + 2 * (pvi % 2)) if ATT_CFG[1] == 2 else 6
                        pvi += 1
                        for h in range(8):
                            c, hh = h // 2, h % 2
                            o_ap = bank(pvb + h // 4)[:, (h % 4) * 65:(h % 4) * 65 + 65]
                            if b > 0:
                                Ep, bEp = prevE
                                P.op("pe", lambda e, o_ap=o_ap, Ep=Ep, c=c, hh=hh, blk=blk, h=h: e.matmul(
                                    o_ap, lhsT=Ep[:, c, hh, 128:256], rhs=Va[:, blk - 1, h, :], start=True, stop=False),
                                    reads=[bEp[c], bVa[blk - 1], bVa1], writes=[pb[pvb + h // 4]])
                            P.op("pe", lambda e, o_ap=o_ap, Ec=Ec, c=c, hh=hh, blk=blk, h=h, b=b: e.matmul(
                                o_ap, lhsT=Ec[:, c, hh, 0:128], rhs=Va[:, blk, h, :], start=(b == 0), stop=True),
                                reads=[bEc[c], bVa[blk], bVa1], writes=[pb[pvb + h // 4]])
                        prevE = (Ec, bEc)
                        oz, boz = OZ[ozi % 3], bOZ[ozi % 3]
                        ozi += 1
                        pv = v3(bank(pvb, 2), 2, 512)[:, :, 0:260].rearrange("p a (h e) -> p a h e", h=4, e=65)
                        P.op("act", lambda e, oz=oz, pv=pv: e.copy(
                            out=v4(oz[:, 0:256].bitcast(BF16), 2, 4, 64), in_=pv[:, :, :, 0:64]),
                            reads=[pb[pvb], pb[pvb + 1]], writes=[boz])
                        P.op("dve", lambda e, oz=oz, pv=pv: e.tensor_copy(
                            out=v4(oz[:, 256:264], 2, 4, 1), in_=pv[:, :, :, 64:65]),
                            reads=[pb[pvb], pb[pvb + 1]], writes=[boz])
                        r0 = s * SEQ + st
                        touched = sorted({(r0 + d * i) // 128 for i in (0, 127)})
                        tb = [bOGZ[g][ti] for ti in range(touched[0], touched[-1] + 1)]
                        P.dma("sp", lambda e, oz=oz, g=g, r0=r0, d=d: e.dma_start(
                            out=OGZ[g][r0:r0 + 127 * d + 1:d, :], in_=oz), reads=[boz], writes=tb)
                if CUT[0] == 5:
                    P.barrier()
                    return
            if CUT[0] == 6:
                P.barrier()
                return
            for t in range(16):
                gt = s * 16 + t
                ol, bol = ozl[t % 2], bozl[t % 2]
                for g in range(3):
                    P.dma("sp", lambda e, g=g, ol=ol, gt=gt: e.dma_start(out=ol[g], in_=OGZ[g][gt * 128:(gt + 1) * 128, :]),
                          reads=[bOGZ[g][gt]], writes=[bol[g]])
                zv = [ol[g][:, 256:264] for g in range(3)]
                uv = [ol[g][:, 0:256].bitcast(BF16) for g in range(3)]
                P.op("dve", lambda e, zv=zv: e.tensor_tensor(out=zs, in0=zv[0], in1=zv[1], op=ALU.add), reads=[bol[0], bol[1]], writes=[bmg])
                P.op("dve", lambda e, zv=zv: e.tensor_tensor(out=zs, in0=zs, in1=zv[2], op=ALU.add), reads=[bol[2], bmg], writes=[bmg])
                P.op("dve", lambda e: e.reciprocal(out=zs, in_=zs), reads=[bmg], writes=[bmg])
                P.op("dve", lambda e, uv=uv: e.tensor_tensor(out=us, in0=uv[0], in1=uv[1], op=ALU.add), reads=[bol[0], bol[1], bmg], writes=[bmg])
                P.op("dve", lambda e, uv=uv: e.tensor_tensor(out=us, in0=us, in1=uv[2], op=ALU.add), reads=[bol[2], bmg], writes=[bmg])
                P.op("dve", lambda e: e.tensor_tensor(out=v3(ob, 8, 64), in0=v3(us, 8, 64),
                                                      in1=zs.unsqueeze(2).to_broadcast([128, 8, 64]), op=ALU.mult),
                     reads=[bmg], writes=[bob])
                bk = 4 + (t % 2)
                pbf = bank_bf(bk)
                for k in range(4):
                    P.op("pe", lambda e, k=k, pbf=pbf: e.transpose(out=pbf[:, k * 128:(k + 1) * 128], in_=ob[:, k * 128:(k + 1) * 128], identity=identb),
                         reads=[bob, bconst], writes=[pb[bk]])
                P.op("act", lambda e, pbf=pbf: e.copy(out=oT, in_=v3(pbf[:, 0:512], 4, 128)), reads=[pb[bk]], writes=[boT])
                b0 = 0 if t % 2 == 0 else 2
                for hf in range(2):
                    for k in range(4):
                        P.op("pe", lambda e, hf=hf, k=k, b0=b0: e.matmul(
                            bank(b0 + hf), lhsT=oT[:, k, :], rhs=wout[:, k, hf * 512:(hf + 1) * 512],
                            start=(k == 0), stop=(k == 3)), reads=[boT, bwout], writes=[pb[b0 + hf]])
                xt, bx = tl["xt"][t % 2], tl["bxt"][t % 2]
                P.dma("sp", lambda e, xt=xt, gt=gt: e.dma_start(out=xt, in_=x_d[gt * 128:(gt + 1) * 128, :]), writes=[bx])
                rs_, brs_ = res[t % 2], bres[t % 2]
                P.op("dve", lambda e, rs_=rs_, xt=xt, b0=b0: e.tensor_tensor(out=rs_, in0=xt, in1=bank(b0, 2), op=ALU.add),
                     reads=[bx, pb[b0], pb[b0 + 1]], writes=[brs_])
                P.dma("sp", lambda e, rs_=rs_, gt=gt: e.dma_start(out=H1[gt * 128:(gt + 1) * 128, :], in_=rs_),
                      reads=[brs_], writes=[bH[id(H1)][gt]])
        P.barrier()

    def moe_phase(li, Hin, Hout, final):
        A.release(persist_mark)
        bHin = bH[id(Hin)]
        gbt = A.f32(D)
        bg = P.buf()
        P.dma("sp", lambda e: e.dma_start(out=gbt, in_=nffn_d[li:li + 1, :].partition_broadcast(128)), writes=[bg])
        idx = A.i32(2 * NT)
        gat = A.f32(2 * NT)
        broute = P.buf()
        m_phase = A.mark()
        A.top_reset()
        w1b = [v3(A.top_bf16(8 * 512), 8, 512), None]
        w3b = [v3(A.top_bf16(8 * 512), 8, 512), None]
        w2b = [v3(A.top_bf16(4 * D), 4, D), None]
        bw = [P.bufs(3) for _ in range(2)]
        pre_w = {}

        def prefetch_expert0(after):
            if pre_w:
                return
            pre_w["done"] = True
            for (wt_, src_, i_) in ((w1b[0], w1_d, 0), (w3b[0], w3_d, 1), (w2b[0], w2_d, 2)):
                P.dma("pool", lambda e, wt_=wt_, src_=src_: e.dma_start(out=wt_, in_=src_[li, 0].rearrange("(k p) n -> p k n", p=128)),
                      reads=list(after), writes=[bw[0][i_]], c=22.0)

        xs_zero_some(1000)
        ybA = v3(A.f32(NT * 516), NT, 516)

        def yb_t(t):
            return ybA[:, t, 0:512].bitcast(BF16)
        bybA = P.bufs(NT)
        wr = v3(A.f32(8 * 20), 8, 20)
        bwr = P.buf()
        rb = A.f32(20)
        triu = A.f32(128)
        onesm = A.f32(128)
        eoff = A.f32(16)
        P.dma("sp", lambda e: e.dma_start(out=wr[:, :, 0:4], in_=rgw_d[li].rearrange("(k p) n -> p k n", p=128)), writes=[bwr])
        for g in range(4):
            P.dma("sp", lambda e, g=g: e.dma_start(out=wr[:, :, 4 + 4 * g:8 + 4 * g],
                                                    in_=rew_d[li, g].rearrange("(k p) n -> p k n", p=128)), writes=[bwr])
        P.dma("sp", lambda e: e.dma_start(out=rb[:, 0:4], in_=rgb_d[li:li + 1, :].partition_broadcast(128)), writes=[bwr])
        P.dma("sp", lambda e: e.dma_start(out=rb[:, 4:20], in_=reb_d[li:li + 1, :].partition_broadcast(128)), writes=[bwr])
        P.dma("sp", lambda e: e.dma_start(out=triu, in_=c_triu_d), writes=[bwr])
        P.dma("sp", lambda e: e.dma_start(out=onesm, in_=c_ones_d), writes=[bwr])
        P.dma("sp", lambda e: e.dma_start(out=eoff, in_=c_eoff_d), writes=[bwr])
        NB3 = 3
        xt2 = [A.f32(D) for _ in range(NB3)]
        bxt2 = P.bufs(NB3)
        yf = [A.f32(D) for _ in range(NB3)]
        byf = P.bufs(NB3)
        junk = A.f32(D)
        bjunk = P.buf()
        ss, rs = A.f32(2), A.f32(2)
        bss = P.buf()
        whl = v3(A.bf16(8 * 40), 8, 40)
        wtmp = v3(A.f32(8 * 20), 8, 20)
        bwhl = P.buf()
        P.op("dve", lambda e: e.tensor_copy(out=whl[:, :, 0:20], in_=wr), reads=[bwr], writes=[bwhl])
        P.op("dve", lambda e: e.tensor_tensor(out=wtmp, in0=wr, in1=whl[:, :, 0:20], op=ALU.subtract), reads=[bwr, bwhl], writes=[bwhl])
        P.op("dve", lambda e: e.tensor_copy(out=whl[:, :, 20:40], in_=wtmp), reads=[bwhl], writes=[bwhl])
        yl = [A.bf16(D) for _ in range(NB3)]
        byl = P.bufs(NB3)
        yhT = [v3(A.bf16(8 * 128), 8, 128) for _ in range(NB3)]
        ylT = [v3(A.bf16(8 * 128), 8, 128) for _ in range(NB3)]
        byhT, bylT = P.bufs(NB3), P.bufs(NB3)
        L = v3(A.f32(NT * 20), NT, 20)
        NG = 4
        GN = NT // NG
        bLg = P.bufs(NG)

        def tile_step(t):
            xt, bx = xt2[t % NB3], bxt2[t % NB3]
            P.dma("sp", lambda e, xt=xt, t=t: e.dma_start(out=xt, in_=Hin[t * 128:(t + 1) * 128, :]),
                  reads=[bHin[t]], writes=[bx])
            yft, byft = yf[t % NB3], byf[t % NB3]
            rmsnorm_tile(xt, bx, gbt, bg, [(yft, byft)], junk, bjunk, ss, rs, bss)
            P.op("act", lambda e, yft=yft, t=t: e.copy(out=yb_t(t), in_=yft), reads=[byft], writes=[bybA[t]], c=1.1)
            if t == 6:
                prefetch_expert0([bybA[t]])
            ylt, bylt = yl[t % NB3], byl[t % NB3]
            P.op("dve", lambda e, yft=yft, ylt=ylt, t=t: e.tensor_tensor(out=ylt, in0=yft, in1=yb_t(t), op=ALU.subtract),
                 reads=[byft, bybA[t]], writes=[bylt], c=1.1)
            b0 = 0 if t % 2 == 0 else 2
            ph_, pl_ = bank_bf(b0), bank_bf(b0 + 1)
            for k in range(8):
                P.op("pe", lambda e, k=k, t=t, ph_=ph_: e.transpose(
                    out=ph_[:, k * 128:(k + 1) * 128], in_=yb_t(t)[:, k * 128:(k + 1) * 128], identity=identb),
                    reads=[bybA[t], bconst], writes=[pb[b0]], c=0.08)
            for k in range(8):
                P.op("pe", lambda e, k=k, ylt=ylt, pl_=pl_: e.transpose(
                    out=pl_[:, k * 128:(k + 1) * 128], in_=ylt[:, k * 128:(k + 1) * 128], identity=identb),
                    reads=[bylt, bconst], writes=[pb[b0 + 1]], c=0.08)
            yh_, byh_ = yhT[t % NB3], byhT[t % NB3]
            yl_, byl_ = ylT[t % NB3], bylT[t % NB3]
            P.op("act", lambda e, yh_=yh_, ph_=ph_: e.copy(out=yh_, in_=v3(ph_, 8, 128)), reads=[pb[b0]], writes=[byh_], c=1.0)
            if t % 2 == 1:
                P.op("act", lambda e, yl_=yl_, pl_=pl_: e.copy(out=yl_, in_=v3(pl_, 8, 128)), reads=[pb[b0 + 1]], writes=[byl_], c=1.0)
            else:
                P.op("dve", lambda e, yl_=yl_, pl_=pl_: e.tensor_copy(out=yl_, in_=v3(pl_, 8, 128)), reads=[pb[b0 + 1]], writes=[byl_], c=1.0)
            lb = 4 + (t // 8) % 2
            c0 = (t % 8) * 60
            for k in range(8):
                P.op("pe", lambda e, k=k, yh_=yh_, lb=lb, c0=c0: e.matmul(
                    bank(lb)[:, c0:c0 + 40], lhsT=yh_[:, k, :], rhs=whl[:, k, :],
                    start=(k == 0), stop=(k == 7)), reads=[byh_, bwhl], writes=[pb[lb]], c=0.08)
            for k in range(8):
                P.op("pe", lambda e, k=k, yl_=yl_, lb=lb, c0=c0: e.matmul(
                    bank(lb)[:, c0 + 40:c0 + 60], lhsT=yl_[:, k, :], rhs=whl[:, k, 0:20],
                    start=(k == 0), stop=(k == 7)), reads=[byl_, bwhl], writes=[pb[lb]], c=0.08)
            if t % 8 == 7:
                hb = t // 8
                pv_ = v3(bank(lb)[:, 0:480], 8, 60)
                Lh = L[:, hb * 8:(hb + 1) * 8, :]
                bL_ = bLg[t // GN]
                P.op("dve", lambda e, pv_=pv_, Lh=Lh: e.tensor_tensor(
                    out=Lh, in0=pv_[:, :, 0:20], in1=rb.unsqueeze(1).to_broadcast([128, 8, 20]), op=ALU.add),
                    reads=[pb[lb], bwr], writes=[bL_])
                P.op("dve", lambda e, pv_=pv_, Lh=Lh: e.tensor_tensor(out=Lh, in0=Lh, in1=pv_[:, :, 20:40], op=ALU.add),
                     reads=[pb[lb], bL_], writes=[bL_])
                P.op("dve", lambda e, pv_=pv_, Lh=Lh: e.tensor_tensor(out=Lh, in0=Lh, in1=pv_[:, :, 40:60], op=ALU.add),
                     reads=[pb[lb], bL_], writes=[bL_])

        def T(n):
            return A.f32(NT * n)
        mg = T(1)
        G = v3(T(4), NT, 4)
        ex4 = v3(T(4), NT, 4)
        se = T(1)
        g1 = T(1)
        tmp16 = v3(T(16), NT, 16)
        sel = v3(T(4), NT, 4)
        m1, m2 = T(1), T(1)
        o1 = v3(T(4), NT, 4)
        o2 = v3(T(4), NT, 4)
        sel2 = v3(T(4), NT, 4)
        dl, exd, w1g, w2g = T(1), T(1), T(1), T(1)
        E1 = v3(T(16), NT, 16)
        E2 = v3(T(16), NT, 16)
        S16 = v3(T(16), NT, 16)
        Bc = [v3(T(16), NT, 16), v3(T(16), NT, 16)]
        rank = v3(T(16), NT, 16)
        valid = v3(T(16), NT, 16)
        pos16 = v3(T(16), NT, 16)
        idf = v3(T(2), 2, NT)
        vld = v3(T(2), 2, NT)
        tokp1 = A.f32(NT)
        btk = P.buf()
        P.dma("sp", lambda e: e.dma_start(out=tokp1, in_=c_tokp1_d), writes=[btk])
        BIG = float(NEXP * CAP + 64)
        idx3 = v3(idx, 2, NT)
        gat3 = v3(gat, 2, NT)
        brg = P.bufs(NG)
        broute_g = P.bufs(NG)
        bexg = P.bufs(NG)
        incs = []

        def route(gi):
            t0, t1 = gi * GN, (gi + 1) * GN
            sl = slice(t0, t1)
            br = brg[gi]
            bL_ = bLg[gi]
            deps_prev = [brg[gi - 1]] if gi > 0 else []

            def dv(fn, reads=(), writes=()):
                P.op("dve", fn, reads=[br, bL_, bwr] + list(reads), writes=[br] + list(writes), c=0.3)

            def bc1(ap1):
                return ap1.unsqueeze(2).to_broadcast([128, GN, 4])

            def g4(ap3):
                return ap3.rearrange("p a (g e) -> p a g e", g=4, e=4)
            lgv = L[:, sl, 0:4]
            lev = L[:, sl, 4:20]
            mg_, se_, g1_, m1_, m2_, dl_, exd_, w1_, w2_ = [x_[:, sl] for x_ in (mg, se, g1, m1, m2, dl, exd, w1g, w2g)]
            G_, ex4_, sel_, o1_, o2_, sel2_ = [x_[:, sl, :] for x_ in (G, ex4, sel, o1, o2, sel2)]
            tmp_, E1_, E2_, S_, rank_, valid_, pos_ = [x_[:, sl, :] for x_ in (tmp16, E1, E2, S16, rank, valid, pos16)]
            B0, B1 = Bc[0][:, sl, :], Bc[1][:, sl, :]
            dv(lambda e: e.tensor_reduce(out=mg_, in_=lgv, axis=AX.X, op=ALU.max))
            dv(lambda e: e.tensor_tensor(out=G_, in0=lgv, in1=bc1(mg_), op=ALU.is_equal))
            dv(lambda e: e.tensor_tensor(out=ex4_, in0=lgv, in1=bc1(mg_), op=ALU.subtract))
            P.op("act", lambda e: e.activation(out=ex4_, in_=ex4_, func=AF.Exp), reads=[br], writes=[br], c=0.3)
            dv(lambda e: e.tensor_reduce(out=se_, in_=ex4_, axis=AX.X, op=ALU.add))
            dv(lambda e: e.reciprocal(out=g1_, in_=se_))
            dv(lambda e: e.tensor_tensor(out=g4(tmp_), in0=g4(lev), in1=G_.unsqueeze(3).to_broadcast([128, GN, 4, 4]), op=ALU.mult))
            dv(lambda e: e.tensor_reduce(out=sel_, in_=tmp_.rearrange("p a (g e) -> p a e g", g=4, e=4), axis=AX.X, op=ALU.add))
            dv(lambda e: e.tensor_reduce(out=m1_, in_=sel_, axis=AX.X, op=ALU.max))
            dv(lambda e: e.tensor_tensor(out=o1_, in0=sel_, in1=bc1(m1_), op=ALU.is_equal))
            dv(lambda e: e.tensor_scalar(out=sel2_, in0=o1_, scalar1=-1e30, scalar2=None, op0=ALU.mult))
            dv(lambda e: e.tensor_tensor(out=sel2_, in0=sel2_, in1=sel_, op=ALU.add))
            dv(lambda e: e.tensor_reduce(out=m2_, in_=sel2_, axis=AX.X, op=ALU.max))
            dv(lambda e: e.tensor_tensor(out=o2_, in0=sel2_, in1=bc1(m2_), op=ALU.is_equal))
            dv(lambda e: e.tensor_tensor(out=dl_, in0=m2_, in1=m1_, op=ALU.subtract))
            P.op("act", lambda e: e.activation(out=exd_, in_=dl_, func=AF.Exp), reads=[br], writes=[br], c=0.3)
            dv(lambda e: e.tensor_scalar(out=w1_, in0=exd_, scalar1=1.0, scalar2=None, op0=ALU.add))
            dv(lambda e: e.reciprocal(out=w1_, in_=w1_))
            dv(lambda e: e.tensor_tensor(out=w2_, in0=exd_, in1=w1_, op=ALU.mult))
            dv(lambda e: e.tensor_tensor(out=w1_, in0=w1_, in1=g1_, op=ALU.mult))
            dv(lambda e: e.tensor_tensor(out=w2_, in0=w2_, in1=g1_, op=ALU.mult))
            for (Ek, ok) in ((E1_, o1_), (E2_, o2_)):
                dv(lambda e, Ek=Ek, ok=ok: e.tensor_tensor(
                    out=g4(Ek), in0=G_.unsqueeze(3).to_broadcast([128, GN, 4, 4]),
                    in1=ok.unsqueeze(2).to_broadcast([128, GN, 4, 4]), op=ALU.mult))
            dv(lambda e: e.tensor_tensor(out=S_, in0=E1_, in1=E2_, op=ALU.add))
            Sf = S16.rearrange("p a b -> p (a b)")[:, t0 * 16:t1 * 16]
            cs = slice(gi * GN * 16, (gi + 1) * GN * 16)
            P.op("pe", lambda e: e.matmul(bank(6)[:, cs], lhsT=triu, rhs=Sf, start=True, stop=True), reads=[br, bwr], writes=[pb[6]])
            P.op("pe", lambda e: e.matmul(bank(7)[:, cs], lhsT=onesm, rhs=Sf, start=True, stop=True), reads=[br, bwr], writes=[pb[7]])
            psA = v3(bank(6)[:, cs], GN, 16)
            psB = v3(bank(7)[:, cs], GN, 16)
            P.op("dve", lambda e: e.tensor_copy(out=B0, in_=psB), reads=[pb[7], br], writes=[br])
            cur = [B0, B1]
            sft = 1
            while sft < GN:
                a_, b_ = cur
                dv(lambda e, a_=a_, b_=b_, sft=sft: e.tensor_copy(out=b_[:, 0:sft, :], in_=a_[:, 0:sft, :]))
                dv(lambda e, a_=a_, b_=b_, sft=sft: e.tensor_tensor(out=b_[:, sft:GN, :], in0=a_[:, sft:GN, :], in1=a_[:, 0:GN - sft, :], op=ALU.add))
                cur = [b_, a_]
                sft *= 2
            inc = cur[0]
            if gi > 0:
                pin = incs[gi - 1]
                dv(lambda e, inc=inc, pin=pin: e.tensor_tensor(out=inc, in0=inc, in1=pin[:, GN - 1:GN, :].to_broadcast([128, GN, 16]), op=ALU.add),
                   reads=deps_prev)
            incs.append(inc)
            P.op("dve", lambda e, inc=inc: e.tensor_tensor(out=rank_, in0=inc, in1=psB, op=ALU.subtract), reads=[pb[7], br], writes=[br])
            P.op("dve", lambda e: e.tensor_tensor(out=rank_, in0=rank_, in1=psA, op=ALU.add), reads=[pb[6], br], writes=[br])
            dv(lambda e: e.tensor_scalar(out=valid_, in0=rank_, scalar1=float(CAP), scalar2=None, op0=ALU.is_lt))
            dv(lambda e: e.tensor_tensor(out=pos_, in0=rank_, in1=eoff.unsqueeze(1).to_broadcast([128, GN, 16]), op=ALU.add))
            dv(lambda e: e.tensor_scalar(out=pos_, in0=pos_, scalar1=-BIG, scalar2=None, op0=ALU.add))
            dv(lambda e: e.tensor_tensor(out=pos_, in0=pos_, in1=valid_, op=ALU.mult))
            dv(lambda e: e.tensor_scalar(out=pos_, in0=pos_, scalar1=BIG, scalar2=None, op0=ALU.add))
            brt = broute_g[gi]
            for k, (Ek, wk) in enumerate(((E1_, w1_), (E2_, w2_))):
                dv(lambda e, Ek=Ek: e.tensor_tensor(out=tmp_, in0=Ek, in1=pos_, op=ALU.mult))
                dv(lambda e, k=k: e.tensor_reduce(out=idf[:, k, sl], in_=tmp_, axis=AX.X, op=ALU.add))
                dv(lambda e, Ek=Ek: e.tensor_tensor(out=tmp_, in0=Ek, in1=valid_, op=ALU.mult))
                dv(lambda e, k=k: e.tensor_reduce(out=vld[:, k, sl], in_=tmp_, axis=AX.X, op=ALU.add))
                P.op("dve", lambda e, k=k, wk=wk: e.tensor_tensor(out=gat3[:, k, sl], in0=wk, in1=vld[:, k, sl], op=ALU.mult),
                     reads=[br], writes=[brt], c=0.3)
            P.op("dve", lambda e: e.tensor_copy(out=idx3[:, :, sl], in_=idf[:, :, sl]), reads=[br], writes=[brt], c=0.3)
            bex = bexg[gi]
            P.op("dve", lambda e: e.tensor_copy(out=ybA[:, sl, 512:513], in_=tokp1[:, sl].unsqueeze(2)), reads=[btk], writes=[bex], c=0.3)
            P.op("dve", lambda e: e.tensor_copy(out=ybA[:, sl, 513:514], in_=gat3[:, 0, sl].unsqueeze(2)), reads=[bex, brt], writes=[bex], c=0.3)
            P.op("dve", lambda e: e.tensor_copy(out=ybA[:, sl, 514:515], in_=gat3[:, 1, sl].unsqueeze(2)), reads=[bex, brt], writes=[bex], c=0.3)
            P.op("dve", lambda e: e.tensor_copy(out=ybA[:, sl, 515:516], in_=idf[:, 1, sl].unsqueeze(2)), reads=[bex, br], writes=[bex], c=0.3)
            for t in range(t0, t1):
                for k in range(2):
                    P.dma("pool", lambda e, t=t, k=k: e.indirect_dma_start(
                        out=XS, out_offset=bass.IndirectOffsetOnAxis(ap=idx[:, k * NT + t:k * NT + t + 1], axis=0),
                        in_=ybA[:, t, :], in_offset=None, bounds_check=bc_reg(e), oob_is_err=False),
                        reads=[bybA[t], brt, bXSz, bex], writes=[bXS], c=5.0)

        for gi in range(NG):
            for t in range(gi * GN, (gi + 1) * GN):
                tile_step(t)
            route(gi)
        P.barrier()
        if CUT[0] == 101:
            return

        A.release(m_phase)
        w1b[1] = v3(A.bf16(8 * 512), 8, 512)
        w3b[1] = v3(A.bf16(8 * 512), 8, 512)
        w2b[1] = v3(A.bf16(4 * D), 4, D)
        NXS = 8
        xs = [A.f32(516) for _ in range(NXS)]
        bxs = P.bufs(NXS)
        xsT = [v3(A.bf16(8 * 128), 8, 128) for _ in range(2)]
        bxsT = P.bufs(2)
        s1 = [A.f32(512) for _ in range(2)]
        bs1 = P.bufs(2)
        hm = [A.bf16(512) for _ in range(2)]
        bhm = P.bufs(2)
        hmT = [v3(A.bf16(4 * 128), 4, 128) for _ in range(2)]
        bhmT = P.bufs(2)
        ys = [A.bf16(D) for _ in range(6)]
        bys = P.bufs(6)
        cslot = A.f32(NEXP * CAP_T)
        bcs = P.buf()
        P.dma("sp", lambda e: e.dma_start(out=cslot, in_=c_slot_d), writes=[bcs])
        BIG2 = 20000.0
        NST = NEXP * CAP_T
        ev = v3(A.f32(NST * 4), NST, 4)
        bev = P.buf()
        for q4 in range(4):
            n0, n1 = q4 * NST // 4, (q4 + 1) * NST // 4
            P.dma("sp", lambda e, n0=n0, n1=n1: e.dma_start(
                out=ev[:, n0:n1, :], in_=XS[n0 * 128:n1 * 128, 512:516].rearrange("(j p) c -> p j c", p=128)),
                reads=[bXS], writes=[bev], c=20.0)
        kf, dg, gate, dest, inv = [A.f32(NST) for _ in range(5)]
        di = A.i32(NST)
        brt_ = P.buf()

        def dv2(fn):
            P.op("dve", fn, reads=[bev, brt_, bcs], writes=[brt_], c=0.3)
        dv2(lambda e: e.tensor_tensor(out=kf, in0=ev[:, :, 3], in1=cslot, op=ALU.is_equal))
        dv2(lambda e: e.tensor_tensor(out=dg, in0=ev[:, :, 2], in1=ev[:, :, 1], op=ALU.subtract))
        dv2(lambda e: e.tensor_tensor(out=dg, in0=dg, in1=kf, op=ALU.mult))
        dv2(lambda e: e.tensor_tensor(out=gate, in0=dg, in1=ev[:, :, 1], op=ALU.add))
        dv2(lambda e: e.scalar_tensor_tensor(out=dest, in0=kf, scalar=float(TPC), in1=ev[:, :, 0], op0=ALU.mult, op1=ALU.add))
        dv2(lambda e: e.tensor_scalar(out=inv, in0=ev[:, :, 0], scalar1=0.0, scalar2=None, op0=ALU.is_equal))
        dv2(lambda e: e.scalar_tensor_tensor(out=dest, in0=inv, scalar=BIG2, in1=dest, op0=ALU.mult, op1=ALU.add))
        dv2(lambda e: e.tensor_scalar(out=dest, in0=dest, scalar1=-1.0, scalar2=None, op0=ALU.add))
        dv2(lambda e: e.tensor_copy(out=di, in_=dest))
        BK = M2_BANKS[M2_LAYOUT[0]]
        yb0 = BK['y']
        it = 0
        for ex in range(NEXP):
            wb_ = ex % 2
            if ex > 0:
                P.dma("pool", lambda e, ex=ex, w_=w1b[wb_]: e.dma_start(out=w_, in_=w1_d[li, ex].rearrange("(k p) n -> p k n", p=128)), writes=[bw[wb_][0]], c=22.0)
                P.dma("pool", lambda e, ex=ex, w_=w3b[wb_]: e.dma_start(out=w_, in_=w3_d[li, ex].rearrange("(k p) n -> p k n", p=128)), writes=[bw[wb_][1]], c=22.0)
                P.dma("pool", lambda e, ex=ex, w_=w2b[wb_]: e.dma_start(out=w_, in_=w2_d[li, ex].rearrange("(k p) n -> p k n", p=128)), writes=[bw[wb_][2]], c=22.0)
            for j in range(CAP_T):
                r0 = ex * CAP + j * 128
                x_, bx_ = xs[it % NXS], bxs[it % NXS]
                P.dma("sp", lambda e, x_=x_, r0=r0: e.dma_start(out=x_, in_=XS[r0:r0 + 128, :]), reads=[bXS], writes=[bx_])
                xb_ = x_[:, 0:512].bitcast(BF16)
                tbx = BK['xt'][it % len(BK['xt'])]
                pbf = bank_bf(tbx)
                for k in range(8):
                    P.op("pe", lambda e, k=k, pbf=pbf, xb_=xb_: e.transpose(out=pbf[:, k * 128:(k + 1) * 128], in_=xb_[:, k * 128:(k + 1) * 128], identity=identb),
                         reads=[bx_, bconst], writes=[pb[tbx]], c=0.08)
                xT_, bxT_ = xsT[it % 2], bxsT[it % 2]
                P.op("act", lambda e, xT_=xT_, pbf=pbf: e.copy(out=xT_, in_=v3(pbf, 8, 128)), reads=[pb[tbx]], writes=[bxT_], c=1.0)
                hb = BK['h'][it % len(BK['h'])]
                for (wi, wt, bk) in ((0, w1b[wb_], hb), (1, w3b[wb_], hb + 1)):
                    for k in range(8):
                        P.op("pe", lambda e, k=k, wt=wt, bk=bk, xT_=xT_: e.matmul(bank(bk), lhsT=xT_[:, k, :], rhs=wt[:, k, :], start=(k == 0), stop=(k == 7)),
                             reads=[bxT_, bw[wb_][wi]], writes=[pb[bk]])
                s1_, bs1_ = s1[it % 2], bs1[it % 2]
                P.op("act", lambda e, s1_=s1_, hb=hb: e.activation(out=s1_, in_=bank(hb), func=AF.Silu), reads=[pb[hb]], writes=[bs1_])
                hm_, bhm_ = hm[it % 2], bhm[it % 2]
                P.op("dve", lambda e, hm_=hm_, s1_=s1_, hb=hb: e.tensor_tensor(out=hm_, in0=s1_, in1=bank(hb + 1), op=ALU.mult), reads=[bs1_, pb[hb + 1]], writes=[bhm_])
                tbh = BK['ht'][it % len(BK['ht'])]
                pbf2 = bank_bf(tbh)
                for k in range(4):
                    P.op("pe", lambda e, k=k, pbf2=pbf2, hm_=hm_: e.transpose(out=pbf2[:, k * 128:(k + 1) * 128], in_=hm_[:, k * 128:(k + 1) * 128], identity=identb),
                         reads=[bhm_, bconst], writes=[pb[tbh]], c=0.08)
                hT_, bhT_ = hmT[it % 2], bhmT[it % 2]
                P.op("dve", lambda e, hT_=hT_, pbf2=pbf2: e.tensor_copy(out=hT_, in_=v3(pbf2[:, 0:512], 4, 128)), reads=[pb[tbh]], writes=[bhT_])
                for hf in range(2):
                    for k in range(4):
                        P.op("pe", lambda e, k=k, hf=hf, hT_=hT_, w2_=w2b[wb_]: e.matmul(bank(yb0 + hf), lhsT=hT_[:, k, :], rhs=w2_[:, k, hf * 512:(hf + 1) * 512],
                                                                      start=(k == 0), stop=(k == 3)),
                             reads=[bhT_, bw[wb_][2]], writes=[pb[yb0 + hf]])
                ys_, bys_ = ys[it % 6], bys[it % 6]
                P.op("act", lambda e, ys_=ys_, it=it: e.activation(out=ys_, in_=bank(yb0, 2), func=AF.Copy, scale=gate[:, it:it + 1]),
                     reads=[pb[yb0], pb[yb0 + 1], brt_], writes=[bys_], c=1.1)
                P.dma("pool", lambda e, ys_=ys_, it=it: e.indirect_dma_start(
                    out=YK, out_offset=bass.IndirectOffsetOnAxis(ap=di[:, it:it + 1], axis=0),
                    in_=ys_, in_offset=None, bounds_check=bc_reg(e, 2 * TPC - 1), oob_is_err=False),
                    reads=[bys_, brt_], writes=[bYK], c=5.0)
                it += 1
        P.barrier()
        if CUT[0] == 102:
            return

        A.release(m_phase)
        A.top_reset()
        if not final:
            pw = dict(win=v3(A.top_bf16(8 * D), 8, D), wou=v3(A.top_bf16(8 * D), 8, D), wgp=v4(A.top_bf16(4 * 2 * 256), 4, 2, 256),
                      bwin=P.buf(), bwou=P.buf(), bwgp=P.buf())
            PF["pool_w"] = pw
            P.dma("pool", lambda e: e.dma_start(out=pw["win"], in_=pwin_d.rearrange("(k p) n -> p k n", p=128)), writes=[pw["bwin"]], c=15.0)
            P.dma("pool", lambda e: e.dma_start(out=pw["wou"], in_=pwout_d.rearrange("(k p) n -> p k n", p=128)), writes=[pw["bwou"]], c=15.0)
            for g in range(4):
                P.dma("pool", lambda e, g=g: e.dma_start(out=pw["wgp"][:, g, :, :], in_=pwg_d[g].rearrange("(k p) n -> p k n", p=128)), writes=[pw["bwgp"]])
        y1 = [A.bf16(D) for _ in range(2)]
        y2 = [A.bf16(D) for _ in range(2)]
        by1, by2 = P.bufs(2), P.bufs(2)
        ht = [A.f32(D) for _ in range(2)]
        bht = P.bufs(2)
        acc = [A.f32(D) for _ in range(2)]
        bacc = P.bufs(2)
        hn = [A.f32(D) for _ in range(2)]
        bhn = P.bufs(2)
        gft = None
        if final:
            gft = A.f32(D)
            bgf = P.buf()
            P.dma("sp", lambda e: e.dma_start(out=gft, in_=nfin_d[0:1, :].partition_broadcast(128)), writes=[bgf])
            junk = A.f32(D)
            bjunk = P.buf()
            ss, rs = A.f32(2), A.f32(2)
            bss = P.buf()
            fo = [A.f32(D) for _ in range(2)]
            bfo = P.bufs(2)
        for t in range(NT):
            i = t % 2
            P.dma("sp", lambda e, t=t, i=i: e.dma_start(out=y1[i], in_=YK[t * 128:(t + 1) * 128, :]), reads=[bYK], writes=[by1[i]])
            P.dma("sp", lambda e, t=t, i=i: e.dma_start(out=y2[i], in_=YK[TPC + t * 128:TPC + (t + 1) * 128, :]), reads=[bYK], writes=[by2[i]])
            P.dma("sp", lambda e, t=t, i=i: e.dma_start(out=ht[i], in_=Hin[t * 128:(t + 1) * 128, :]), reads=[bHin[t]], writes=[bht[i]])
            P.op("dve", lambda e, i=i: e.tensor_tensor(out=acc[i], in0=ht[i], in1=y1[i], op=ALU.add),
                 reads=[by1[i], bht[i]], writes=[bacc[i]], c=1.1)
            P.op("dve", lambda e, i=i: e.tensor_tensor(out=hn[i], in0=acc[i], in1=y2[i], op=ALU.add),
                 reads=[by2[i], bacc[i]], writes=[bhn[i]], c=1.1)
            if final:
                rmsnorm_tile(hn[i], bhn[i], gft, bgf, [(fo[i], bfo[i])], junk, bjunk, ss, rs, bss)
                P.dma("sp", lambda e, t=t, i=i: e.dma_start(out=out_d[t * 128:(t + 1) * 128, :], in_=fo[i]), reads=[bfo[i]], writes=[bOUT])
            else:
                P.dma("sp", lambda e, t=t, i=i: e.dma_start(out=Hout[t * 128:(t + 1) * 128, :], in_=hn[i]), reads=[bhn[i]], writes=[bH[id(Hout)][t]])
        P.barrier()

    def pool_phase():
        A.release(persist_mark)
        gbt = A.f32(D)
        bg = P.buf()
        P.dma("sp", lambda e: e.dma_start(out=gbt, in_=nmix_d[1:2, :].partition_broadcast(128)), writes=[bg])
        yT = v3(A.bf16(8 * SEQ), 8, SEQ)
        byT = P.bufs(16)
        zT = v3(A.bf16(8 * SEQ), 8, SEQ)
        bzT = P.buf()
        plT = v3(A.bf16(4 * SEQ), 4, SEQ)
        bplT = P.bufs(4)
        pw = PF["pool_w"]
        win, wou, wgp = pw["win"], pw["wou"], pw["wgp"]
        bwin, bwou, bwgp = pw["bwin"], pw["bwou"], pw["bwgp"]
        psc = A.f32(8)
        rc = A.f32(16)
        bpc = P.buf()
        P.dma("sp", lambda e: e.dma_start(out=psc, in_=pscT_d), writes=[bpc])
        P.dma("sp", lambda e: e.dma_start(out=rc, in_=c_rc_d), writes=[bpc])
        tl = dict(xt=[A.f32(D), A.f32(D)], bxt=P.bufs(2), yb=[A.bf16(D), A.bf16(D)], byb=P.bufs(2),
                  junk=A.f32(D), bjunk=P.buf(), ss=A.f32(2), rs=A.f32(2), bss=P.buf())
        uT = [A.f32(SEQ) for _ in range(2)]
        buT = P.bufs(2)
        sA = [A.f32(SEQ) for _ in range(2)]
        bsA = P.bufs(2)
        res = [tl["junk"], A.f32(D)]
        bres = [tl["bjunk"], P.buf()]
        h2_tiles = bH[id(H2)]
        xs_zero_begin()
        for s in range(2):
            seq_norm_transpose(H2, h2_tiles, s, gbt, bg, yT, byT, tl)
            for c in range(8):
                g = c // 2
                w = 2 << g
                u_, bu_ = uT[c % 2], buT[c % 2]
                xs_zero_some(5, after=[bu_])
                for q in range(4):
                    bk = (c * 4 + q) % 4
                    for k in range(8):
                        P.op("pe", lambda e, k=k, c=c, q=q, bk=bk: e.matmul(bank(bk), lhsT=win[:, k, c * 128:(c + 1) * 128], rhs=yT[:, k, q * 512:(q + 1) * 512],
                                                                         start=(k == 0), stop=(k == 7)), reads=[bwin] + byT[4 * q:4 * q + 4], writes=[pb[bk]])
                    P.op("act", lambda e, u_=u_, q=q, bk=bk: e.copy(out=u_[:, q * 512:(q + 1) * 512], in_=bank(bk)), reads=[pb[bk]], writes=[bu_])
                cur, bcur = u_, bu_
                sft = 1
                pp = 0
                while sft < w:
                    nx, bnx = sA[pp], bsA[pp]
                    P.op("dve", lambda e, nx=nx, cur=cur, sft=sft: e.tensor_copy(out=nx[:, 0:sft], in_=cur[:, 0:sft]), reads=[bcur], writes=[bnx])
                    P.op("dve", lambda e, nx=nx, cur=cur, sft=sft: e.tensor_tensor(out=nx[:, sft:SEQ], in0=cur[:, sft:SEQ], in1=cur[:, 0:SEQ - sft], op=ALU.add),
                         reads=[bcur], writes=[bnx])
                    cur, bcur = nx, bnx
                    pp = 1 - pp
                    sft *= 2
                P.op("dve", lambda e, cur=cur, u_=u_, c=c, w=w: e.scalar_tensor_tensor(out=plT[:, c % 4, :], in0=cur, scalar=1.0 / w, in1=u_, op0=ALU.mult, op1=ALU.subtract),
                     reads=[bcur, bu_], writes=[bplT[c % 4]])
                tmpc = sA[pp]
                btmpc = bsA[pp]
                P.op("dve", lambda e, cur=cur, tmpc=tmpc, w=w: e.tensor_tensor(out=tmpc[:, 0:w - 1], in0=cur[:, 0:w - 1], in1=rc[:, 0:w - 1], op=ALU.mult),
                     reads=[bcur, bpc], writes=[btmpc])
                P.op("dve", lambda e, tmpc=tmpc, u_=u_, c=c, w=w: e.tensor_tensor(out=plT[:, c % 4, 0:w - 1], in0=tmpc[:, 0:w - 1], in1=u_[:, 0:w - 1], op=ALU.subtract),
                     reads=[btmpc, bu_], writes=[bplT[c % 4]])
                if c % 2 == 1:
                    for eo in range(2):
                        co = 2 * g + eo
                        for q in range(4):
                            bk = 4 + (co * 4 + q) % 4
                            for ci in range(2):
                                P.op("pe", lambda e, g=g, eo=eo, ci=ci, q=q, bk=bk: e.matmul(
                                    bank(bk), lhsT=wgp[:, g, ci, eo * 128:(eo + 1) * 128], rhs=plT[:, (2 * g + ci) % 4, q * 512:(q + 1) * 512],
                                    start=(ci == 0), stop=(ci == 1)), reads=[bwgp, bplT[(2 * g + ci) % 4]], writes=[pb[bk]])
                            P.op("act", lambda e, co=co, q=q, bk=bk: e.activation(out=zT[:, co, q * 512:(q + 1) * 512], in_=bank(bk), func=AF.Copy, scale=psc[:, co:co + 1]),
                                 reads=[pb[bk], bpc], writes=[bzT])
            for t in range(16):
                gt = s * 16 + t
                b0 = 0 if t % 2 == 0 else 2
                for hf in range(2):
                    for k in range(8):
                        P.op("pe", lambda e, hf=hf, k=k, b0=b0, t=t: e.matmul(bank(b0 + hf), lhsT=zT[:, k, t * 128:(t + 1) * 128], rhs=wou[:, k, hf * 512:(hf + 1) * 512],
                                                                          start=(k == 0), stop=(k == 7)), reads=[bzT, bwou], writes=[pb[b0 + hf]])
                xt, bx = tl["xt"][t % 2], tl["bxt"][t % 2]
                P.dma("sp", lambda e, xt=xt, gt=gt: e.dma_start(out=xt, in_=H2[gt * 128:(gt + 1) * 128, :]), reads=[h2_tiles[gt]], writes=[bx])
                rs_, brs_ = res[t % 2], bres[t % 2]
                P.op("dve", lambda e, rs_=rs_, xt=xt, b0=b0: e.tensor_tensor(out=rs_, in0=xt, in1=bank(b0, 2), op=ALU.add),
                     reads=[bx, pb[b0], pb[b0 + 1]], writes=[brs_])
                P.dma("sp", lambda e, rs_=rs_, gt=gt: e.dma_start(out=H3[gt * 128:(gt + 1) * 128, :], in_=rs_), reads=[brs_], writes=[bH[id(H3)][gt]])
        P.barrier()

    phases = [("attn", attn_phase), ("moe0", lambda: moe_phase(0, H1, H2, False)),
              ("pool", pool_phase), ("moe1", lambda: moe_phase(1, H3, None, True))]
    for name, fn in phases:
        fn()
        if stop_after == name:
            break
    stats = P.finalize()
    P.es.close()
    return nc, stats


def make_constants():
    k = np.arange(128)[:, None]
    q = np.arange(128)[None, :]
    m = np.concatenate([(k <= q), (k >= q)], axis=1).astype(np.float32)
    mask = np.concatenate([m, m], axis=1)
    inv_freq = (500000.0 ** (-(np.arange(8, dtype=np.float32) * 2.0 / 16.0))).astype(np.float32)
    return dict(
        c_identf=np.eye(128, dtype=np.float32),
        c_mask=mask,
        c_triu=(k < q).astype(np.float32),
        c_ones=np.ones((128, 128), np.float32),
        c_invf=np.broadcast_to(inv_freq[None, :], (128, 8)).copy(),
        c_eoff=np.broadcast_to((np.arange(16, dtype=np.float32) * CAP)[None, :], (128, 16)).copy(),
        c_rc=np.broadcast_to((1.0 / np.arange(1, 17, dtype=np.float32))[None, :], (128, 16)).copy(),
        c_tokp1=(np.arange(NT, dtype=np.float32)[None, :] * 128 + np.arange(128, dtype=np.float32)[:, None] + 1.0).astype(np.float32),
        c_slot=(np.arange(NEXP * CAP_T, dtype=np.float32)[None, :] * 128 + np.arange(128, dtype=np.float32)[:, None]).astype(np.float32),
    )


def make_in_maps(inputs, ncores=NCORES):
    f = lambda a: np.ascontiguousarray(np.asarray(a, dtype=np.float32))
    x = f(inputs["x"])
    pos = np.asarray(inputs["positions"]).astype(np.int32)
    shared = dict(
        norm_mix=f(inputs["norm_mix"]), norm_ffn=f(inputs["norm_ffn"]),
        norm_final=f(inputs["norm_final"]).reshape(1, D),
        attn_w_in=f(inputs["attn_w_in"])[0], attn_w_out=f(inputs["attn_w_out"])[0],
        pool_w_in=f(inputs["pool_w_in"])[0], pool_w_group=f(inputs["pool_w_group"])[0],
        pool_scaleT=np.ascontiguousarray(f(inputs["pool_scale"])[0].reshape(8, 128).T),
        pool_w_out=f(inputs["pool_w_out"])[0],
        router_group_w=f(inputs["router_group_w"]), router_group_b=f(inputs["router_group_b"]),
        router_expert_w=f(inputs["router_expert_w"]),
        router_expert_b=f(inputs["router_expert_b"]).reshape(2, 16),
        expert_w1=f(inputs["expert_w1"]), expert_w3=f(inputs["expert_w3"]), expert_w2=f(inputs["expert_w2"]),
    )
    shared.update(make_constants())
    maps = []
    for c in range(ncores):
        xs = x[2 * c:2 * c + 2].reshape(TPC, D)
        p = pos[2 * c:2 * c + 2].reshape(NT, 128).T
        m = dict(shared)
        m["x"] = np.ascontiguousarray(xs)
        m["posT"] = np.ascontiguousarray(p)
        maps.append(m)
    return maps


_CACHE = {}


def kernel(**inputs):
    if "nc" not in _CACHE:
        _CACHE["nc"] = build_program()[0]
    nc = _CACHE["nc"]
    maps = make_in_maps(inputs)
    res = run_bass_kernel_spmd(nc, maps, core_ids=list(range(NCORES)))
    outs = [np.asarray(r["out"]).reshape(2, SEQ, D) for r in res.results]
    return np.concatenate(outs, axis=0).astype(np.float32)
```
